# Optimizing a Trainium2 kernel written in Bass

```python
import math
import functools
import jax
import jax.numpy as jnp
from jax import lax
import numpy as np

D_MODEL = 1024
BATCH = 16
SEQ = 2048
DEPTH = 2

F32 = jnp.float32
GRID_W = 64
CTX_LEN = 256
N_BRANCH = 4
MIX_W = 512
HEAD_DIM = 64
RMS_EPS = 1e-6

HY_BANDS = 16
HY_EMB = 1 + 2 * HY_BANDS
HY_FFN = 64
HY_DECAY_TARGET = 1e-2
HY_SHORT_PCT = 0.3
HY_LONG_PCT = 1.5

RW_HEADS = MIX_W // HEAD_DIM
RW_DECAY_RANK = 64
RW_A_RANK = 64
RW_G_RANK = 128
RW_DECAY_SCALE = 0.606531
RW_GN_EPS = 64e-5
RW_SPLITS = (MIX_W, MIX_W, MIX_W, RW_DECAY_RANK, RW_DECAY_RANK, RW_A_RANK, RW_A_RANK, RW_G_RANK)
RW_IN = sum(RW_SPLITS)

S5_GROUP = 16
S5_GROUPS = MIX_W // S5_GROUP
S5_STATE = 64
S5_DT_MIN = 1e-3
S5_DT_MAX = 1e-1

GLA_HEADS = 4
GLA_DK = MIX_W // 2
GLA_DV = MIX_W
GLA_HK = GLA_DK // GLA_HEADS
GLA_HV = GLA_DV // GLA_HEADS
GLA_GATE_RANK = 16
GLA_GATE_NORM = 16.0
GLA_CHUNK = 64
GLA_SPLITS = (GLA_DK, GLA_DK, GLA_DV, GLA_DV, GLA_GATE_RANK, GLA_GATE_RANK)
GLA_IN = sum(GLA_SPLITS)

D_FF = 2816
N_EXPERTS = 8
TOP_K = 2
D_FF_EXPERT = 3584
MOE_BLOCK = 128
N_DENSE = (DEPTH + 1) // 2
N_MOE = DEPTH // 2

IN_SPLITS = (3 * MIX_W, RW_IN, MIX_W, GLA_IN, N_BRANCH * D_MODEL)
IN_TOTAL = sum(IN_SPLITS)

kernel_name = 'hybrid_hyena_rwkv7_s5_gla_moe_dit'


def _split(t, sizes):
    return jnp.split(t, np.cumsum(sizes)[:-1].tolist(), axis=-1)


def _rmsnorm(x, g):
    xf = x.astype(F32)
    return xf * lax.rsqrt(jnp.mean(xf * xf, axis=-1, keepdims=True) + RMS_EPS) * g


def _swiglu(h, w1, w3, w2):
    return (jax.nn.silu(h @ w1) * (h @ w3)) @ w2


def _flip(t):
    return jnp.flip(t, axis=1)


def _ident(t):
    return t


def _to_col_major(t):
    b, l, ch = t.shape
    rows = l // GRID_W
    return jnp.swapaxes(t.reshape(b, rows, GRID_W, ch), 1, 2).reshape(b, l, ch)


def _to_raster(t):
    b, l, ch = t.shape
    rows = l // GRID_W
    return jnp.swapaxes(t.reshape(b, GRID_W, rows, ch), 1, 2).reshape(b, l, ch)


def _conv3(u, w):
    up = jnp.pad(u, ((0, 0), (1, 1), (0, 0)))
    return up[:, :-2] * w[0] + up[:, 1:-1] * w[1] + up[:, 2:] * w[2]


def _hyena_filter(L, fw1, fb1, fw2, fb2, fw3, fb3):
    pos = jnp.arange(L, dtype=F32)
    t = pos / max(L - 1, 1)
    freqs = jnp.linspace(1e-4, HY_BANDS - 1, HY_BANDS, dtype=F32)
    ang = (2.0 * math.pi / L) * pos[:, None] * freqs[None, :]
    z = jnp.concatenate([t[:, None], jnp.cos(ang), -jnp.sin(ang)], axis=-1)
    hdn = jnp.sin(z @ fw1 + fb1)
    hdn = jnp.sin(hdn @ fw2 + fb2)
    filt = hdn @ fw3 + fb3
    rates = jnp.abs(jnp.linspace(math.log(HY_DECAY_TARGET) / HY_LONG_PCT,
                                 math.log(HY_DECAY_TARGET) / HY_SHORT_PCT, MIX_W, dtype=F32))
    rates = jnp.concatenate([rates, rates])
    return filt * jnp.exp(-t[:, None] * rates[None, :])


def _bidir_long_conv(u, filt, bias):
    b, L, ch = u.shape
    h_f, h_b = filt[:, :ch], filt[:, ch:]
    k = jnp.concatenate([h_f, jnp.zeros((1, ch), F32), h_b[:0:-1]], axis=0)
    U = jnp.fft.rfft(u, n=2 * L, axis=1)
    K = jnp.fft.rfft(k, n=2 * L, axis=0)
    y = jnp.fft.irfft(U * K[None], n=2 * L, axis=1)[:, :L]
    return y + u * bias


def _hyena_mixer(p, conv_w, conv_b, fw1, fb1, fw2, fb2, fw3, fb3, bias):
    u = _conv3(p.astype(F32), conv_w) + conv_b
    x0, x1, v = _split(u, (MIX_W, MIX_W, MIX_W))
    filt = _hyena_filter(p.shape[1], fw1, fb1, fw2, fb2, fw3, fb3)
    return x0 * _bidir_long_conv(v * x1, filt, bias)


def _centred_shift(p, mu):
    pp = jnp.pad(p, ((0, 0), (1, 1), (0, 0)))
    return p + mu[0] * (pp[:, :-2] - p) + mu[1] * (pp[:, 2:] - p)


def _rwkv7_inputs(p, mu, w0, w_up, a0, a_up, g_up, k_k, k_a):
    b, L, _ = p.shape
    p = _centred_shift(p.astype(F32), mu)
    r, k, v, wd_f, wd_b, ad_f, ad_b, gd = _split(p, RW_SPLITS)
    heads = lambda t: t.reshape(b, L, RW_HEADS, HEAD_DIM)
    g = jax.nn.sigmoid(gd) @ g_up
    kk = heads(k * k_k)
    kk = kk / jnp.maximum(jnp.sqrt(jnp.sum(kk * kk, axis=-1, keepdims=True)), 1e-12)
    dirs = []
    for d, (wd, ad) in enumerate(((wd_f, ad_f), (wd_b, ad_b))):
        logw = -RW_DECAY_SCALE * jax.nn.sigmoid(w0[d] + jnp.tanh(wd) @ w_up[d])
        a = jax.nn.sigmoid(a0[d] + ad @ a_up[d])
        k_d = k * (1.0 + (a - 1.0) * k_a)
        dirs.append((heads(jnp.exp(logw)), heads(k_d), heads(a)))
    return heads(r), heads(v), kk, g, dirs


def _rwkv7_scan(s0, seq, d, reverse):
    r, v, kk, _, dirs = seq
    w, k, a = dirs[d]

    def step(s, inp):
        r_t, w_t, k_t, v_t, kk_t, a_t = inp
        s = (s * w_t[:, :, None, :]
             - jnp.einsum('bhvk,bhk->bhv', s, kk_t)[..., None] * (kk_t * a_t)[:, :, None, :]
             + v_t[..., None] * k_t[:, :, None, :])
        return s, jnp.einsum('bhvk,bhk->bhv', s, r_t)

    xs = tuple(jnp.swapaxes(t, 0, 1) for t in (r, w, k, v, kk, a))
    s_fin, o = lax.scan(step, s0, xs, reverse=reverse)
    return s_fin, jnp.swapaxes(o, 0, 1)


def _rwkv7_readout(o, seq, r_k, ln_g, ln_b):
    r, v, _, g, dirs = seq
    b, L = o.shape[:2]
    mean = jnp.mean(o, axis=-1, keepdims=True)
    var = jnp.mean(jnp.square(o - mean), axis=-1, keepdims=True)
    on = ((o - mean) * lax.rsqrt(var + RW_GN_EPS)).reshape(b, L, MIX_W) * ln_g + ln_b
    bonus = sum(jnp.sum(r * k_d * r_k, axis=-1, keepdims=True) * v for (_, k_d, _) in dirs)
    return (on + bonus.reshape(b, L, MIX_W)) * g


def _rwkv7_mixer(pc, pl, want_ctx, mu, w0, w_up, a0, a_up, g_up, k_k, k_a, r_k, ln_g, ln_b):
    seq_c = _rwkv7_inputs(pc, mu, w0, w_up, a0, a_up, g_up, k_k, k_a)
    seq_l = _rwkv7_inputs(pl, mu, w0, w_up, a0, a_up, g_up, k_k, k_a)
    s0 = jnp.zeros((pl.shape[0], RW_HEADS, HEAD_DIM, HEAD_DIM), F32)
    o_c = 0.0
    o_l = 0.0
    for d in range(2):
        s_c, oc = _rwkv7_scan(s0, seq_c, d, d == 1)
        _, ol = _rwkv7_scan(s_c, seq_l, d, d == 1)
        o_c = o_c + oc
        o_l = o_l + ol
    y_l = _rwkv7_readout(o_l, seq_l, r_k, ln_g, ln_b)
    y_c = _rwkv7_readout(o_c, seq_c, r_k, ln_g, ln_b) if want_ctx else None
    return y_c, y_l


def _s5_discretise(a_re, a_im, log_dt):
    dt = jnp.exp(log_dt)[:, None]
    mag = jnp.exp(a_re * dt)
    ang = a_im * dt
    ab_re, ab_im = mag * jnp.cos(ang), mag * jnp.sin(ang)
    nr, ni = ab_re - 1.0, ab_im
    den = a_re * a_re + a_im * a_im
    return ab_re, ab_im, (nr * a_re + ni * a_im) / den, (ni * a_re - nr * a_im) / den


def _cplx_affine(e1, e2):
    a1r, a1i, b1r, b1i = e1
    a2r, a2i, b2r, b2i = e2
    return (a1r * a2r - a1i * a2i, a1r * a2i + a1i * a2r,
            a2r * b1r - a2i * b1i + b2r, a2r * b1i + a2i * b1r + b2i)


def _s5_scan(ab_re, ab_im, x_re, x_im, h_re, h_im, reverse):
    if reverse:
        x_re, x_im = _flip(x_re), _flip(x_im)
    x_re = x_re.at[:, 0].add(ab_re * h_re - ab_im * h_im)
    x_im = x_im.at[:, 0].add(ab_re * h_im + ab_im * h_re)
    L = x_re.shape[1]
    a_re = jnp.broadcast_to(ab_re, (1, L) + ab_re.shape)
    a_im = jnp.broadcast_to(ab_im, (1, L) + ab_im.shape)
    _, _, s_re, s_im = lax.associative_scan(_cplx_affine, (a_re, a_im, x_re, x_im), axis=1)
    if reverse:
        s_re, s_im = _flip(s_re), _flip(s_im)
    return s_re, s_im


def _s5_readout(s_re, s_im, c_re, c_im):
    return jnp.einsum('gpn,blgn->blgp', c_re, s_re) - jnp.einsum('gpn,blgn->blgp', c_im, s_im)


def _s5_output(y, u, d_skip, glu_w, glu_b):
    y = jax.nn.gelu(y.reshape(u.shape) + d_skip * u)
    return y * jax.nn.sigmoid(y @ glu_w + glu_b)


def _s5_mixer(pc, pl, want_ctx, a_re, a_im, log_dt, b_re, b_im, c_re, c_im, d_skip, glu_w, glu_b):
    def drive(p):
        u = p.astype(F32)
        ug = u.reshape(u.shape[0], u.shape[1], S5_GROUPS, S5_GROUP)
        return u, jnp.einsum('blgp,gnp->blgn', ug, b_re), jnp.einsum('blgp,gnp->blgn', ug, b_im)

    uc, bc_re, bc_im = drive(pc)
    ul, bl_re, bl_im = drive(pl)
    zero = jnp.zeros((ul.shape[0], S5_GROUPS, S5_STATE), F32)
    y_c = 0.0
    y_l = 0.0
    for d in range(2):
        rev = d == 1
        ab_re, ab_im, f_re, f_im = _s5_discretise(a_re[d], a_im[d], log_dt[d])
        sc_re, sc_im = _s5_scan(ab_re, ab_im, f_re * bc_re - f_im * bc_im, f_re * bc_im + f_im * bc_re,
                                zero, zero, rev)
        edge = 0 if rev else -1
        sl_re, sl_im = _s5_scan(ab_re, ab_im, f_re * bl_re - f_im * bl_im, f_re * bl_im + f_im * bl_re,
                                sc_re[:, edge], sc_im[:, edge], rev)
        y_l = y_l + _s5_readout(sl_re, sl_im, c_re, c_im)
        if want_ctx:
            y_c = y_c + _s5_readout(sc_re, sc_im, c_re, c_im)
    out_l = _s5_output(y_l, ul, d_skip, glu_w, glu_b)
    out_c = _s5_output(y_c, uc, d_skip, glu_w, glu_b) if want_ctx else None
    return out_c, out_l


def _gla_prep(p, gate_up, gate_b):
    b, L, _ = p.shape
    q, k, v, g, gd_f, gd_b = _split(p.astype(F32), GLA_SPLITS)
    hk = lambda t: t.reshape(b, L, GLA_HEADS, GLA_HK)
    logs = tuple(hk(jax.nn.log_sigmoid(gd @ gate_up[d] + gate_b[d]) / GLA_GATE_NORM)
                 for d, gd in enumerate((gd_f, gd_b)))
    return hk(q) * (GLA_HK ** -0.5), hk(k), v.reshape(b, L, GLA_HEADS, GLA_HV), g, logs


def _gla_chunked(q, k, v, logg, s0):
    b, L, h, dk = q.shape
    dv = v.shape[-1]
    C = min(GLA_CHUNK, L)
    n = L // C
    q, k, v, logg = (t.reshape(b, n, C, h, t.shape[-1]) for t in (q, k, v, logg))
    cum = jnp.cumsum(logg, axis=2)
    ref = cum[:, :, C // 2 - 1:C // 2]
    last = cum[:, :, C - 1:]
    scores = jnp.einsum('bnihd,bnjhd->bnhij', q * jnp.exp(cum - ref), k * jnp.exp(ref - cum))
    scores = jnp.where(jnp.tril(jnp.ones((C, C), bool)), scores, 0.0)
    o = jnp.einsum('bnhij,bnjhe->bnihe', scores, v)
    u = jnp.einsum('bnjhd,bnjhe->bnhde', k * jnp.exp(last - cum), v)
    decay = jnp.exp(last[:, :, 0])

    def step(s, inp):
        u_n, g_n = inp
        return s * g_n[..., None] + u_n, s

    s_fin, s_start = lax.scan(step, s0, (jnp.swapaxes(u, 0, 1), jnp.swapaxes(decay, 0, 1)))
    o = o + jnp.einsum('bnihd,nbhde->bnihe', q * jnp.exp(cum), s_start)
    return o.reshape(b, L, h, dv), s_fin


def _gla_mixer(pc, pl, want_ctx, gate_up, gate_b, norm_g):
    qc, kc, vc, gc, lc = _gla_prep(pc, gate_up, gate_b)
    ql, kl, vl, gl, ll = _gla_prep(pl, gate_up, gate_b)
    s0 = jnp.zeros((pl.shape[0], GLA_HEADS, GLA_HK, GLA_HV), F32)
    o_c = 0.0
    o_l = 0.0
    for d in range(2):
        fl = _flip if d == 1 else _ident
        oc, s_c = _gla_chunked(fl(qc), fl(kc), fl(vc), fl(lc[d]), s0)
        ol, _ = _gla_chunked(fl(ql), fl(kl), fl(vl), fl(ll[d]), s_c)
        o_c = o_c + fl(oc)
        o_l = o_l + fl(ol)

    def readout(o, g):
        b, L = o.shape[:2]
        on = o * lax.rsqrt(jnp.mean(o * o, axis=-1, keepdims=True) + RMS_EPS) * norm_g
        return on.reshape(b, L, GLA_DV) * jax.nn.silu(g)

    return (readout(o_c, gc) if want_ctx else None), readout(o_l, gl)


def _merge(branches, gate_pre, w_branch, w_out):
    g = jax.nn.sigmoid(gate_pre.astype(F32))
    acc = 0.0
    for m, y in enumerate(branches):
        acc = acc + g[..., m * D_MODEL:(m + 1) * D_MODEL] * (y @ w_branch[m])
    return acc @ w_out


def _mixer_block(hc, hl, want_ctx, lp):
    pc = hc @ lp['w_in']
    pl = hl @ lp['w_in']
    hy_c, rw_c, s5_c, gla_c, gate_c = _split(pc, IN_SPLITS)
    hy_l, rw_l, s5_l, gla_l, gate_l = _split(pl, IN_SPLITS)
    y_rw_c, y_rw_l = _rwkv7_mixer(rw_c, rw_l, want_ctx, *lp['rw'])
    y_s5_c, y_s5_l = _s5_mixer(s5_c, s5_l, want_ctx, *lp['s5'])
    y_gla_c, y_gla_l = _gla_mixer(gla_c, _to_col_major(gla_l), want_ctx, *lp['gla'])
    branches_l = (_hyena_mixer(hy_l, *lp['hy']), y_rw_l, y_s5_l, _to_raster(y_gla_l))
    out_l = _merge(branches_l, gate_l, lp['w_branch'], lp['w_out'])
    if not want_ctx:
        return None, out_l
    branches_c = (_hyena_mixer(hy_c, *lp['hy']), y_rw_c, y_s5_c, y_gla_c)
    return _merge(branches_c, gate_c, lp['w_branch'], lp['w_out']), out_l


def _moe_ffn(h, router_w, router_b, w1, w3, w2):
    shape = h.shape
    tok = h.reshape(-1, shape[-1])
    n_tok = tok.shape[0]
    logits = tok.astype(F32) @ router_w + router_b
    top_v, top_i = lax.top_k(logits, TOP_K)
    gates = jax.nn.softmax(top_v, axis=-1)
    n_assign = n_tok * TOP_K
    e_flat = top_i.reshape(-1)
    order = jnp.argsort(e_flat)
    e_sorted = e_flat[order]
    counts = jnp.bincount(e_flat, length=N_EXPERTS)
    padded = (counts + MOE_BLOCK - 1) // MOE_BLOCK * MOE_BLOCK
    start = jnp.cumsum(counts) - counts
    pend = jnp.cumsum(padded)
    pstart = pend - padded
    dest = pstart[e_sorted] + jnp.arange(n_assign) - start[e_sorted]
    n_blocks = -(-n_assign // MOE_BLOCK) + N_EXPERTS
    row_tok = jnp.full((n_blocks * MOE_BLOCK,), n_tok, jnp.int32).at[dest].set((order // TOP_K).astype(jnp.int32))
    block_exp = jnp.minimum(jnp.searchsorted(pend, jnp.arange(n_blocks) * MOE_BLOCK, side='right'), N_EXPERTS - 1)
    tok_pad = jnp.concatenate([tok, jnp.zeros((1, shape[-1]), tok.dtype)], axis=0)
    xb = tok_pad[row_tok].reshape(n_blocks, MOE_BLOCK, shape[-1])
    yb = lax.map(lambda a: _swiglu(a[0], w1[a[1]], w3[a[1]], w2[a[1]]), (xb, block_exp))
    yb = yb.reshape(n_blocks * MOE_BLOCK, shape[-1])
    dest_orig = jnp.zeros((n_assign,), dest.dtype).at[order].set(dest)
    y = yb[dest_orig].reshape(n_tok, TOP_K, shape[-1])
    return jnp.einsum('tkd,tk->td', y, gates.astype(y.dtype)).reshape(shape)


def _trunk_layer(h, hc, mod_l, mod_c, g_mix, g_ffn, lp, ffn, last):
    sh1, sc1, gt1, sh2, sc2, gt2 = jnp.split(mod_l[:, None, :], 6, axis=-1)
    csh1, csc1, cgt1, csh2, csc2, cgt2 = jnp.split(mod_c, 6, axis=-1)
    yc, yl = _mixer_block(_rmsnorm(hc, g_mix) * (1.0 + csc1) + csh1,
                          _rmsnorm(h, g_mix) * (1.0 + sc1) + sh1, not last, lp)
    h = h + gt1 * yl
    h = h + gt2 * ffn(_rmsnorm(h, g_ffn) * (1.0 + sc2) + sh2)
    if not last:
        hc = hc + cgt1 * yc
        hc = hc + cgt2 * ffn(_rmsnorm(hc, g_ffn) * (1.0 + csc2) + csh2)
    return h, hc


def setup_inputs(seed: int = 0) -> dict:
    key = jax.random.key(seed)
    ks = jax.random.split(key, 64)
    ctr = [0]

    def nxt():
        k = ks[ctr[0]]
        ctr[0] += 1
        return k

    def nrm(shape, scale):
        return scale * jax.random.normal(nxt(), shape, F32)

    def unif(shape, lo, hi):
        return jax.random.uniform(nxt(), shape, F32, lo, hi)

    D = D_MODEL
    G, N = S5_GROUPS, S5_STATE
    a_im0 = jnp.broadcast_to(math.pi * jnp.arange(N, dtype=F32), (DEPTH, 2, G, N))
    return {
        'x': nrm((BATCH, SEQ, D), 1.0),
        'c': nrm((BATCH, D), 1.0),
        'ctx': nrm((BATCH, CTX_LEN, D), 1.0),
        'c_ctx': nrm((D,), 1.0),
        'ada_w': nrm((DEPTH, D, 6 * D), 0.5 * D ** -0.5),
        'ada_b': nrm((DEPTH, 6 * D), 0.02),
        'norm_mix_g': 1.0 + nrm((DEPTH, D), 0.02),
        'norm_ffn_g': 1.0 + nrm((DEPTH, D), 0.02),
        'w_in': nrm((DEPTH, D, IN_TOTAL), D ** -0.5),
        'hy_conv_w': nrm((DEPTH, 3, 3 * MIX_W), 3 ** -0.5),
        'hy_conv_b': nrm((DEPTH, 3 * MIX_W), 0.02),
        'hy_f_w1': nrm((DEPTH, HY_EMB, HY_FFN), HY_EMB ** -0.5),
        'hy_f_b1': nrm((DEPTH, HY_FFN), 0.1),
        'hy_f_w2': nrm((DEPTH, HY_FFN, HY_FFN), HY_FFN ** -0.5),
        'hy_f_b2': nrm((DEPTH, HY_FFN), 0.1),
        'hy_f_w3': nrm((DEPTH, HY_FFN, 2 * MIX_W), 0.02),
        'hy_f_b3': nrm((DEPTH, 2 * MIX_W), 0.01),
        'hy_bias': nrm((DEPTH, MIX_W), 0.5),
        'rw_mu': unif((DEPTH, 2, RW_IN), 0.0, 0.5),
        'rw_w0': unif((DEPTH, 2, MIX_W), -1.0, 2.0),
        'rw_w_up': nrm((DEPTH, 2, RW_DECAY_RANK, MIX_W), 0.1),
        'rw_a0': nrm((DEPTH, 2, MIX_W), 0.3),
        'rw_a_up': nrm((DEPTH, 2, RW_A_RANK, MIX_W), 0.1),
        'rw_g_up': nrm((DEPTH, RW_G_RANK, MIX_W), RW_G_RANK ** -0.5),
        'rw_k_k': 0.85 + nrm((DEPTH, MIX_W), 0.02),
        'rw_k_a': 1.0 + nrm((DEPTH, MIX_W), 0.02),
        'rw_r_k': nrm((DEPTH, RW_HEADS, HEAD_DIM), 0.1),
        'rw_ln_g': 1.0 + nrm((DEPTH, MIX_W), 0.02),
        'rw_ln_b': nrm((DEPTH, MIX_W), 0.02),
        's5_a_re': -0.5 + nrm((DEPTH, 2, G, N), 0.01),
        's5_a_im': a_im0 + nrm((DEPTH, 2, G, N), 0.01),
        's5_log_dt': unif((DEPTH, 2, G), math.log(S5_DT_MIN), math.log(S5_DT_MAX)),
        's5_b_re': nrm((DEPTH, G, N, S5_GROUP), (2 * S5_GROUP) ** -0.5),
        's5_b_im': nrm((DEPTH, G, N, S5_GROUP), (2 * S5_GROUP) ** -0.5),
        's5_c_re': nrm((DEPTH, G, S5_GROUP, N), (2 * N) ** -0.5),
        's5_c_im': nrm((DEPTH, G, S5_GROUP, N), (2 * N) ** -0.5),
        's5_d': nrm((DEPTH, MIX_W), 1.0),
        's5_glu_w': nrm((DEPTH, MIX_W, MIX_W), MIX_W ** -0.5),
        's5_glu_b': nrm((DEPTH, MIX_W), 0.02),
        'gla_gate_up': nrm((DEPTH, 2, GLA_GATE_RANK, GLA_DK), GLA_GATE_RANK ** -0.5),
        'gla_gate_b': nrm((DEPTH, 2, GLA_DK), 0.1),
        'gla_norm_g': 1.0 + nrm((DEPTH, GLA_HV), 0.02),
        'w_branch': nrm((DEPTH, N_BRANCH, MIX_W, D), MIX_W ** -0.5),
        'w_out': nrm((DEPTH, D, D), D ** -0.5),
        'ffn_w1': nrm((N_DENSE, D, D_FF), D ** -0.5),
        'ffn_w3': nrm((N_DENSE, D, D_FF), D ** -0.5),
        'ffn_w2': nrm((N_DENSE, D_FF, D), D_FF ** -0.5),
        'moe_router_w': nrm((N_MOE, D, N_EXPERTS), D ** -0.5),
        'moe_router_b': nrm((N_MOE, N_EXPERTS), 0.01),
        'moe_w1': nrm((N_MOE, N_EXPERTS, D, D_FF_EXPERT), D ** -0.5),
        'moe_w3': nrm((N_MOE, N_EXPERTS, D, D_FF_EXPERT), D ** -0.5),
        'moe_w2': nrm((N_MOE, N_EXPERTS, D_FF_EXPERT, D), D_FF_EXPERT ** -0.5),
        'final_norm_g': 1.0 + nrm((D,), 0.02),
    }


def reference(x, c, ctx, c_ctx, ada_w, ada_b, norm_mix_g, norm_ffn_g, w_in,
              hy_conv_w, hy_conv_b, hy_f_w1, hy_f_b1, hy_f_w2, hy_f_b2, hy_f_w3, hy_f_b3, hy_bias,
              rw_mu, rw_w0, rw_w_up, rw_a0, rw_a_up, rw_g_up, rw_k_k, rw_k_a, rw_r_k, rw_ln_g, rw_ln_b,
              s5_a_re, s5_a_im, s5_log_dt, s5_b_re, s5_b_im, s5_c_re, s5_c_im, s5_d, s5_glu_w, s5_glu_b,
              gla_gate_up, gla_gate_b, gla_norm_g, w_branch, w_out,
              ffn_w1, ffn_w3, ffn_w2, moe_router_w, moe_router_b, moe_w1, moe_w3, moe_w2,
              final_norm_g):
    s_lat = jax.nn.silu(c.astype(F32))
    s_ctx = jax.nn.silu(c_ctx.astype(F32))
    h = x.astype(F32)
    hc = ctx.astype(F32)
    for l in range(DEPTH):
        lp = {
            'w_in': w_in[l],
            'hy': (hy_conv_w[l], hy_conv_b[l], hy_f_w1[l], hy_f_b1[l], hy_f_w2[l], hy_f_b2[l],
                   hy_f_w3[l], hy_f_b3[l], hy_bias[l]),
            'rw': (rw_mu[l], rw_w0[l], rw_w_up[l], rw_a0[l], rw_a_up[l], rw_g_up[l], rw_k_k[l],
                   rw_k_a[l], rw_r_k[l], rw_ln_g[l], rw_ln_b[l]),
            's5': (s5_a_re[l], s5_a_im[l], s5_log_dt[l], s5_b_re[l], s5_b_im[l], s5_c_re[l],
                   s5_c_im[l], s5_d[l], s5_glu_w[l], s5_glu_b[l]),
            'gla': (gla_gate_up[l], gla_gate_b[l], gla_norm_g[l]),
            'w_branch': w_branch[l],
            'w_out': w_out[l],
        }
        i = l // 2
        if l % 2 == 0:
            ffn = functools.partial(_swiglu, w1=ffn_w1[i], w3=ffn_w3[i], w2=ffn_w2[i])
        else:
            ffn = functools.partial(_moe_ffn, router_w=moe_router_w[i], router_b=moe_router_b[i],
                                    w1=moe_w1[i], w3=moe_w3[i], w2=moe_w2[i])
        mod_l = s_lat @ ada_w[l] + ada_b[l]
        mod_c = s_ctx @ ada_w[l] + ada_b[l]
        h, hc = _trunk_layer(h, hc, mod_l, mod_c, norm_mix_g[l], norm_ffn_g[l], lp, ffn, l == DEPTH - 1)
    return _rmsnorm(h, final_norm_g).astype(x.dtype)
```

```python
import math
from contextlib import ExitStack
import numpy as np
import concourse.bass as bass
import concourse.mybir as mybir
from concourse.bass_utils import run_bass_kernel_spmd

F32 = mybir.dt.float32
BF16 = mybir.dt.bfloat16
I32 = mybir.dt.int32
ALU = mybir.AluOpType
AF = mybir.ActivationFunctionType
AX = mybir.AxisListType

D = 1024
TC = 256
TL = 2048
T = TC + TL
NB = 2
NCORE = 8
DEPTH = 2
MIXW = 512
IN_TOTAL = 9632
OFF_HY, OFF_RW, OFF_S5, OFF_GLA, OFF_GATE = 0, 1536, 3456, 3968, 5536
D_FF = 2816
D_FFE = 3584
NEXP = 8
RMS_EPS = 1e-6


class Res:
    __slots__ = ("name", "lw", "rd", "psum", "dram", "ws")

    def __init__(self, name, psum=False, dram=False):
        self.name = name
        self.lw = None
        self.rd = {}
        self.psum = psum
        self.dram = dram
        self.ws = {}


class V:
    __slots__ = ("ap", "res")

    def __init__(self, ap, res):
        self.ap = ap
        self.res = res

    def __getitem__(self, idx):
        return V(self.ap[idx], self.res)

    def bc(self, shape):
        return V(self.ap.to_broadcast(shape), self.res)

    def rr(self, pat, **kw):
        return V(self.ap.rearrange(pat, **kw), self.res)


class KB:
    ENG = ("pe", "act", "dve", "pool", "sp")
    NS = 8

    def __init__(self, nc):
        self.nc = nc
        self.es = ExitStack()
        self.e = dict(pe=nc.tensor, act=nc.scalar, dve=nc.vector, pool=nc.gpsimd, sp=nc.sync)
        self.cnt = {k: 0 for k in self.ENG}
        self.sem = {k: self.es.enter_context(nc.semaphore("s_" + k)) for k in self.ENG}
        self.waited = {k: {} for k in self.ENG}
        self.dq = {}
        for q in ("sp", "act", "pool"):
            self.dq[q] = dict(i=0, sems=[self.es.enter_context(nc.semaphore("d_%s%d" % (q, s))) for s in range(self.NS)],
                              vals=[0] * self.NS)
        self.scopes = []
        self.dram_res = []
        self.marks = []
        self.uid = 0
        self.n_ins = 0

    def _nm(self, name):
        self.uid += 1
        return "%s_%d" % (name, self.uid)

    def _stack(self):
        return self.scopes[-1] if self.scopes else self.es

    def sb(self, name, shape, dtype=F32):
        t = self._stack().enter_context(self.nc.sbuf_tensor(self._nm(name), list(shape), dtype))
        return V(t[tuple(slice(None) for _ in shape)], Res(name))

    def ps(self, name, shape, dtype=F32):
        t = self._stack().enter_context(self.nc.psum_tensor(self._nm(name), list(shape), dtype))
        return V(t[tuple(slice(None) for _ in shape)], Res(name, psum=True))

    def ps_split(self, name, nbanks, width):
        out = []
        for bk in range(nbanks):
            t = self.ps("%s_b%d" % (name, bk), [128, 512])
            for j in range(512 // width):
                out.append(V(t.ap[:, j * width:(j + 1) * width], Res("%s_%d_%d" % (name, bk, j))))
        return out

    def dram(self, name, shape, dtype=F32, kind="Internal"):
        t = self.nc.dram_tensor(name, list(shape), dtype, kind=kind)
        r = Res(name, dram=True)
        self.dram_res.append(r)
        return V(t.ap(), r)

    def push(self):
        st = ExitStack()
        self.scopes.append(st)
        return st

    def pop(self):
        if len(self.scopes) == 1:
            self.marks.append(dict(self.cnt))
        self.barrier()
        st = self.scopes.pop()
        st.close()

    def _semof(self, key):
        if isinstance(key, str):
            return self.sem[key]
        _, q, s = key
        return self.dq[q]["sems"][s]

    def _wait(self, x, ev):
        key, val = ev
        if x == "pe" and key == "pe":
            return
        if self.waited[x].get(key, 0) >= val:
            return
        self.e[x].wait_ge(self._semof(key), val)
        self.waited[x][key] = val
        self.n_ins += 1

    def _deps(self, reads, writes):
        evs = {}

        def add(ev):
            if ev is None:
                return
            k, v = ev
            if evs.get(k, 0) < v:
                evs[k] = v
        for r in reads:
            add(r.lw)
            for k, v in r.ws.items():
                add((k, v))
            if r.psum:
                for k, v in r.rd.items():
                    add((k, v))
        for w in writes:
            if not w.dram:
                add(w.lw)
            for k, v in w.rd.items():
                add((k, v))
        return list(evs.items())

    def _commit(self, ev, reads, writes):
        k, v = ev
        for r in reads:
            if r.rd.get(k, 0) < v:
                r.rd[k] = v
        for w in writes:
            w.lw = ev
            if w.dram:
                if w.ws.get(k, 0) < v:
                    w.ws[k] = v
            else:
                w.rd = {}

    def op(self, x, fn, reads, writes, *args, **kw):
        for ev in self._deps(reads, writes):
            self._wait(x, ev)
        ins = getattr(self.e[x], fn)(*args, **kw)
        self.cnt[x] += 1
        ev = (x, self.cnt[x])
        ins.then_inc(self.sem[x], 1)
        self._commit(ev, reads, writes)
        self.n_ins += 1
        return ins

    def dma(self, q, out, in_, **kw):
        d = self.dq[q]
        s = d["i"] % self.NS
        d["i"] += 1
        key = ("d", q, s)
        if d["vals"][s] > 0:
            self._wait(q, (key, d["vals"][s]))
        for ev in self._deps([in_.res], [out.res]):
            self._wait(q, ev)
        ins = self.e[q].dma_start(out=out.ap, in_=in_.ap, **kw)
        d["vals"][s] += 16
        ins.then_inc(d["sems"][s], 16)
        self._commit((key, d["vals"][s]), [in_.res], [out.res])
        self.n_ins += 1

    def barrier(self):
        evs = [(k, self.cnt[k]) for k in self.ENG if self.cnt[k] > 0]
        for q, d in self.dq.items():
            for s in range(self.NS):
                if d["vals"][s] > 0:
                    evs.append((("d", q, s), d["vals"][s]))
        for x in self.ENG:
            for ev in evs:
                if ev[0] == x:
                    continue
                self._wait(x, ev)
        for r in self.dram_res:
            r.ws = {}
            r.rd = {}

    R32 = False

    def mm(self, out, lhsT, rhs, start=True, stop=True, r32=None):
        la, ra = lhsT.ap, rhs.ap
        if (self.R32 if r32 is None else r32) and la.dtype == F32 and ra.dtype == F32:
            la = la.bitcast(mybir.dt.float32r)
            ra = ra.bitcast(mybir.dt.float32r)
        self.op("pe", "matmul", [lhsT.res, rhs.res], [out.res], out.ap, la, ra, start=start, stop=stop)

    def tr(self, out, in_, ident):
        self.op("pe", "transpose", [in_.res, ident.res], [out.res], out.ap, in_.ap, ident.ap)

    def act(self, out, in_, func, bias=None, scale=None, x="act"):
        rd = [in_.res]
        kw = {}
        if bias is not None:
            if isinstance(bias, V):
                rd.append(bias.res)
                kw["bias"] = bias.ap
            else:
                kw["bias"] = float(bias)
        if scale is not None:
            if isinstance(scale, V):
                rd.append(scale.res)
                kw["scale"] = scale.ap
            else:
                kw["scale"] = float(scale)
        self.op("act", "activation", rd, [out.res], out.ap, in_.ap, func, **kw)

    def tt(self, out, in0, in1, op, x="dve"):
        self.op(x, "tensor_tensor", [in0.res, in1.res], [out.res], out.ap, in0.ap, in1.ap, op)

    def ts(self, out, in0, s1, op0, s2=None, op1=None, x="dve"):
        rd = [in0.res]
        a1 = s1
        if isinstance(s1, V):
            rd.append(s1.res)
            a1 = s1.ap
        a2 = s2
        if isinstance(s2, V):
            rd.append(s2.res)
            a2 = s2.ap
        if op1 is None:
            self.op(x, "tensor_scalar", rd, [out.res], out.ap, in0.ap, a1, None, op0)
        else:
            self.op(x, "tensor_scalar", rd, [out.res], out.ap, in0.ap, a1, a2, op0, op1)

    def stt(self, out, in0, scalar, in1, op0, op1):
        rd = [in0.res, in1.res]
        a = scalar
        if isinstance(scalar, V):
            rd.append(scalar.res)
            a = scalar.ap
        self.op("dve", "scalar_tensor_tensor", rd, [out.res], out.ap, in0.ap, a, in1.ap, op0, op1)

    def cp(self, out, in_, x="dve"):
        if x == "act":
            self.op("act", "copy", [in_.res], [out.res], out.ap, in_.ap)
        else:
            self.op(x, "tensor_copy", [in_.res], [out.res], out.ap, in_.ap)

    def memset(self, out, val, x="dve"):
        self.op(x, "memset", [], [out.res], out.ap, val)

    def scan(self, out, d0, d1, init, op0=ALU.mult, op1=ALU.add):
        rd = [d0.res, d1.res]
        a = init
        if isinstance(init, V):
            rd.append(init.res)
            a = init.ap
        self.op("dve", "tensor_tensor_scan", rd, [out.res], out.ap, d0.ap, d1.ap, a, op0, op1)

    def recip(self, out, in_):
        self.op("dve", "reciprocal", [in_.res], [out.res], out.ap, in_.ap)

    def reduce(self, out, in_, op, axis=AX.X):
        self.op("dve", "tensor_reduce", [in_.res], [out.res], out.ap, in_.ap, axis, op)

    def iota(self, out, pattern, base=0, cm=0):
        self.op("pool", "iota", [], [out.res], out.ap, pattern, base=base, channel_multiplier=cm,
                allow_small_or_imprecise_dtypes=True)

    def finish(self):
        self.barrier()
        self.es.close()


def w_in_chunks():
    ch = []
    c = 0
    while c < IN_TOTAL:
        if c == OFF_GLA + 1536:
            ch.append((c, 32))
            c += 32
        else:
            ch.append((c, 128))
            c += 128
    assert c == IN_TOTAL
    return ch


def tok_tiles():
    tl = []
    for b in range(NB):
        tl.append((b, 0, TC, True))
        for i in range(TL // 512):
            tl.append((b, TC + i * 512, 512, False))
    return tl


class Prog:
    def __init__(self, nc, dbg=(), upto="all", p_input=False, only=None):
        self.p_input = p_input
        self.only = only
        self.nc = nc
        self.k = KB(nc)
        self.dbg = set(dbg)
        self.upto = upto
        self.IN = {}
        self.S = {}

    def inp(self, name, shape, dtype=F32):
        self.IN[name] = self.k.dram(name, shape, dtype, kind="ExternalInput")
        return self.IN[name]

    def scratch(self, name, shape, dtype=F32):
        kind = "ExternalOutput" if name in self.dbg else "Internal"
        self.S[name] = self.k.dram(name, shape, dtype, kind=kind)
        return self.S[name]

    def declare(self):
        inp = self.inp
        inp("h0", [NB, D, T])
        inp("cT", [128, 8, 3])
        inp("ident", [128, 128])
        inp("cmask", [4, 128, 128])
        inp("rmask", [128, 256])
        inp("ada_w", [DEPTH, D, 6 * D])
        inp("ada_bT", [DEPTH, 128, 48])
        inp("gmixT", [DEPTH, 128, 8])
        inp("gffnT", [DEPTH, 128, 8])
        inp("w_in", [DEPTH, D, IN_TOTAL])
        sc = self.scratch
        if self.only is not None and any(o in ("ffn0", "moe", "final") for o in self.only):
            self.S["hbuf"] = self.k.dram("hbuf", [NB, D, T], F32, kind="ExternalOutput")
            inp("hbuf_in", [NB, D, T])
        else:
            sc("hbuf", [NB, D, T])
        if self.p_input:
            inp("P", [NB, IN_TOTAL, T])
            self.S["P"] = self.IN["P"]
        else:
            sc("P", [NB, IN_TOTAL, T])
        sc("YBR", [4, NB, MIXW, T])
        rwkv_declare(self)
        gla_declare(self)
        s5_declare(self)
        hyena_declare(self)
        ffn_declare(self)
        sc("modout", [DEPTH, 128, 48 * 3])
        sc("xnout", [128, 8 * NB * T], BF16)

    def consts(self):
        k = self.k
        self.ident = k.sb("ident", [128, 128])
        k.dma("sp", self.ident, self.IN["ident"])
        self.ones = k.sb("ones", [128, 128])
        k.memset(self.ones, 1.0)
        self.cmask = k.sb("cmask", [128, 4, 128])
        k.dma("sp", self.cmask, self.IN["cmask"].rr("i p t -> p i t"))
        self.m_up, self.m_lo, self.m_upi, self.ones_bd = (self.cmask[:, i, :] for i in range(4))
        self.rmask = k.sb("rmask", [128, SEG])
        k.dma("sp", self.rmask, self.IN["rmask"])
        self.mod = [k.sb("mod%d" % l, [128, 48, 3]) for l in range(DEPTH)]
        self.A1 = [k.sb("A1_%d" % l, [128, 8, 3]) for l in range(DEPTH)]
        self.A2 = [k.sb("A2_%d" % l, [128, 8, 3]) for l in range(DEPTH)]

    def stage_mods(self):
        k = self.k
        k.push()
        sT = k.sb("sT", [128, 8, 3])
        k.dma("sp", sT, self.IN["cT"])
        k.act(sT, sT, AF.Silu)
        wb = [k.sb("adaw%d" % i, [128, 8, 512]) for i in range(2)]
        pm = [k.ps("pmod%d" % i, [128, 4, 3]) for i in range(2)]
        adab = k.sb("adab", [128, 48])
        gm = k.sb("gm", [128, 8])
        gf = k.sb("gf", [128, 8])
        it = 0
        for l in range(DEPTH):
            k.dma("sp", adab, self.IN["ada_bT"][l])
            k.dma("sp", gm, self.IN["gmixT"][l])
            k.dma("sp", gf, self.IN["gffnT"][l])
            for og in range(12):
                w = wb[it % 2]
                p = pm[it % 2]
                it += 1
                k.dma("sp" if og % 2 == 0 else "pool", w,
                      self.IN["ada_w"][l, :, og * 512:(og + 1) * 512].rr("(kc p) n -> p kc n", p=128))
                for j in range(4):
                    for kc in range(8):
                        k.mm(p[:, j, :], w[:, kc, j * 128:(j + 1) * 128], sT[:, kc, :], start=(kc == 0), stop=(kc == 7))
                for j in range(4):
                    oc = og * 4 + j
                    k.ts(self.mod[l][:, oc, :], p[:, j, :], adab[:, oc:oc + 1], ALU.add)
            k.stt(self.A1[l], self.mod[l][:, 8:16, :], 1.0, gm[:, :, None].bc([128, 8, 3]), ALU.add, ALU.mult)
            k.stt(self.A2[l], self.mod[l][:, 32:40, :], 1.0, gf[:, :, None].bc([128, 8, 3]), ALU.add, ALU.mult)
            if "modout" in self.dbg:
                k.dma("sp", self.S["modout"][l], self.mod[l].rr("p a b -> p (a b)"))
        k.pop()

    def norm_tile(self, l, which, ht, n, b, is_ctx, xn_out, sq, pss, rs, tmp):
        k = self.k
        j = 2 if is_ctx else b
        A = (self.A1 if which == 1 else self.A2)[l]
        sh0 = 0 if which == 1 else 24
        k.act(sq[:, :, :n], ht[:, :, :n], AF.Square)
        for kc in range(8):
            k.mm(pss[:, :n], self.ones, sq[:, kc, :n], start=(kc == 0), stop=(kc == 7))
        k.ts(rs[:, :n], pss[:, :n], 1.0 / D, ALU.mult, RMS_EPS, ALU.add)
        k.act(rs[:, :n], rs[:, :n], AF.Sqrt)
        k.recip(rs[:, :n], rs[:, :n])
        for kc in range(8):
            k.stt(tmp[:, kc, :n], ht[:, kc, :n], A[:, kc, j:j + 1], rs[:, :n], ALU.mult, ALU.mult)
            k.act(xn_out[:, kc, :n], tmp[:, kc, :n], AF.Identity, bias=self.mod[l][:, sh0 + kc, j:j + 1], scale=1.0)

    def stage_norm1(self, l, src):
        k = self.k
        hts = [k.sb("ht%d" % i, [128, 8, 512]) for i in range(2)]
        sq = k.sb("sq", [128, 8, 512])
        tmp = k.sb("ntmp", [128, 8, 512])
        rs = k.sb("rs", [128, 512])
        pss = k.ps("pss", [128, 512])
        for i, (b, t0, n, is_ctx) in enumerate(tok_tiles()):
            ht = hts[i % 2]
            k.dma("sp", ht[:, :, :n], src[b, :, t0:t0 + n].rr("(kc p) t -> p kc t", p=128))
            self.norm_tile(l, 1, ht, n, b, is_ctx, self.xn[:, :, b * T + t0: b * T + t0 + n], sq, pss, rs, tmp)

    def stage_win(self, l):
        k = self.k
        chunks = w_in_chunks()
        groups = []
        cur = []
        for c in chunks:
            if cur and (c[0] + c[1] - cur[0][0] > 512):
                groups.append(cur)
                cur = []
            cur.append(c)
        groups.append(cur)
        wst = [k.sb("wst%d" % i, [128, 8, 512]) for i in range(2)]
        wbf = [k.sb("wbf%d" % i, [128, 8, 512], BF16) for i in range(2)]
        pp = [k.ps("pwin%d" % i, [128, 512]) for i in range(4)]
        ost = [k.sb("ost%d" % i, [128, 512]) for i in range(4)]
        tiles = tok_tiles()
        it = 0
        P = self.S["P"]
        for gi, g in enumerate(groups):
            g0 = g[0][0]
            g1 = g[-1][0] + g[-1][1]
            ws = wst[gi % 2]
            wb = wbf[gi % 2]
            k.dma("sp", ws[:, :, :g1 - g0],
                  self.IN["w_in"][l, :, g0:g1].rr("(kc p) n -> p kc n", p=128))
            if gi % 2 == 0:
                k.cp(wb[:, :, :g1 - g0], ws[:, :, :g1 - g0], x="dve")
            else:
                k.cp(wb[:, :, :g1 - g0], ws[:, :, :g1 - g0], x="act")
            for (c0, m) in g:
                is_gate = c0 >= OFF_GATE
                for (b, t0, n, is_ctx) in tiles:
                    p = pp[it % 4]
                    o = ost[it % 4]
                    for kc in range(8):
                        k.mm(p[:m, :n], wb[:, kc, c0 - g0:c0 - g0 + m], self.xn[:, kc, b * T + t0:b * T + t0 + n],
                             start=(kc == 0), stop=(kc == 7))
                    if is_gate:
                        k.act(o[:m, :n], p[:m, :n], AF.Sigmoid)
                    elif it % 2 == 0:
                        k.cp(o[:m, :n], p[:m, :n], x="dve")
                    else:
                        k.cp(o[:m, :n], p[:m, :n], x="act")
                    k.dma("sp" if it % 4 != 3 else "pool", P[b, c0:c0 + m, t0:t0 + n], o[:m, :n])
                    it += 1

    def build(self):
        k = self.k
        self.declare()
        self.consts()
        if self.only is not None:
            for l in range(1):
                if "rwkv" in self.only or "rwkv_prep" in self.only:
                    rwkv_prep(self, l)
                if "rwkv" in self.only or "rwkv_scan" in self.only:
                    rwkv_scan(self, l)
                if "rwkv" in self.only or "rwkv_read" in self.only:
                    rwkv_readout(self, l, self.S["YBR"][1])
                if "rws5" in self.only:
                    rwkv_prep(self, l)
                    rwkv_s5_scan(self, l)
                    rwkv_readout(self, l, self.S["YBR"][1])
                    s5_out(self, l, self.S["YBR"][2])
                if "hyena" in self.only:
                    hyena_stage(self, l, TL, TC, self.S["YBR"][0])
                    hyena_stage(self, l, TC, 0, self.S["YBR"][0])
                if "s5" in self.only:
                    s5_scan(self, l)
                    s5_out(self, l, self.S["YBR"][2])
                if "gla" in self.only:
                    gla_prep(self, l)
                    gla_scan(self, l)
                    gla_readout(self, l, self.S["YBR"][3])
            if any(o in ("ffn0", "moe", "final") for o in self.only):
                self.stage_mods()
                k.dma("sp", self.S["hbuf"], self.IN["hbuf_in"])
                alltiles = tok_tiles()
                lat = [t for t in alltiles if not t[3]]
                k.push()
                if "ffn0" in self.only:
                    xn2 = k.sb("xn2", [128, 8, NB * T], BF16)
                    norm2_stage(self, 0, alltiles, xn2)
                    swiglu_stage(self, 0, alltiles, xn2, self.IN["ffn_w1"][0], self.IN["ffn_w3"][0],
                                 self.IN["ffn_w2"][0], D_FF, 1280)
                if "moe" in self.only:
                    l = 1
                    gate_tm = k.sb("gate_tm", [128, NB * TL // 128, NEXP])
                    rw_ = k.sb("router_w", [128, 8, NEXP])
                    rb_ = k.sb("router_b", [1, NEXP])
                    k.dma("sp", rw_, self.IN["moe_router_w"][0].rr("(kc p) e -> p kc e", p=128))
                    k.dma("sp", rb_, self.IN["moe_router_b"][0])
                    norm2_stage(self, l, lat, None, router=(gate_tm, rw_, rb_), xn2_dram=self.S["XN2"])
                    swiglu_stage(self, l, lat, None, self.IN["moe_w1"][0], self.IN["moe_w3"][0],
                                 self.IN["moe_w2"][0], D_FFE, 1024, experts=NEXP, gate_tm=gate_tm, xn2_dram=self.S["XN2"])
                k.pop()
                if "final" in self.only:
                    final_stage(self)
            return self.end()
        self.stage_mods()
        if self.upto == "mods":
            return self.end()
        S = self.S
        alltiles = tok_tiles()
        lat = [t for t in alltiles if not t[3]]
        for l in range(DEPTH):
            last = (l == DEPTH - 1)
            hsrc = self.IN["h0"] if l == 0 else S["hbuf"]
            if not self.p_input:
                k.push()
                self.xn = k.sb("xn", [128, 8, NB * T], BF16)
                self.stage_norm1(l, hsrc)
                self.stage_win(l)
                k.pop()
            if self.upto == "win":
                return self.end()
            hyena_stage(self, l, TL, TC, S["YBR"][0])
            if not last:
                hyena_stage(self, l, TC, 0, S["YBR"][0])
            rwkv_prep(self, l)
            rwkv_scan(self, l)
            rwkv_readout(self, l, S["YBR"][1])
            s5_scan(self, l)
            s5_out(self, l, S["YBR"][2])
            gla_prep(self, l)
            gla_scan(self, l)
            gla_readout(self, l, S["YBR"][3])
            if self.upto == "mixers":
                return self.end()
            merge_stage(self, l, hsrc, lat if last else alltiles)
            if self.upto == "merge":
                return self.end()
            k.push()
            if l % 2 == 0:
                xn2 = k.sb("xn2", [128, 8, NB * T], BF16)
                norm2_stage(self, l, alltiles, xn2)
                swiglu_stage(self, l, alltiles, xn2, self.IN["ffn_w1"][l // 2], self.IN["ffn_w3"][l // 2],
                             self.IN["ffn_w2"][l // 2], D_FF, 1280)
            else:
                gate_tm = k.sb("gate_tm", [128, NB * TL // 128, NEXP])
                rw_ = k.sb("router_w", [128, 8, NEXP])
                rb_ = k.sb("router_b", [1, NEXP])
                k.dma("sp", rw_, self.IN["moe_router_w"][l // 2].rr("(kc p) e -> p kc e", p=128))
                k.dma("sp", rb_, self.IN["moe_router_b"][l // 2])
                norm2_stage(self, l, lat, None, router=(gate_tm, rw_, rb_), xn2_dram=S["XN2"])
                swiglu_stage(self, l, lat, None, self.IN["moe_w1"][l // 2], self.IN["moe_w3"][l // 2],
                             self.IN["moe_w2"][l // 2], D_FFE, 1024, experts=NEXP, gate_tm=gate_tm, xn2_dram=S["XN2"])
            k.pop()
            if self.upto == "layer%d" % l:
                return self.end()
        final_stage(self)
        return self.end()

    def end(self):
        self.k.finish()
        return self.nc


def _fm(v, nch):
    return np.ascontiguousarray(np.asarray(v, np.float32).reshape(nch, 128).T)


def shared_inputs(inputs):
    sh = {}
    sh["ident"] = np.eye(128, dtype=np.float32)
    r = np.arange(128)
    same = (r[:, None] // 64) == (r[None, :] // 64)
    sh["cmask"] = np.stack([same & (r[:, None] < r[None, :]), same & (r[:, None] > r[None, :]),
                            same & (r[:, None] <= r[None, :]), same]).astype(np.float32)
    sh["rmask"] = np.ascontiguousarray(np.broadcast_to((np.arange(256) % 64 != 0).astype(np.float32), (128, 256)))
    rw = lambda n: np.asarray(inputs[n], np.float32)
    sh["rw_muT"] = np.stack([[_fm(rw("rw_mu")[l, i], 15) for i in range(2)] for l in range(DEPTH)])
    sh["rw_w0T"] = np.stack([[_fm(rw("rw_w0")[l, i], 4) for i in range(2)] for l in range(DEPTH)])
    sh["rw_a0T"] = np.stack([[_fm(rw("rw_a0")[l, i], 4) for i in range(2)] for l in range(DEPTH)])
    sh["rw_w_up"] = np.ascontiguousarray(rw("rw_w_up").reshape(DEPTH, 128, 512))
    sh["rw_a_up"] = np.ascontiguousarray(rw("rw_a_up").reshape(DEPTH, 128, 512))
    sh["rw_g_up"] = np.ascontiguousarray(rw("rw_g_up"))
    sh["gla_gate_up"] = np.ascontiguousarray(rw("gla_gate_up"))
    sh["gla_gate_bT"] = np.stack([[_fm(rw("gla_gate_b")[l, d], 2) for d in range(2)] for l in range(DEPTH)])
    sh["gla_norm_gT"] = np.ascontiguousarray(rw("gla_norm_g").reshape(DEPTH, 128, 1))
    for L in (TL, TC):
        n = 2 * L
        pos = np.arange(L, dtype=np.float64)
        t = pos / max(L - 1, 1)
        freqs = np.linspace(1e-4, 16 - 1, 16)
        ang = (2.0 * math.pi / L) * pos[:, None] * freqs[None, :]
        z = np.concatenate([t[:, None], np.cos(ang), -np.sin(ang)], axis=-1)
        sh["hy_zT%d" % L] = np.ascontiguousarray(z.T.astype(np.float32))
        rates = np.abs(np.linspace(math.log(1e-2) / 1.5, math.log(1e-2) / 0.3, MIXW))
        sh["hy_win%d" % L] = np.exp(-t[:, None] * rates[None, :]).astype(np.float32)
        om = 2.0 * math.pi * (np.arange(L, dtype=np.float64) + 0.5) / n
        ph = pos[:, None] * om[None, :]
        sh["dft_c%d" % L] = np.cos(ph).astype(np.float32)
        sh["dft_s%d" % L] = np.sin(ph).astype(np.float32)
        sh["idft_c%d" % L] = np.ascontiguousarray((np.cos(ph).T / L).astype(np.float32))
        sh["idft_s%d" % L] = np.ascontiguousarray((-np.sin(ph).T / L).astype(np.float32))
    sh["hy_cwT"] = np.stack([[_fm(rw("hy_conv_w")[l, i], 12) for i in range(3)] for l in range(DEPTH)])
    sh["hy_cbT"] = np.stack([_fm(rw("hy_conv_b")[l], 12) for l in range(DEPTH)])
    sh["hy_biasT"] = np.stack([_fm(rw("hy_bias")[l], 4) for l in range(DEPTH)])
    sh["hy_f_w1"] = rw("hy_f_w1"); sh["hy_f_w2"] = rw("hy_f_w2"); sh["hy_f_w3"] = rw("hy_f_w3")
    sh["hy_f_b1T"] = np.ascontiguousarray(rw("hy_f_b1")[:, :, None]); sh["hy_f_b2T"] = np.ascontiguousarray(rw("hy_f_b2")[:, :, None])
    sh["hy_f_b3"] = np.ascontiguousarray(rw("hy_f_b3")[:, None, :])
    for n in ("w_branch", "w_out", "ffn_w1", "ffn_w3", "ffn_w2", "moe_router_w", "moe_w1", "moe_w3", "moe_w2"):
        sh[n] = np.ascontiguousarray(rw(n))
    sh["moe_router_b"] = np.ascontiguousarray(rw("moe_router_b")[:, None, :])
    sh["fin_gT"] = _fm(rw("final_norm_g"), 8)
    sh["tau"] = np.ascontiguousarray(np.broadcast_to(np.arange(513, dtype=np.float32), (128, 513)))
    def pairT(a):
        a = np.asarray(a, np.float32)
        rest = a.shape[2:]
        return np.ascontiguousarray(np.moveaxis(a.reshape((16, 128) + rest), 0, 1))
    sh["s5_aT"] = np.stack([[[pairT(rw(n)[l, d]) for n in ("s5_a_re", "s5_a_im")] for d in range(2)] for l in range(DEPTH)])
    sh["s5_ldtT"] = np.stack([[pairT(np.repeat(rw("s5_log_dt")[l, d][:, None], 64, axis=1)) for d in range(2)] for l in range(DEPTH)])
    sh["s5_bT"] = np.stack([[pairT(rw(n)[l]) for n in ("s5_b_re", "s5_b_im")] for l in range(DEPTH)])
    cT = np.zeros((DEPTH, 2, 128, 16, 32), np.float32)
    for l in range(DEPTH):
        for x, n in enumerate(("s5_c_re", "s5_c_im")):
            c = rw(n)[l]
            for g in range(32):
                j, gl = g // 2, g % 2
                cT[l, x, gl * 64:(gl + 1) * 64, j, gl * 16:(gl + 1) * 16] = c[g].T
    sh["s5_cT"] = cT
    sh["s5_dT"] = np.stack([_fm(rw("s5_d")[l], 4) for l in range(DEPTH)])
    sh["s5_glu_w"] = np.ascontiguousarray(rw("s5_glu_w"))
    sh["s5_glu_bT"] = np.stack([_fm(rw("s5_glu_b")[l], 4) for l in range(DEPTH)])
    sh["rw_vecT"] = np.stack([[_fm(rw(n)[l].reshape(-1), 4) for n in ("rw_k_k", "rw_k_a", "rw_r_k", "rw_ln_g", "rw_ln_b")]
                              for l in range(DEPTH)])
    sh["ada_w"] = np.ascontiguousarray(inputs["ada_w"], dtype=np.float32)
    sh["ada_bT"] = np.stack([_fm(inputs["ada_b"][l], 48) for l in range(DEPTH)])
    sh["gmixT"] = np.stack([_fm(inputs["norm_mix_g"][l], 8) for l in range(DEPTH)])
    sh["gffnT"] = np.stack([_fm(inputs["norm_ffn_g"][l], 8) for l in range(DEPTH)])
    sh["w_in"] = np.ascontiguousarray(inputs["w_in"], dtype=np.float32)
    return sh


def core_inputs(inputs, core):
    b0 = core * NB
    x = np.asarray(inputs["x"][b0:b0 + NB], np.float32)
    ctx = np.asarray(inputs["ctx"][b0:b0 + NB], np.float32)
    h0 = np.concatenate([ctx.transpose(0, 2, 1), x.transpose(0, 2, 1)], axis=2)
    cm = {"h0": np.ascontiguousarray(h0)}
    cols = np.stack([inputs["c"][b0], inputs["c"][b0 + 1], inputs["c_ctx"]], axis=1).astype(np.float32)
    cm["cT"] = np.ascontiguousarray(cols.reshape(8, 128, 3).transpose(1, 0, 2))
    return cm


CH = 64
SEGC = 4
SEG = CH * SEGC


class CLA:
    MMDT = BF16

    def __init__(self, prog, delta, nvb, nbanks=8, nbuf=2):
        self.nbuf = nbuf
        dt = self.dt = CLA.MMDT
        self.pr = prog
        k = self.k = prog.k
        self.delta = delta
        self.nvb = nvb
        self.nop = 5 if delta else 3
        nv = nvb * 128
        self.nv = nv
        self.opin = [k.sb("cla_in%d" % i, [128, self.nop, SEG]) for i in range(nbuf)]
        self.vin = [k.sb("cla_vin%d" % i, [128, nvb, SEG]) for i in range(nbuf)]
        self.cum = k.sb("cla_cum", [128, SEG])
        self.tmp = k.sb("cla_tmp", [128, SEG])
        self.E = {n: k.sb("cla_E" + n, [128, SEG]) for n in (["p", "m", "l", "v"] if delta else ["p", "m", "l"])}
        self.WC = k.sb("cla_WC", [128, SEGC])
        self.mkt = [k.sb("cla_mkt%d" % i, [128, SEGC, CH]) for i in range(2)]
        self.mki = 0
        names = ["Rq", "Kd", "Kc"] + (["Kq", "Bd", "Bc"] if delta else [])
        self.bd = {n: k.sb("cla_bd" + n, [128, SEGC, 2, CH], dt) for n in names}
        for n in names:
            k.memset(self.bd[n], 0.0, x="pool")
        self.vpad = [k.sb("cla_vpad%d" % v, [128, SEGC, 2, CH]) for v in range(nvb)]
        for v in range(nvb):
            k.memset(self.vpad[v], 0.0, x="pool")
        nslot = 14 if delta else 5
        self.SL = [k.sb("cla_s%d" % s, [128, SEGC, 128], dt) for s in range(nslot)]
        self.slots = [[self.SL[s][:, c, :] for s in range(nslot)] for c in range(SEGC)]
        self.VBT = k.sb("cla_VB", [128, SEGC, nv], dt)
        self.OtT = k.sb("cla_Ot", [128, SEGC, nv], dt)
        self.HT = k.sb("cla_H", [128, SEGC, nv], dt)
        self.VB = [self.VBT[:, c, :] for c in range(SEGC)]
        self.Ot = [self.OtT[:, c, :] for c in range(SEGC)]
        self.H = [self.HT[:, c, :] for c in range(SEGC)]
        self.Tst = [k.sb("cla_T%d" % i, [128, nv]) for i in range(2)]
        self.Tsh = self.Tst if dt == F32 else [k.sb("cla_Tb%d" % i, [128, nv], dt) for i in range(2)]
        if dt == F32:
            self.Ib = prog.ident
        else:
            self.Ib = k.sb("cla_Ib", [128, 128], dt)
            k.cp(self.Ib, prog.ident, x="act")
        self.oseg = [k.sb("cla_oseg%d" % i, [128, nvb, SEG]) for i in range(2)]
        self.pst = [k.ps("cla_ps%d" % i, [128, 512]) for i in range(nbanks)]
        self.psn = nbanks
        self.pi = 0
        self.ei = 0

    def P(self):
        p = self.pst[self.pi % self.psn]
        self.pi += 1
        return p

    def evac(self, out, in_, neg=False):
        k = self.k
        if neg:
            k.act(out, in_, AF.Copy, scale=-1.0)
        elif self.ei % 3 == 0:
            k.cp(out, in_, x="dve")
        else:
            k.cp(out, in_, x="act")

    def run_gen(self, ops_dram, v_dram, o_dram):
        k = self.k
        pr = self.pr
        delta = self.delta
        nvb = self.nvb
        nv = self.nv
        I = pr.ident
        Ib = self.Ib
        f32 = (self.dt == F32)

        def trn(p, x):
            if f32:
                k.tr(p, x, I)
            else:
                k.mm(p, x, Ib)
        k.memset(self.Tst[0], 0.0)
        if not f32:
            k.memset(self.Tsh[0], 0.0)
        tcur = 0
        nseg = T // SEG
        for sg in range(nseg):
            s0 = sg * SEG
            oin = self.opin[sg % self.nbuf]
            vin = self.vin[sg % self.nbuf]
            k.dma("sp", oin, ops_dram[:, :, s0:s0 + SEG].rr("i p t -> p i t"))
            k.dma("pool", vin, v_dram[:, :, s0:s0 + SEG].rr("i p t -> p i t"))
            Rr, Kr, LW = oin[:, 0, :], oin[:, 1, :], oin[:, 2, :]
            k.scan(self.cum, pr.rmask, LW, 0.0)
            cum3 = self.cum.rr("p (c t) -> p c t", t=CH)
            cl = cum3[:, :, CH - 1:CH]
            k.act(self.E["p"], self.cum, AF.Exp)
            k.act(self.E["m"], self.cum, AF.Exp, scale=-1.0)
            k.tt(self.tmp.rr("p (c t) -> p c t", t=CH), cl.bc([128, SEGC, CH]), cum3, ALU.subtract, x="pool")
            k.act(self.E["l"], self.tmp, AF.Exp)
            k.act(self.WC.rr("p (c o) -> p c o", o=1), cl, AF.Exp)
            if delta:
                k.tt(self.tmp, self.cum, LW, ALU.subtract, x="pool")
                k.act(self.E["v"], self.tmp, AF.Exp)
            def mk(name, src, e):
                e3 = self.E[e].rr("p (c t) -> p c t", t=CH)
                s3 = src.rr("p (c t) -> p c t", t=CH)
                t3 = self.mkt[self.mki % 2]
                self.mki += 1
                k.tt(t3, s3, e3, ALU.mult, x="dve")
                k.cp(self.bd[name][0:64, :, 0, :], t3[0:64], x="act")
                k.cp(self.bd[name][64:128, :, 1, :], t3[64:128], x="act")
            mk("Rq", Rr, "p")
            mk("Kd", Kr, "m")
            mk("Kc", Kr, "l")
            if delta:
                KKr, Br = oin[:, 3, :], oin[:, 4, :]
                mk("Kq", KKr, "v")
                mk("Bd", Br, "m")
                mk("Bc", Br, "l")
            for v in range(nvb):
                v3 = vin[:, v, :].rr("p (c t) -> p c t", t=CH)
                if nvb == 1:
                    k.cp(self.vpad[0][0:64, :, 0, :], v3[0:64], x="act")
                    k.cp(self.vpad[0][64:128, :, 1, :], v3[64:128], x="act")
                else:
                    k.cp(self.vpad[v][:, :, v, :], v3, x="act")
            B = {n: [self.bd[n][:, c, :, :].rr("p h t -> p (h t)") for c in range(SEGC)] for n in self.bd}
            S = self.slots
            SL = self.SL
            CC = range(SEGC)

            def step(mmf, evall, w=128):
                rpb = 512 // w
                self.ei += 1
                regs = []
                banks = []
                for c in CC:
                    if c % rpb == 0:
                        bank = self.P()
                        banks.append((bank, c))
                    regs.append(bank[:, (c % rpb) * w:(c % rpb + 1) * w])
                for c in CC:
                    mmf(c, regs[c])
                yield
                for bank, c0 in banks:
                    ncb = min(rpb, SEGC - c0)
                    pv = bank[:, 0:ncb * w].rr("p (c x) -> p c x", x=w)
                    evall(pv, slice(c0, c0 + ncb), ncb)

            def m3(m, n_):
                return m[:, None, :].bc([128, n_, 128])
            bd3 = {n_: self.bd[n_].rr("p c h t -> p c (h t)") for n_ in self.bd}
            for v in range(nvb):
                yield from step(lambda c, p: k.tr(p, self.vpad[v][:, c, :, :].rr("p h t -> p (h t)"), I),
                                lambda pv, cs, n_: self.evac(self.VBT[:, cs, v * 128:(v + 1) * 128], pv))
            if delta:
                yield from step(lambda c, p: k.mm(p, B["Bd"][c], B["Kq"][c]),
                                lambda pv, cs, n_: k.stt(SL[1][:, cs], pv, -1.0, m3(pr.m_up, n_), ALU.mult, ALU.mult))
                yield from step(lambda c, p: k.mm(p, B["Kq"][c], B["Bd"][c]),
                                lambda pv, cs, n_: k.stt(SL[0][:, cs], pv, -1.0, m3(pr.m_lo, n_), ALU.mult, ALU.mult))
                yield from step(lambda c, p: k.mm(p, B["Kd"][c], B["Kq"][c]),
                                lambda pv, cs, n_: k.tt(SL[6][:, cs], pv, m3(pr.m_up, n_), ALU.mult))
                yield from step(lambda c, p: k.mm(p, B["Kd"][c], B["Rq"][c]),
                                lambda pv, cs, n_: k.tt(SL[7][:, cs], pv, m3(pr.m_upi, n_), ALU.mult))
                yield from step(lambda c, p: k.mm(p, B["Bd"][c], B["Rq"][c]),
                                lambda pv, cs, n_: k.stt(SL[8][:, cs], pv, -1.0, m3(pr.m_upi, n_), ALU.mult, ALU.mult))
                k.tt(SL[4], SL[1], m3(I, SEGC), ALU.add, x="dve")
                pc, pt, yc = 0, 1, 4
                for j in range(1, 6):
                    pn, ptn, yn = 2 - pc, 4 - pt, 9 - yc
                    yield from step(lambda c, p: k.mm(p, S[c][pt], S[c][pc]),
                                    lambda pv, cs, n_: self.evac(SL[pn][:, cs], pv))
                    if j < 5:
                        yield from step(lambda c, p: k.mm(p, S[c][pc], S[c][pt]),
                                        lambda pv, cs, n_: self.evac(SL[ptn][:, cs], pv))
                    yield from step(lambda c, p: k.mm(p, S[c][pn], S[c][yc]),
                                    lambda pv, cs, n_: k.tt(SL[yn][:, cs], pv, SL[yc][:, cs], ALU.add))
                    pc, pt, yc = pn, ptn, yn
                XT = yc
                yield from step(lambda c, p: trn(p, B["Kq"][c]), lambda pv, cs, n_: self.evac(SL[9][:, cs], pv))
                yield from step(lambda c, p: trn(p, B["Kc"][c]), lambda pv, cs, n_: self.evac(SL[10][:, cs], pv))
                yield from step(lambda c, p: trn(p, B["Bc"][c]), lambda pv, cs, n_: self.evac(SL[11][:, cs], pv, neg=True))
                free = [s_ for s_ in (0, 1, 2, 3, 4, 5) if s_ != XT]
                sAV, sUt, sKqt, sRt, sGT = free[0], free[1], free[2], free[3], free[4]
                yield from step(lambda c, p: k.mm(p, S[c][6], self.VB[c]), lambda pv, cs, n_: self.evac(SL[sAV][:, cs], pv))
                yield from step(lambda c, p: k.mm(p, S[c][XT], S[c][sAV]), lambda pv, cs, n_: self.evac(SL[sUt][:, cs], pv))
                yield from step(lambda c, p: k.mm(p, S[c][XT], S[c][9]), lambda pv, cs, n_: self.evac(SL[sKqt][:, cs], pv))
                yield from step(lambda c, p: k.mm(p, S[c][sKqt], S[c][8]),
                                lambda pv, cs, n_: k.tt(SL[sRt][:, cs], pv, bd3["Rq"][:, cs], ALU.add))

                def mm_ot(c, p):
                    k.mm(p, S[c][7], self.VB[c], start=True, stop=False)
                    k.mm(p, S[c][8], S[c][sUt], start=False, stop=True)
                yield from step(mm_ot, lambda pv, cs, n_: self.evac(self.OtT[:, cs], pv))

                def mm_h(c, p):
                    k.mm(p, S[c][10], self.VB[c], start=True, stop=False)
                    k.mm(p, S[c][11], S[c][sUt], start=False, stop=True)
                yield from step(mm_h, lambda pv, cs, n_: self.evac(self.HT[:, cs], pv))
                yield from step(lambda c, p: k.mm(p, S[c][sKqt], S[c][11]),
                                lambda pv, cs, n_: self.evac(SL[sGT][:, cs], pv))
                RtT = [S[c][sRt] for c in CC]
                GT = [S[c][sGT] for c in CC]
            else:
                yield from step(lambda c, p: k.mm(p, B["Kd"][c], B["Rq"][c]),
                                lambda pv, cs, n_: k.tt(SL[0][:, cs], pv, m3(pr.m_upi, n_), ALU.mult))
                yield from step(lambda c, p: trn(p, B["Kc"][c]), lambda pv, cs, n_: self.evac(SL[1][:, cs], pv))
                yield from step(lambda c, p: k.mm(p, S[c][0], self.VB[c]), lambda pv, cs, n_: self.evac(self.OtT[:, cs], pv), w=nv)
                yield from step(lambda c, p: k.mm(p, S[c][1], self.VB[c]), lambda pv, cs, n_: self.evac(self.HT[:, cs], pv), w=nv)
                RtT = [B["Rq"][c] for c in CC]
            osg = self.oseg[sg % 2]
            for c in CC:
                Tc = self.Tst[tcur]
                Tn = self.Tst[1 - tcur]
                Tcb = self.Tsh[tcur]
                Tnb = self.Tsh[1 - tcur]
                for v in range(nvb):
                    p = self.P()[:, 0:128]
                    k.mm(p, Tcb[:, v * 128:(v + 1) * 128], RtT[c], start=True, stop=False)
                    k.mm(p, self.Ot[c][:, v * 128:(v + 1) * 128], Ib, start=False, stop=True)
                    yield
                    if nvb == 1:
                        k.cp(osg[0:64, 0, c * CH:(c + 1) * CH], p[0:64, 0:64], x="dve")
                        k.cp(osg[64:128, 0, c * CH:(c + 1) * CH], p[64:128, 64:128], x="dve")
                    else:
                        k.cp(osg[:, v, c * CH:(c + 1) * CH], p[:, v * CH:(v + 1) * CH], x="act")
                if delta:
                    p = self.P()[:, 0:nv]
                    k.mm(p, GT[c], Tcb, start=True, stop=False)
                    k.mm(p, Ib, self.H[c], start=False, stop=True)
                    yield
                    k.stt(Tn, Tc, self.WC[:, c:c + 1], p, ALU.mult, ALU.add)
                else:
                    k.stt(Tn, Tc, self.WC[:, c:c + 1], self.H[c], ALU.mult, ALU.add)
                if not f32:
                    k.cp(Tnb, Tn, x="act")
                tcur = 1 - tcur
            k.dma("sp", o_dram[:, :, s0:s0 + SEG].rr("i p t -> p i t"), osg)


    def run(self, ops_dram, v_dram, o_dram):
        for _ in self.run_gen(ops_dram, v_dram, o_dram):
            pass


def cla_run_many(clas, jobs):
    pending = list(jobs)
    active = [None] * len(clas)
    while pending or any(a is not None for a in active):
        for i, cla in enumerate(clas):
            if active[i] is None and pending:
                active[i] = cla.run_gen(*pending.pop(0))
            if active[i] is not None:
                try:
                    next(active[i])
                except StopIteration:
                    active[i] = None


RW_DSCALE = 0.606531
RW_GN_EPS = 64e-5


def nat_tiles():
    tl = [(0, TC, 0, TC)]
    for i in range(TL // 512):
        tl.append((TC + i * 512, 512, TC, T))
    return tl


def rwkv_declare(pr):
    pr.inp("rw_muT", [DEPTH, 2, 128, 15])
    pr.inp("rw_w0T", [DEPTH, 2, 128, 4])
    pr.inp("rw_a0T", [DEPTH, 2, 128, 4])
    pr.inp("rw_w_up", [DEPTH, 128, 512])
    pr.inp("rw_a_up", [DEPTH, 128, 512])
    pr.inp("rw_g_up", [DEPTH, 128, 512])
    pr.inp("rw_vecT", [DEPTH, 5, 128, 4])
    pr.scratch("RWop", [NB, 2, 4, 5, 128, T])
    pr.scratch("RWv", [NB, 2, 4, 1, 128, T])
    pr.scratch("RWo", [NB, 2, 4, 1, 128, T])
    pr.scratch("RWg", [NB, 4, 128, T])
    pr.scratch("RWcb", [NB, 4, 128, T])


def rwkv_prep(pr, l):
    k = pr.k
    k.push()
    IN = pr.IN
    mu = [k.sb("rw_mu%d" % i, [128, 15]) for i in range(2)]
    c0 = k.sb("rw_c0", [128, 15])
    for i in range(2):
        k.dma("sp", mu[i], IN["rw_muT"][l, i])
    k.tt(c0, mu[0], mu[1], ALU.add)
    k.ts(c0, c0, -1.0, ALU.mult, 1.0, ALU.add)
    w0 = k.sb("rw_w0", [128, 2, 4])
    a0 = k.sb("rw_a0", [128, 2, 4])
    k.dma("sp", w0, IN["rw_w0T"][l].rr("d p c -> p d c"))
    k.dma("sp", a0, IN["rw_a0T"][l].rr("d p c -> p d c"))
    wup = k.sb("rw_wup", [128, 512])
    aup = k.sb("rw_aup", [128, 512])
    gup = k.sb("rw_gup", [128, 512])
    k.dma("sp", wup, IN["rw_w_up"][l])
    k.dma("sp", aup, IN["rw_a_up"][l])
    k.dma("sp", gup, IN["rw_g_up"][l])
    vec = k.sb("rw_vec", [128, 5, 4])
    k.dma("sp", vec, IN["rw_vecT"][l].rr("i p c -> p i c"))
    omka = k.sb("rw_omka", [128, 4])
    k.ts(omka, vec[:, 1, :], -1.0, ALU.mult, 1.0, ALU.add)
    raw = [k.sb("rw_raw%d" % i, [128, 15, 514]) for i in range(1)]
    sh = k.sb("rw_sh", [128, 15, 512])
    t15 = k.sb("rw_t15", [128, 15, 512])
    TW = k.sb("rw_TW", [128, 512])
    SG = k.sb("rw_SG", [128, 512])
    kkr = k.sb("rw_kkr", [128, 512])
    sq = k.sb("rw_sq", [128, 512])
    nrm = k.sb("rw_nrm", [128, 512])
    kk = k.sb("rw_kk", [128, 512])
    NO = 3
    ost = [[k.sb("rw_o%d_%d" % (j, i), [128, 512]) for i in range(7)] for j in range(NO)]
    A = k.sb("rw_A", [128, 512])
    tA = k.sb("rw_tA", [128, 512])
    sgw = k.sb("rw_sgw", [128, 512])
    cbin = k.sb("rw_cbin", [128, 512])
    pss = [k.ps("rw_ps%d" % i, [128, 512]) for i in range(5)]
    pcb = k.ps("rw_pcb", [128, 512])
    P = pr.S["P"]
    it = 0
    oi = 0
    pi = 0
    for b in range(NB):
        for (t0, n, ra, rb) in nat_tiles():
            rw = raw[0]
            it += 1
            lo = max(ra, t0 - 1)
            hi = min(rb, t0 + n + 1)
            if lo == t0:
                k.memset(rw[:, :, 0:1], 0.0, x="pool")
            if hi == t0 + n:
                k.memset(rw[:, :, n + 1:n + 2], 0.0, x="pool")
            k.dma("sp", rw[:, :, lo - (t0 - 1):hi - (t0 - 1)],
                  P[b, OFF_RW:OFF_RW + 1920, lo:hi].rr("(j p) t -> p j t", p=128))
            k.tt(sh[:, :, :n], rw[:, :, 1:n + 1], c0[:, :, None].bc([128, 15, n]), ALU.mult, x="dve")
            for jj in range(15):
                k.act(t15[:, jj, :n], rw[:, jj, 0:n], AF.Identity, scale=mu[0][:, jj:jj + 1])
            k.tt(sh[:, :, :n], sh[:, :, :n], t15[:, :, :n], ALU.add, x="dve")
            for jj in range(15):
                k.act(t15[:, jj, :n], rw[:, jj, 2:n + 2], AF.Identity, scale=mu[1][:, jj:jj + 1])
            k.tt(sh[:, :, :n], sh[:, :, :n], t15[:, :, :n], ALU.add, x="dve")
            k.act(TW[:, :n], sh[:, 12, :n], AF.Tanh)
            k.act(SG[:, :n], sh[:, 14, :n], AF.Sigmoid)
            AD = sh[:, 13, :]
            m0 = ra + rb - t0 - n
            for p in range(4):
                rS, kS, vS = sh[:, p, :], sh[:, 4 + p, :], sh[:, 8 + p, :]
                cs = slice(p * 128, (p + 1) * 128)
                k.act(kkr[:, :n], kS[:, :n], AF.Identity, scale=vec[:, 0, p:p + 1])
                k.act(sq[:, :n], kkr[:, :n], AF.Square)
                ps = pss[pi % 5]
                pi += 1
                k.mm(ps[:, :n], pr.ones_bd, sq[:, :n])
                k.act(nrm[:, :n], ps[:, :n], AF.Sqrt)
                k.ts(nrm[:, :n], nrm[:, :n], 1e-12, ALU.max)
                k.recip(nrm[:, :n], nrm[:, :n])
                k.tt(kk[:, :n], kkr[:, :n], nrm[:, :n], ALU.mult)
                ps = pss[pi % 5]
                pi += 1
                k.mm(ps[:, :n], gup[:, cs], SG[:, :n])
                o = ost[oi % NO]
                oi += 1
                k.cp(o[6][:, :n], ps[:, :n], x="act")
                k.dma("pool", pr.S["RWg"][b, p, :, t0:t0 + n], o[6][:, :n])
                for d in range(2):
                    ds = slice(d * 64, (d + 1) * 64)
                    if d == 1:
                        o = ost[oi % NO]
                        oi += 1

                    def ov(tile):
                        return tile[:, :n] if d == 0 else tile[:, :n][:, ::-1]
                    ps = pss[pi % 5]
                    pi += 1
                    k.mm(ps[:, :n], wup[ds, cs], TW[ds, :n])
                    k.act(sgw[:, :n], ps[:, :n], AF.Sigmoid, bias=w0[:, d, p:p + 1], scale=1.0)
                    k.ts(ov(o[2]), sgw[:, :n], -RW_DSCALE, ALU.mult)
                    ps = pss[pi % 5]
                    pi += 1
                    k.mm(ps[:, :n], aup[ds, cs], AD[ds, :n])
                    k.act(A[:, :n], ps[:, :n], AF.Sigmoid, bias=a0[:, d, p:p + 1], scale=1.0)
                    k.ts(tA[:, :n], A[:, :n], vec[:, 1, p:p + 1], ALU.mult, omka[:, p:p + 1], ALU.add)
                    k.tt(ov(o[1]), kS[:, :n], tA[:, :n], ALU.mult)
                    k.tt(ov(o[4]), A[:, :n], kk[:, :n], ALU.mult, x="dve")
                    k.stt(cbin[:, :n], rS[:, :n], vec[:, 2, p:p + 1], o[1][:, :n] if d == 0 else o[1][:, :n][:, ::-1],
                          ALU.mult, ALU.mult)
                    k.mm(pcb[:, :n], pr.ones_bd, cbin[:, :n], start=(d == 0), stop=(d == 1))
                    k.cp(ov(o[0]), rS[:, :n], x="act")
                    k.cp(ov(o[3]), kk[:, :n], x="act")
                    k.cp(ov(o[5]), vS[:, :n], x="act")
                    s0 = t0 if d == 0 else m0
                    for i in range(5):
                        k.dma("sp" if i % 2 == 0 else "pool", pr.S["RWop"][b, d, p, i, :, s0:s0 + n], o[i][:, :n])
                    k.dma("sp", pr.S["RWv"][b, d, p, 0, :, s0:s0 + n], o[5][:, :n])
                k.cp(cbin[:, :n], pcb[:, :n], x="dve")
                k.dma("pool", pr.S["RWcb"][b, p, :, t0:t0 + n], cbin[:, :n])
    k.pop()


def rwkv_scan(pr, l):
    k = pr.k
    k.push()
    clas = [CLA(pr, True, 1, nbanks=4) for _ in range(2)]
    jobs = [(pr.S["RWop"][b, d, p], pr.S["RWv"][b, d, p], pr.S["RWo"][b, d, p])
            for b in range(NB) for d in range(2) for p in range(4)]
    cla_run_many(clas, jobs)
    k.pop()


def rwkv_s5_scan(pr, l):
    k = pr.k
    k.push()
    clas = [CLA(pr, True, 1, nbanks=3, nbuf=1)]
    jobs = [(pr.S["RWop"][b, d, p], pr.S["RWv"][b, d, p], pr.S["RWo"][b, d, p])
            for b in range(NB) for d in range(2) for p in range(4)]
    s5g = s5_scan_gen(pr, l, npy=1, nwork=1)
    pending = list(jobs)
    active = None
    s5_alive = True
    n = 0
    while pending or active is not None or s5_alive:
        if active is None and pending:
            active = clas[0].run_gen(*pending.pop(0))
        if active is not None:
            try:
                next(active)
            except StopIteration:
                active = None
        n += 1
        if s5_alive and (n % 3 == 0 or (active is None and not pending)):
            try:
                next(s5g)
            except StopIteration:
                s5_alive = False
    k.pop()


def rwkv_readout(pr, l, ybr):
    k = pr.k
    k.push()
    vec = k.sb("rwr_vec", [128, 5, 4])
    k.dma("sp", vec, pr.IN["rw_vecT"][l].rr("i p c -> p i c"))
    NBUF = 2
    o0 = [k.sb("rwr_o0_%d" % i, [128, 512]) for i in range(NBUF)]
    o1 = [k.sb("rwr_o1_%d" % i, [128, 512]) for i in range(NBUF)]
    vv = [k.sb("rwr_v_%d" % i, [128, 512]) for i in range(NBUF)]
    gg = [k.sb("rwr_g_%d" % i, [128, 512]) for i in range(NBUF)]
    cb = [k.sb("rwr_cb_%d" % i, [128, 512]) for i in range(NBUF)]
    yo = [k.sb("rwr_y_%d" % i, [128, 512]) for i in range(NBUF)]
    sq = k.sb("rwr_sq", [128, 512])
    mean = k.sb("rwr_mean", [128, 512])
    var = k.sb("rwr_var", [128, 512])
    t1 = k.sb("rwr_t1", [128, 512])
    ps1 = k.ps("rwr_ps1", [128, 512])
    ps2 = k.ps("rwr_ps2", [128, 512])
    it = 0
    for b in range(NB):
        for p in range(4):
            for (t0, n, ra, rb) in nat_tiles():
                i = it % NBUF
                it += 1
                m0 = ra + rb - t0 - n
                k.dma("sp", o0[i][:, :n], pr.S["RWo"][b, 0, p, 0, :, t0:t0 + n])
                k.dma("pool", o1[i][:, :n], pr.S["RWo"][b, 1, p, 0, :, m0:m0 + n])
                k.dma("sp", vv[i][:, :n], pr.S["RWv"][b, 0, p, 0, :, t0:t0 + n])
                k.dma("pool", gg[i][:, :n], pr.S["RWg"][b, p, :, t0:t0 + n])
                k.dma("sp", cb[i][:, :n], pr.S["RWcb"][b, p, :, t0:t0 + n])
                o = o0[i]
                k.tt(o[:, :n], o[:, :n], o1[i][:, :n][:, ::-1], ALU.add)
                k.mm(ps1[:, :n], pr.ones_bd, o[:, :n])
                k.act(sq[:, :n], o[:, :n], AF.Square)
                k.mm(ps2[:, :n], pr.ones_bd, sq[:, :n])
                k.ts(mean[:, :n], ps1[:, :n], 1.0 / 64, ALU.mult)
                k.tt(t1[:, :n], mean[:, :n], mean[:, :n], ALU.mult, x="pool")
                k.stt(var[:, :n], ps2[:, :n], 1.0 / 64, t1[:, :n], ALU.mult, ALU.subtract)
                k.ts(var[:, :n], var[:, :n], RW_GN_EPS, ALU.add)
                k.act(var[:, :n], var[:, :n], AF.Sqrt)
                k.recip(var[:, :n], var[:, :n])
                k.tt(t1[:, :n], o[:, :n], mean[:, :n], ALU.subtract, x="pool")
                k.stt(t1[:, :n], t1[:, :n], vec[:, 3, p:p + 1], var[:, :n], ALU.mult, ALU.mult)
                k.tt(sq[:, :n], cb[i][:, :n], vv[i][:, :n], ALU.mult, x="pool")
                k.stt(t1[:, :n], t1[:, :n], vec[:, 4, p:p + 1], sq[:, :n], ALU.add, ALU.add)
                k.tt(yo[i][:, :n], t1[:, :n], gg[i][:, :n], ALU.mult)
                k.dma("sp", ybr[b, p * 128:(p + 1) * 128, t0:t0 + n], yo[i][:, :n])
    k.pop()


def col_tiles():
    return [(0, 512), (512, 512), (1024, 512), (1536, 512), (2048, 256)]


def seq_copy(k, dst, src, colmajor, rev, x, scale=None):
    def emit(o, i):
        if scale is not None:
            k.act(o, i, AF.Copy, scale=scale)
        else:
            k.cp(o, i, x=x)
    c_src = src[:, 0:TC]
    emit(dst[:, 0:TC], c_src[:, ::-1] if rev else c_src)
    if colmajor:
        l_src = src[:, TC:T].rr("p (r c) -> p c r", c=64)
        if rev:
            l_src = l_src[:, ::-1, ::-1]
        emit(dst[:, TC:T].rr("p (c r) -> p c r", r=32), l_src)
    else:
        l_src = src[:, TC:T]
        emit(dst[:, TC:T], l_src[:, ::-1] if rev else l_src)


def gla_declare(pr):
    pr.inp("gla_gate_up", [DEPTH, 2, 16, 256])
    pr.inp("gla_gate_bT", [DEPTH, 2, 128, 2])
    pr.inp("gla_norm_gT", [DEPTH, 128, 1])
    pr.scratch("GLop", [NB, 2, 2, 3, 128, T])
    pr.scratch("GLv", [NB, 2, 2, 2, 128, T])
    pr.scratch("GLo", [NB, 2, 2, 2, 128, T])


def gla_prep(pr, l):
    k = pr.k
    k.push()
    IN = pr.IN
    P = pr.S["P"]
    gup = [k.sb("gl_gup%d" % d, [16, 256]) for d in range(2)]
    nb = k.sb("gl_nb", [128, 2, 2])
    for d in range(2):
        k.dma("sp", gup[d], IN["gla_gate_up"][l, d])
    k.dma("sp", nb, IN["gla_gate_bT"][l].rr("d p c -> p d c"))
    k.ts(nb, nb, -1.0, ALU.mult)
    gd = [k.sb("gl_gd%d" % d, [16, T]) for d in range(2)]
    nat = [k.sb("gl_nat%d" % i, [128, T]) for i in range(2)]
    sq_ = [k.sb("gl_seq%d" % i, [128, T]) for i in range(3)]
    lwn = k.sb("gl_lwn", [128, T])
    ps = [k.ps("gl_ps%d" % i, [128, 512]) for i in range(2)]
    ni = 0
    si = 0
    for b in range(NB):
        for d in range(2):
            k.dma("sp", gd[d], P[b, OFF_GLA + 1536 + d * 16:OFF_GLA + 1536 + (d + 1) * 16, :])
        for p in range(2):
            for i, (row0, scale) in enumerate(((p * 128, 0.125), (256 + p * 128, None))):
                nt = nat[ni % 2]
                ni += 1
                k.dma("sp", nt, P[b, OFF_GLA + row0:OFF_GLA + row0 + 128, :])
                for d in range(2):
                    st = sq_[si % 3]
                    si += 1
                    seq_copy(k, st, nt, True, d == 1, "act", scale=scale)
                    k.dma("pool", pr.S["GLop"][b, d, p, i], st)
            for d in range(2):
                for j, (c0, n) in enumerate(col_tiles()):
                    pp = ps[j % 2]
                    k.mm(pp[:, :n], gup[d][:, p * 128:(p + 1) * 128], gd[d][:, c0:c0 + n])
                    k.act(lwn[:, c0:c0 + n], pp[:, :n], AF.Exp, bias=nb[:, d, p:p + 1], scale=-1.0)
                k.act(lwn, lwn, AF.Ln, bias=1.0, scale=1.0)
                st = sq_[si % 3]
                si += 1
                seq_copy(k, st, lwn, True, d == 1, "pool", scale=-1.0 / 16.0)
                k.dma("pool", pr.S["GLop"][b, d, p, 2], st)
            for vb in range(2):
                h = 2 * p + vb
                nt = nat[ni % 2]
                ni += 1
                k.dma("sp", nt, P[b, OFF_GLA + 512 + h * 128:OFF_GLA + 512 + (h + 1) * 128, :])
                for d in range(2):
                    st = sq_[si % 3]
                    si += 1
                    seq_copy(k, st, nt, True, d == 1, "act" if d == 0 else "dve")
                    k.dma("pool", pr.S["GLv"][b, d, p, vb], st)
    k.pop()


def gla_scan(pr, l):
    k = pr.k
    k.push()
    clas = [CLA(pr, False, 2, nbanks=4) for _ in range(2)]
    jobs = [(pr.S["GLop"][b, d, p], pr.S["GLv"][b, d, p], pr.S["GLo"][b, d, p])
            for b in range(NB) for d in range(2) for p in range(2)]
    cla_run_many(clas, jobs)
    k.pop()


def gla_readout(pr, l, ybr):
    k = pr.k
    k.push()
    P = pr.S["P"]
    ng = k.sb("glr_ng", [128, 1])
    k.dma("sp", ng, pr.IN["gla_norm_gT"][l])
    o0 = k.sb("glr_o0", [128, T])
    o1 = k.sb("glr_o1", [128, T])
    gn = k.sb("glr_gn", [128, T])
    gs = k.sb("glr_gs", [128, T])
    sq = k.sb("glr_sq", [128, T])
    rs = k.sb("glr_rs", [128, T])
    yn = k.sb("glr_yn", [128, T])
    ps = [k.ps("glr_ps%d" % i, [128, 512]) for i in range(2)]
    for b in range(NB):
        for h in range(4):
            p, vb = h // 2, h % 2
            k.dma("sp", o0, pr.S["GLo"][b, 0, p, vb])
            k.dma("pool", o1, pr.S["GLo"][b, 1, p, vb])
            k.dma("sp", gn, P[b, OFF_GLA + 1024 + h * 128:OFF_GLA + 1024 + (h + 1) * 128, :])
            k.tt(o0[:, 0:TC], o0[:, 0:TC], o1[:, 0:TC][:, ::-1], ALU.add)
            k.tt(o0[:, TC:T], o0[:, TC:T], o1[:, TC:T][:, ::-1], ALU.add)
            k.act(sq, o0, AF.Square)
            for j, (c0, n) in enumerate(col_tiles()):
                pp = ps[j % 2]
                k.mm(pp[:, :n], pr.ones, sq[:, c0:c0 + n])
                k.ts(rs[:, c0:c0 + n], pp[:, :n], 1.0 / 128, ALU.mult, RMS_EPS, ALU.add)
            k.act(rs, rs, AF.Sqrt)
            k.recip(rs, rs)
            k.stt(o0, o0, ng[:, 0:1], rs, ALU.mult, ALU.mult)
            k.act(gn, gn, AF.Silu)
            seq_copy(k, gs, gn, True, False, "pool")
            k.tt(o0, o0, gs, ALU.mult)
            k.cp(yn[:, 0:TC], o0[:, 0:TC], x="act")
            k.cp(yn[:, TC:T].rr("p (r c) -> p c r", c=64), o0[:, TC:T].rr("p (c r) -> p c r", r=32), x="pool")
            k.dma("sp", ybr[b, h * 128:(h + 1) * 128, :], yn)
    k.pop()


MAGIC = 12582912.0
TWO_PI_LO = 6.2831850


def sincos_turns(k, out_s, out_c, turns, t1, t2):
    for (dst, off) in ((out_s, 0.0), (out_c, 0.25)):
        if off != 0.0:
            k.ts(t2, turns, off, ALU.add)
            src = t2
        else:
            src = turns
        k.ts(t1, src, MAGIC, ALU.add)
        k.ts(t1, t1, -MAGIC, ALU.add)
        k.tt(t1, src, t1, ALU.subtract)
        k.ts(dst, t1, 0.5, ALU.is_gt)
        k.tt(t1, t1, dst, ALU.subtract)
        k.ts(dst, t1, -0.5, ALU.is_lt)
        k.tt(t1, t1, dst, ALU.add)
        k.act(dst, t1, AF.Sin, scale=TWO_PI_LO)


def s5_declare(pr):
    pr.inp("tau", [128, 513])
    pr.inp("s5_aT", [DEPTH, 2, 2, 128, 16])
    pr.inp("s5_ldtT", [DEPTH, 2, 128, 16])
    pr.inp("s5_bT", [DEPTH, 2, 128, 16, 16])
    pr.inp("s5_cT", [DEPTH, 2, 128, 16, 32])
    pr.inp("s5_dT", [DEPTH, 128, 4])
    pr.inp("s5_glu_w", [DEPTH, 512, 512])
    pr.inp("s5_glu_bT", [DEPTH, 128, 4])
    pr.scratch("S5y", [NB, MIXW, T])


def s5_scan(pr, l):
    k = pr.k
    k.push()
    for _ in s5_scan_gen(pr, l):
        pass
    k.pop()


def s5_scan_gen(pr, l, npy=2, nwork=2):
    k = pr.k
    IN = pr.IN
    P = pr.S["P"]
    I = pr.ident
    tau = k.sb("s5_tau", [128, 513])
    k.dma("sp", tau, IN["tau"])
    cbd32 = [k.sb("s5_c32_%d" % x, [128, 16, 32]) for x in range(2)]
    cbd = [k.sb("s5_c%d" % x, [128, 16, 32], BF16) for x in range(2)]
    for x in range(2):
        k.dma("sp", cbd32[x], IN["s5_cT"][l, x])
    k.cp(cbd[0], cbd32[0], x="act")
    k.ts(cbd[1], cbd32[1], -1.0, ALU.mult)
    bt = [k.sb("s5_b%d" % x, [128, 16, 16]) for x in range(2)]
    for x in range(2):
        k.dma("sp", bt[x], IN["s5_bT"][l, x])
    names = ["are", "aim", "dt", "mag", "tht", "s", "c", "nr", "ni", "den", "fre", "fim", "t1", "t2", "t3"]
    drv = [[k.sb("s5_drv%d%d" % (d, x), [32, 16, 128], BF16) for x in range(2)] for d in range(2)]
    rho = [k.sb("s5_rho%d" % d, [128, 16]) for d in range(2)]
    tht = [k.sb("s5_tht%d" % d, [128, 16]) for d in range(2)]
    cth = [k.sb("s5_cth%d" % d, [128, 16]) for d in range(2)]
    sth = [k.sb("s5_sth%d" % d, [128, 16]) for d in range(2)]
    pAs = [k.ps("s5_pA%d" % q, [128, 512]) for q in range(2)]
    pBs = [k.ps("s5_pB%d" % q, [128, 512]) for q in range(2)]
    pYs = [k.ps("s5_pY%d" % q, [128, 512]) for q in range(npy)]
    pst = pYs[0]
    fbd = [k.sb("s5_fbd%d" % x, [128, 16, 2, 16]) for x in range(2)]
    for x in range(2):
        k.memset(fbd[x], 0.0)
    ft = [k.sb("s5_ft%d" % x, [128, 16, 16]) for x in range(2)]
    ftmp = k.sb("s5_ftmp", [128, 16, 16])
    w = {n: k.sb("s5w_" + n, [128, 16]) for n in names}
    for d in range(2):
        k.dma("sp", w["are"], IN["s5_aT"][l, d, 0])
        k.dma("sp", w["aim"], IN["s5_aT"][l, d, 1])
        k.dma("sp", w["dt"], IN["s5_ldtT"][l, d])
        k.act(w["dt"], w["dt"], AF.Exp)
        k.tt(w["t1"], w["are"], w["dt"], ALU.mult)
        k.act(rho[d], w["t1"], AF.Exp)
        k.tt(w["t1"], w["aim"], w["dt"], ALU.mult)
        k.ts(tht[d], w["t1"], 1.0 / (2.0 * math.pi), ALU.mult)
        sincos_turns(k, sth[d], cth[d], tht[d], w["t2"], w["t3"])
        k.tt(w["nr"], rho[d], cth[d], ALU.mult)
        k.ts(w["nr"], w["nr"], -1.0, ALU.add)
        k.tt(w["ni"], rho[d], sth[d], ALU.mult)
        k.tt(w["den"], w["are"], w["are"], ALU.mult)
        k.tt(w["t1"], w["aim"], w["aim"], ALU.mult)
        k.tt(w["den"], w["den"], w["t1"], ALU.add)
        k.recip(w["den"], w["den"])
        k.tt(w["fre"], w["nr"], w["are"], ALU.mult)
        k.tt(w["t1"], w["ni"], w["aim"], ALU.mult)
        k.tt(w["fre"], w["fre"], w["t1"], ALU.add)
        k.tt(w["fre"], w["fre"], w["den"], ALU.mult)
        k.tt(w["fim"], w["ni"], w["are"], ALU.mult)
        k.tt(w["t1"], w["nr"], w["aim"], ALU.mult)
        k.tt(w["fim"], w["fim"], w["t1"], ALU.subtract)
        k.tt(w["fim"], w["fim"], w["den"], ALU.mult)
        fre3 = w["fre"][:, :, None].bc([128, 16, 16])
        fim3 = w["fim"][:, :, None].bc([128, 16, 16])
        k.tt(ft[0], bt[0], fre3, ALU.mult)
        k.tt(ftmp, bt[1], fim3, ALU.mult)
        k.tt(ft[0], ft[0], ftmp, ALU.subtract)
        k.tt(ft[1], bt[1], fre3, ALU.mult)
        k.tt(ftmp, bt[0], fim3, ALU.mult)
        k.tt(ft[1], ft[1], ftmp, ALU.add)
        for x in range(2):
            k.cp(fbd[x][0:64, :, 0, :], ft[x][0:64], x="dve")
            k.cp(fbd[x][64:128, :, 1, :], ft[x][64:128], x="dve")
            for j0 in range(0, 16, 4):
                for jj in range(4):
                    j = j0 + jj
                    k.tr(pst[0:32, jj * 128:(jj + 1) * 128], fbd[x][:, j, :, :].rr("p a b -> p (a b)"), I)
                k.cp(drv[d][x][:, j0:j0 + 4, :], pst[0:32, :].rr("p (a b) -> p a b", a=4), x="act")
        yield
    Ct = k.sb("s5_Ct", [128, 513])
    St = k.sb("s5_St", [128, 513])
    turns = k.sb("s5_turns", [128, 513])
    ta = k.sb("s5_ta", [128, 513])
    tb = k.sb("s5_tb", [128, 513])
    un32 = [k.sb("s5_un32_%d" % b, [32, T]) for b in range(NB)]
    un = [k.sb("s5_un%d" % b, [32, T], BF16) for b in range(NB)]
    ur = [k.sb("s5_ur%d" % b, [32, T], BF16) for b in range(NB)]
    yacc = [k.sb("s5_yacc%d" % b, [32, T]) for b in range(NB)]
    W = []
    for q in range(nwork):
        W.append({n_: k.sb("s5_%s%d" % (n_, q), [128, 512], BF16 if n_ in ("hr", "hi") else F32)
                  for n_ in ("m1", "m2", "m3", "m4", "xmr", "xmi", "hr", "hi")})
    ini = k.sb("s5_ini", [128, 2])
    it1 = k.sb("s5_it1", [128, 2])
    G2 = [(k.sb("s5_gr%d" % q, [128, 512]), k.sb("s5_gi%d" % q, [128, 512])) for q in range(2)]
    ytmp = k.sb("s5_ytmp", [32, 512])
    tcount = 0
    for j in range(16):
        for b in range(NB):
            k.dma("sp", un32[b], P[b, OFF_S5 + j * 32:OFF_S5 + (j + 1) * 32, :])
            k.cp(un[b], un32[b], x="act")
            seq_copy(k, ur[b], un32[b], False, True, "act")
        for d in range(2):
            k.ts(turns, tau, tht[d][:, j:j + 1], ALU.mult)
            sincos_turns(k, St, Ct, turns, ta, tb)
            yield
            for b in range(NB):
                u = un[b] if d == 0 else ur[b]
                for ti, (s0, n, _ra, _rb) in enumerate(nat_tiles()):
                    q = tcount % 2
                    tcount += 1
                    w_ = W[q % nwork]
                    pA, pB, pY = pAs[q], pBs[q], pYs[q % npy]
                    m1, m2, m3, m4, xmr, xmi, hr, hi = (w_[n_] for n_ in ("m1", "m2", "m3", "m4", "xmr", "xmi", "hr", "hi"))
                    gr, gi = G2[q]
                    k.mm(pA[:, :n], drv[d][0][:, j, :], u[:, s0:s0 + n])
                    k.mm(pB[:, :n], drv[d][1][:, j, :], u[:, s0:s0 + n])
                    k.tt(m1[:, :n], pA[:, :n], Ct[:, :n], ALU.mult, x="dve")
                    k.tt(m2[:, :n], pB[:, :n], St[:, :n], ALU.mult, x="dve")
                    k.tt(xmr[:, :n], m1[:, :n], m2[:, :n], ALU.add, x="dve")
                    k.tt(m3[:, :n], pB[:, :n], Ct[:, :n], ALU.mult, x="dve")
                    k.tt(m4[:, :n], pA[:, :n], St[:, :n], ALU.mult, x="dve")
                    k.tt(xmi[:, :n], m3[:, :n], m4[:, :n], ALU.subtract, x="dve")
                    yield
                    if ti == 0:
                        k.memset(ini, 0.0)
                    else:
                        grp, gip = G2[1 - q]
                        cc, ss = Ct[:, pn:pn + 1], St[:, pn:pn + 1]
                        k.ts(it1[:, 0:1], gip[:, pn - 1:pn], ss, ALU.mult)
                        k.stt(ini[:, 0:1], grp[:, pn - 1:pn], cc, it1[:, 0:1], ALU.mult, ALU.subtract)
                        k.ts(it1[:, 1:2], gip[:, pn - 1:pn], cc, ALU.mult)
                        k.stt(ini[:, 1:2], grp[:, pn - 1:pn], ss, it1[:, 1:2], ALU.mult, ALU.add)
                    k.scan(gr[:, :n], rho[d][:, j:j + 1].bc([128, n]), xmr[:, :n], ini[:, 0:1])
                    k.scan(gi[:, :n], rho[d][:, j:j + 1].bc([128, n]), xmi[:, :n], ini[:, 1:2])
                    yield
                    k.tt(m1[:, :n], gr[:, :n], Ct[:, :n], ALU.mult, x="dve")
                    k.tt(m2[:, :n], gi[:, :n], St[:, :n], ALU.mult, x="dve")
                    k.tt(hr[:, :n], m1[:, :n], m2[:, :n], ALU.subtract, x="dve")
                    k.tt(m3[:, :n], gr[:, :n], St[:, :n], ALU.mult, x="dve")
                    k.tt(m4[:, :n], gi[:, :n], Ct[:, :n], ALU.mult, x="dve")
                    k.tt(hi[:, :n], m3[:, :n], m4[:, :n], ALU.add, x="dve")
                    yield
                    pn = n
                    k.mm(pY[0:32, :n], cbd[0][:, j, :], hr[:, :n], start=True, stop=False)
                    k.mm(pY[0:32, :n], cbd[1][:, j, :], hi[:, :n], start=False, stop=True)
                    if d == 0:
                        k.cp(yacc[b][:, s0:s0 + n], pY[0:32, :n], x="act")
                    else:
                        if s0 < TC:
                            dst = yacc[b][:, 0:TC][:, ::-1]
                        else:
                            dst = yacc[b][:, TC:T][:, ::-1][:, s0 - TC:s0 - TC + n]
                        k.cp(ytmp[:, :n], pY[0:32, :n], x="act")
                        k.tt(dst, dst, ytmp[:, :n], ALU.add, x="pool")
        for b in range(NB):
            k.dma("sp", pr.S["S5y"][b, j * 32:(j + 1) * 32, :], yacc[b])
        yield


def s5_out(pr, l, ybr):
    k = pr.k
    k.push()
    IN = pr.IN
    P = pr.S["P"]
    dT = k.sb("s5o_d", [128, 4])
    gb = k.sb("s5o_gb", [128, 4])
    gw = k.sb("s5o_gw", [128, 4, 512])
    k.dma("sp", dT, IN["s5_dT"][l])
    k.dma("sp", gb, IN["s5_glu_bT"][l])
    k.dma("sp", gw, IN["s5_glu_w"][l].rr("(kc p) n -> p kc n", p=128))
    yt = [k.sb("s5o_y%d" % i, [128, 4, 512]) for i in range(2)]
    ut = [k.sb("s5o_u%d" % i, [128, 4, 512]) for i in range(2)]
    z = k.sb("s5o_z", [128, 4, 512])
    t1 = k.sb("s5o_t1", [128, 4, 512])
    gl = k.sb("s5o_gl", [128, 4, 512])
    ot = [k.sb("s5o_o%d" % i, [128, 512]) for i in range(2)]
    ps = [k.ps("s5o_ps%d" % i, [128, 512]) for i in range(2)]
    it = 0
    oi = 0
    for b in range(NB):
        for (t0, n, ra, rb) in nat_tiles():
            y = yt[it % 2]
            u = ut[it % 2]
            it += 1
            k.dma("sp", y[:, :, :n], pr.S["S5y"][b, :, t0:t0 + n].rr("(c p) t -> p c t", p=128))
            k.dma("pool", u[:, :, :n], P[b, OFF_S5:OFF_S5 + MIXW, t0:t0 + n].rr("(c p) t -> p c t", p=128))
            for c in range(4):
                k.stt(z[:, c, :n], u[:, c, :n], dT[:, c:c + 1], y[:, c, :n], ALU.mult, ALU.add)
            k.tt(t1[:, :, :n], z[:, :, :n], z[:, :, :n], ALU.mult, x="pool")
            k.ts(t1[:, :, :n], t1[:, :, :n], 0.044715, ALU.mult, 1.0, ALU.add)
            k.tt(t1[:, :, :n], t1[:, :, :n], z[:, :, :n], ALU.mult, x="pool")
            k.act(t1[:, :, :n], t1[:, :, :n], AF.Sigmoid, scale=2.0 * math.sqrt(2.0 / math.pi))
            k.tt(gl[:, :, :n], z[:, :, :n], t1[:, :, :n], ALU.mult)
            for oc in range(4):
                pp = ps[oi % 2]
                o = ot[oi % 2]
                oi += 1
                for kc in range(4):
                    k.mm(pp[:, :n], gw[:, kc, oc * 128:(oc + 1) * 128], gl[:, kc, :n], start=(kc == 0), stop=(kc == 3))
                k.act(o[:, :n], pp[:, :n], AF.Sigmoid, bias=gb[:, oc:oc + 1], scale=1.0)
                k.tt(o[:, :n], o[:, :n], gl[:, oc, :n], ALU.mult)
                k.dma("sp", ybr[b, oc * 128:(oc + 1) * 128, t0:t0 + n], o[:, :n])
    k.pop()


def sin_turns(k, dst, turns, t1):
    k.ts(t1, turns, MAGIC, ALU.add)
    k.ts(t1, t1, -MAGIC, ALU.add)
    k.tt(t1, turns, t1, ALU.subtract)
    k.ts(dst, t1, 0.5, ALU.is_gt)
    k.tt(t1, t1, dst, ALU.subtract)
    k.ts(dst, t1, -0.5, ALU.is_lt)
    k.tt(t1, t1, dst, ALU.add)
    k.act(dst, t1, AF.Sin, scale=TWO_PI_LO)


def hyena_declare(pr):
    for L in (TL, TC):
        pr.inp("hy_zT%d" % L, [33, L])
        pr.inp("hy_win%d" % L, [L, MIXW])
        pr.inp("dft_c%d" % L, [L, L])
        pr.inp("dft_s%d" % L, [L, L])
        pr.inp("idft_c%d" % L, [L, L])
        pr.inp("idft_s%d" % L, [L, L])
    pr.inp("hy_cwT", [DEPTH, 3, 128, 12])
    pr.inp("hy_cbT", [DEPTH, 128, 12])
    pr.inp("hy_biasT", [DEPTH, 128, 4])
    pr.inp("hy_f_w1", [DEPTH, 33, 64])
    pr.inp("hy_f_b1T", [DEPTH, 64, 1])
    pr.inp("hy_f_w2", [DEPTH, 64, 64])
    pr.inp("hy_f_b2T", [DEPTH, 64, 1])
    pr.inp("hy_f_w3", [DEPTH, 64, 1024])
    pr.inp("hy_f_b3", [DEPTH, 1, 1024])
    pr.scratch("HYx0", [NB, MIXW, TL])
    pr.scratch("HYz", [NB, MIXW, TL])
    pr.scratch("HYY", [NB, 2, TL, MIXW])


def hyena_stage(pr, l, L, toff, ybr):
    k = pr.k
    IN = pr.IN
    P = pr.S["P"]
    I = pr.ident
    NT = L // 128
    TW = min(L, 512)
    NTT = L // TW
    sfx = "%d" % L
    k.push()
    KC = k.sb("hy_KC", [128, NT, MIXW])
    KS = k.sb("hy_KS", [128, NT, MIXW])
    tabs_cur = [None]
    pacc = [k.ps("hy_pacc%d" % i, [128, 512]) for i in range(4)]
    pw = [k.ps("hy_pw%d" % i, [128, 512]) for i in range(2)]
    tabi = [0]

    def load_tabs(fc):
        tb = tabs_cur[0][tabi[0] % 3]
        tabi[0] += 1
        k.dma("sp", tb[0], IN["dft_c" + sfx][:, fc * 128:(fc + 1) * 128].rr("(tc p) f -> p tc f", p=128))
        k.dma("sp", tb[1], IN["dft_s" + sfx][:, fc * 128:(fc + 1) * 128].rr("(tc p) f -> p tc f", p=128))
        return tb

    k.push()
    tabs_cur[0] = [[k.sb("hy_tabA%d%d" % (i, x), [128, NT, 128]) for x in range(2)] for i in range(3)]
    fw1 = k.sb("hy_fw1", [33, 64])
    fw2 = k.sb("hy_fw2", [64, 64])
    fw3 = k.sb("hy_fw3", [64, 1024])
    fb = k.sb("hy_fb", [64, 2])
    fb3 = k.sb("hy_fb3", [1, 1024])
    h2 = k.sb("hy_h2", [64, L])
    k.push()
    zT = k.sb("hy_zT", [33, L])
    k.dma("sp", fw1, IN["hy_f_w1"][l])
    k.dma("sp", fw2, IN["hy_f_w2"][l])
    k.dma("sp", fw3, IN["hy_f_w3"][l])
    k.dma("sp", fb[:, 0:1], IN["hy_f_b1T"][l])
    k.dma("sp", fb[:, 1:2], IN["hy_f_b2T"][l])
    k.dma("sp", fb3, IN["hy_f_b3"][l])
    k.dma("sp", zT, IN["hy_zT" + sfx])
    h1 = k.sb("hy_h1", [64, L])
    tu = k.sb("hy_tu", [64, 512])
    tv = k.sb("hy_tv", [64, 512])
    for (src, w, bcol, dst) in ((zT, fw1, 0, h1), (h1, fw2, 1, h2)):
        for i in range(NTT):
            pp = pw[i % 2]
            k.mm(pp[0:64, :TW], w, src[:, i * TW:(i + 1) * TW])
            k.ts(tu[:, :TW], pp[0:64, :TW], fb[:, bcol:bcol + 1], ALU.add, 1.0 / (2.0 * math.pi), ALU.mult)
            sin_turns(k, dst[:, i * TW:(i + 1) * TW], tu[:, :TW], tv[:, :TW])
    k.pop()
    hsum = k.sb("hy_hsum", [128, NT, MIXW])
    hdif = k.sb("hy_hdif", [128, NT, MIXW])
    win = [k.sb("hy_win%d" % i, [128, MIXW]) for i in range(2)]
    hf = k.sb("hy_hf", [128, MIXW])
    hb = k.sb("hy_hb", [128, MIXW])
    for tc in range(NT):
        wt = win[tc % 2]
        k.dma("sp", wt, IN["hy_win" + sfx][tc * 128:(tc + 1) * 128, :])
        for half, dst in ((0, hf), (1, hb)):
            pp = pw[half]
            k.mm(pp, h2[:, tc * 128:(tc + 1) * 128], fw3[:, half * 512:(half + 1) * 512], start=True, stop=False)
            k.mm(pp, pr.ones[0:1, :], fb3[:, half * 512:(half + 1) * 512], start=False, stop=True)
            k.tt(dst, pp, wt, ALU.mult)
        k.tt(hdif[:, tc, :], hb, hf, ALU.subtract, x="pool")
        if tc == 0:
            k.memset(hb[0:1, :], 0.0)
        k.tt(hsum[:, tc, :], hf, hb, ALU.add, x="pool")
    for fc in range(NT):
        tb = load_tabs(fc)
        for x, src, dst in ((0, hsum, KC), (1, hdif, KS)):
            pp = pw[x]
            for tc in range(NT):
                k.mm(pp, tb[x][:, tc, :], src[:, tc, :], start=(tc == 0), stop=(tc == NT - 1))
            k.cp(dst[:, fc, :], pp, x=("act" if x == 0 else "dve"))
    k.pop()
    cw = k.sb("hy_cw", [128, 3, 12])
    cb = k.sb("hy_cb", [128, 12])
    hbias = k.sb("hy_bias", [128, 4])
    k.dma("sp", cw, IN["hy_cwT"][l].rr("i p c -> p i c"))
    k.dma("sp", cb, IN["hy_cbT"][l])
    k.dma("sp", hbias, IN["hy_biasT"][l])
    for b in range(NB):
        k.push()
        tabs_cur[0] = [[k.sb("hy_tabB%d%d" % (i, x), [128, NT, 128]) for x in range(2)] for i in range(3)]
        zTm = k.sb("hy_zTm", [128, NT, MIXW])
        raw = [k.sb("hy_raw%d" % i, [128, L + 2]) for i in range(2)]
        uu = [k.sb("hy_u%d" % i, [128, L]) for i in range(3)]
        ri = 0
        for i in range(4):
            for s in range(3):
                ch = s * 4 + i
                r = raw[ri % 2]
                ri += 1
                k.memset(r[:, 0:1], 0.0, x="pool")
                k.memset(r[:, L + 1:L + 2], 0.0, x="pool")
                k.dma("sp", r[:, 1:L + 1], P[b, OFF_HY + ch * 128:OFF_HY + (ch + 1) * 128, toff:toff + L])
                u = uu[s]
                k.act(u, r[:, 1:L + 1], AF.Identity, bias=cb[:, ch:ch + 1], scale=cw[:, 1, ch:ch + 1])
                k.stt(u, r[:, 0:L], cw[:, 0, ch:ch + 1], u, ALU.mult, ALU.add)
                k.stt(u, r[:, 2:L + 2], cw[:, 2, ch:ch + 1], u, ALU.mult, ALU.add)
            k.dma("pool", pr.S["HYx0"][b, i * 128:(i + 1) * 128, 0:L], uu[0])
            k.tt(uu[2], uu[2], uu[1], ALU.mult, x="pool")
            k.dma("pool", pr.S["HYz"][b, i * 128:(i + 1) * 128, 0:L], uu[2])
            for t4 in range(0, NT, 4):
                nn = min(4, NT - t4)
                pp = pw[(t4 // 4) % 2]
                for j in range(nn):
                    k.tr(pp[:, j * 128:(j + 1) * 128], uu[2][:, (t4 + j) * 128:(t4 + j + 1) * 128], I)
                k.cp(zTm[:, t4:t4 + nn, i * 128:(i + 1) * 128], pp[:, 0:nn * 128].rr("p (a b) -> p a b", b=128), x="act")
        uc = k.sb("hy_uc", [128, MIXW])
        us = k.sb("hy_us", [128, MIXW])
        yy = [[k.sb("hy_yy%d%d" % (i, x), [128, MIXW]) for x in range(2)] for i in range(2)]
        m1 = k.sb("hy_m1", [128, MIXW])
        for fc in range(NT):
            tb = load_tabs(fc)
            for x in range(2):
                pp = pw[x]
                for tc in range(NT):
                    k.mm(pp, tb[x][:, tc, :], zTm[:, tc, :], start=(tc == 0), stop=(tc == NT - 1))
            k.cp(uc, pw[0], x="act")
            k.cp(us, pw[1], x="act")
            y = yy[fc % 2]
            k.tt(y[0], uc, KC[:, fc, :], ALU.mult, x="dve")
            k.tt(m1, us, KS[:, fc, :], ALU.mult, x="pool")
            k.tt(y[0], y[0], m1, ALU.add, x="dve")
            k.tt(y[1], uc, KS[:, fc, :], ALU.mult, x="pool")
            k.tt(m1, us, KC[:, fc, :], ALU.mult, x="dve")
            k.tt(y[1], y[1], m1, ALU.subtract, x="pool")
            for x in range(2):
                k.dma("sp" if x == 0 else "pool", pr.S["HYY"][b, x, fc * 128:(fc + 1) * 128, :], y[x])
        k.pop()
        k.push()
        Y = [k.sb("hy_Y%d" % x, [128, NT, MIXW]) for x in range(2)]
        for x in range(2):
            k.dma("sp", Y[x], pr.S["HYY"][b, x, 0:L, :].rr("(fc p) c -> p fc c", p=128))
        itab = [[k.sb("hy_itab%d%d" % (i, x), [128, 512]) for x in range(2)] for i in range(6)]
        zt = [k.sb("hy_zt%d" % i, [128, 512]) for i in range(2)]
        x0t = [k.sb("hy_x0t%d" % i, [128, 512]) for i in range(2)]
        ot = [k.sb("hy_ot%d" % i, [128, 512]) for i in range(2)]
        ii = 0
        oi = 0
        for tt in range(NTT):
            for fc in range(NT):
                tb = itab[ii % 6]
                ii += 1
                k.dma("sp", tb[0][:, :TW], IN["idft_c" + sfx][fc * 128:(fc + 1) * 128, tt * TW:(tt + 1) * TW])
                k.dma("sp", tb[1][:, :TW], IN["idft_s" + sfx][fc * 128:(fc + 1) * 128, tt * TW:(tt + 1) * TW])
                for ci in range(4):
                    k.mm(pacc[ci][:, :TW], Y[0][:, fc, ci * 128:(ci + 1) * 128], tb[0][:, :TW],
                         start=(fc == 0), stop=False)
                    k.mm(pacc[ci][:, :TW], Y[1][:, fc, ci * 128:(ci + 1) * 128], tb[1][:, :TW],
                         start=False, stop=(fc == NT - 1))
            for ci in range(4):
                z = zt[oi % 2]
                x0 = x0t[oi % 2]
                o = ot[oi % 2]
                oi += 1
                k.dma("sp", z[:, :TW], pr.S["HYz"][b, ci * 128:(ci + 1) * 128, tt * TW:(tt + 1) * TW])
                k.dma("pool", x0[:, :TW], pr.S["HYx0"][b, ci * 128:(ci + 1) * 128, tt * TW:(tt + 1) * TW])
                k.stt(o[:, :TW], z[:, :TW], hbias[:, ci:ci + 1], pacc[ci][:, :TW], ALU.mult, ALU.add)
                k.tt(o[:, :TW], o[:, :TW], x0[:, :TW], ALU.mult, x="pool")
                k.dma("sp", ybr[b, ci * 128:(ci + 1) * 128, toff + tt * TW:toff + (tt + 1) * TW], o[:, :TW])
        k.pop()
    k.pop()


def ffn_declare(pr):
    pr.inp("w_branch", [DEPTH, 4, MIXW, D])
    pr.inp("w_out", [DEPTH, D, D])
    pr.inp("ffn_w1", [1, D, D_FF])
    pr.inp("ffn_w3", [1, D, D_FF])
    pr.inp("ffn_w2", [1, D_FF, D])
    pr.inp("moe_router_w", [1, D, NEXP])
    pr.inp("moe_router_b", [1, 1, NEXP])
    pr.inp("moe_w1", [1, NEXP, D, D_FFE])
    pr.inp("moe_w3", [1, NEXP, D, D_FFE])
    pr.inp("moe_w2", [1, NEXP, D_FFE, D])
    pr.inp("fin_gT", [128, 8])
    pr.out = pr.k.dram("out", [NB, D, TL], F32, kind="ExternalOutput")
    pr.scratch("XN2", [128, 8, NB * T], BF16)


def merge_stage(pr, l, hsrc, tiles):
    k = pr.k
    k.push()
    IN = pr.IN
    P = pr.S["P"]
    YBR = pr.S["YBR"]
    stg = k.sb("mg_stg", [128, 4, 1024])
    wbr = k.sb("mg_wbr", [128, 4, 4, 1024], BF16)
    wout = k.sb("mg_wout", [128, 8, 1024], BF16)
    for m in range(4):
        k.dma("sp", stg, IN["w_branch"][l, m].rr("(kc p) n -> p kc n", p=128))
        k.cp(wbr[:, m], stg, x=("act" if m % 2 == 0 else "pool"))
    for hf in range(2):
        k.dma("sp", stg, IN["w_out"][l, hf * 512:(hf + 1) * 512, :].rr("(kc p) n -> p kc n", p=128))
        k.cp(wout[:, hf * 4:(hf + 1) * 4], stg, x=("act" if hf == 0 else "pool"))
    acc = k.sb("mg_acc", [128, 8, 512])
    accb = k.sb("mg_accb", [128, 8, 512], BF16)
    yt = [k.sb("mg_y%d" % i, [128, 4, 512]) for i in range(1)]
    ybf = [k.sb("mg_ybf%d" % i, [128, 4, 512], BF16) for i in range(2)]
    gt = [k.sb("mg_g%d" % i, [128, 8, 512]) for i in range(2)]
    tmp = [k.sb("mg_tmp%d" % i, [128, 512]) for i in range(2)]
    ht = k.sb("mg_h", [128, 8, 512])
    hn = k.sb("mg_hn", [128, 8, 512])
    ps = [k.ps("mg_ps%d" % i, [128, 512]) for i in range(4)]
    it = 0
    pi = 0
    for (b, t0, n, is_ctx) in tiles:
        j = 2 if is_ctx else b
        k.dma("sp", ht[:, :, :n], hsrc[b, :, t0:t0 + n].rr("(kc p) t -> p kc t", p=128))
        for m in range(4):
            y = yt[0]
            yb = ybf[it % 2]
            g = gt[it % 2]
            it += 1
            k.dma("sp", y[:, :, :n], YBR[m, b, :, t0:t0 + n].rr("(kc p) t -> p kc t", p=128))
            k.dma("pool", g[:, :, :n], P[b, OFF_GATE + m * D:OFF_GATE + (m + 1) * D, t0:t0 + n].rr("(oc p) t -> p oc t", p=128))
            k.cp(yb[:, :, :n], y[:, :, :n], x="act")
            for oc in range(8):
                pp = ps[pi % 4]
                pi += 1
                for kc in range(4):
                    k.mm(pp[:, :n], wbr[:, m, kc, oc * 128:(oc + 1) * 128], yb[:, kc, :n], start=(kc == 0), stop=(kc == 3))
                if m == 0:
                    k.tt(acc[:, oc, :n], pp[:, :n], g[:, oc, :n], ALU.mult)
                else:
                    tp = tmp[pi % 2]
                    k.tt(tp[:, :n], pp[:, :n], g[:, oc, :n], ALU.mult)
                    k.tt(acc[:, oc, :n], acc[:, oc, :n], tp[:, :n], ALU.add, x="dve")
        k.cp(accb[:, :, :n], acc[:, :, :n], x="act")
        for oc in range(8):
            pp = ps[pi % 4]
            pi += 1
            for kc in range(8):
                k.mm(pp[:, :n], wout[:, kc, oc * 128:(oc + 1) * 128], accb[:, kc, :n], start=(kc == 0), stop=(kc == 7))
            k.stt(hn[:, oc, :n], pp[:, :n], pr.mod[l][:, 16 + oc, j:j + 1], ht[:, oc, :n], ALU.mult, ALU.add)
        k.dma("pool", pr.S["hbuf"][b, :, t0:t0 + n].rr("(kc p) t -> p kc t", p=128), hn[:, :, :n])
    k.pop()


def norm2_stage(pr, l, tiles, xn2, router=None, xn2_dram=None):
    k = pr.k
    k.push()
    hts = [k.sb("n2_ht%d" % i, [128, 8, 512]) for i in range(2)]
    sq = k.sb("n2_sq", [128, 8, 512])
    tmp = k.sb("n2_tmp", [128, 8, 512])
    rs = k.sb("n2_rs", [128, 512])
    pss = k.ps("n2_pss", [128, 512])
    if xn2 is None:
        xob = [k.sb("n2_xo%d" % i, [128, 8, 512], BF16) for i in range(2)]
    if router is not None:
        gate_tm, rw_, rb_ = router
        xf = k.sb("n2_xf", [128, 8, 512])
        plg = k.ps("n2_plg", [128, 512])
        lg = k.sb("n2_lg", [128, 8])
        e1 = k.sb("n2_e1", [128, 8])
        e2 = k.sb("n2_e2", [128, 8])
        l2 = k.sb("n2_l2", [128, 8])
        sc = k.sb("n2_sc", [128, 8])
    for i, (b, t0, n, is_ctx) in enumerate(tiles):
        ht = hts[i % 2]
        k.dma("sp", ht[:, :, :n], pr.S["hbuf"][b, :, t0:t0 + n].rr("(kc p) t -> p kc t", p=128))
        if xn2 is None:
            xo = xob[i % 2]
            pr.norm_tile(l, 2, ht, n, b, is_ctx, xo[:, :, :n], sq, pss, rs, tmp)
            k.dma("sp", xn2_dram[:, :, b * T + t0:b * T + t0 + n], xo[:, :, :n])
        else:
            pr.norm_tile(l, 2, ht, n, b, is_ctx, xn2[:, :, b * T + t0: b * T + t0 + n], sq, pss, rs, tmp)
        if router is not None:
            j = b
            for kc in range(8):
                k.act(xf[:, kc, :n], tmp[:, kc, :n], AF.Identity, bias=pr.mod[l][:, 24 + kc, j:j + 1], scale=1.0)
            for blk in range(n // 128):
                gblk = (b * TL + (t0 - TC)) // 128 + blk
                pl = plg[:, blk * 8:(blk + 1) * 8]
                for kc in range(8):
                    k.mm(pl, xf[:, kc, blk * 128:(blk + 1) * 128], rw_[:, kc, :], start=(kc == 0), stop=False)
                k.mm(pl, pr.ones[0:1, :], rb_, start=False, stop=True)
            for blk in range(n // 128):
                gblk = (b * TL + (t0 - TC)) // 128 + blk
                k.cp(lg, plg[:, blk * 8:(blk + 1) * 8])
                k.reduce(sc[:, 0:1], lg, ALU.max)
                k.ts(e1, lg, sc[:, 0:1], ALU.is_equal)
                k.stt(l2, e1, -1e30, lg, ALU.mult, ALU.add)
                k.reduce(sc[:, 1:2], l2, ALU.max)
                k.ts(e2, l2, sc[:, 1:2], ALU.is_equal)
                k.tt(sc[:, 2:3], sc[:, 1:2], sc[:, 0:1], ALU.subtract)
                k.act(sc[:, 3:4], sc[:, 2:3], AF.Exp)
                k.ts(sc[:, 4:5], sc[:, 3:4], 1.0, ALU.add)
                k.recip(sc[:, 5:6], sc[:, 4:5])
                k.tt(sc[:, 6:7], sc[:, 3:4], sc[:, 5:6], ALU.mult)
                k.ts(gate_tm[:, gblk, :], e1, sc[:, 5:6], ALU.mult)
                k.stt(gate_tm[:, gblk, :], e2, sc[:, 6:7], gate_tm[:, gblk, :], ALU.mult, ALU.add)
    k.pop()


def super_tiles(tiles, maxtok):
    sts = []
    cur = []
    tot = 0
    for t in tiles:
        if cur and (tot + t[2] > maxtok or cur[0][0] != t[0]):
            sts.append(cur)
            cur, tot = [], 0
        cur.append(t)
        tot += t[2]
    sts.append(cur)
    return sts


def swiglu_stage(pr, l, tiles, xn2, w1, w3, w2, dff, maxtok, experts=None, gate_tm=None, xn2_dram=None):
    k = pr.k
    k.push()
    nff = dff // 128
    I = pr.ident
    actb = k.sb("ff_act", [128, nff, maxtok], BF16)
    NW = 4
    wst = [k.sb("ff_wst%d" % i, [128, 8, 128]) for i in range(NW)]
    wbf = [k.sb("ff_wbf%d" % i, [128, 8, 128], BF16) for i in range(NW)]
    w2st = [k.sb("ff_w2st%d" % i, [128, nff, 128]) for i in range(1)]
    w2bf = [k.sb("ff_w2bf%d" % i, [128, nff, 128], BF16) for i in range(2)]
    st1 = [k.sb("ff_s1_%d" % i, [128, 512]) for i in range(2)]
    pa = [k.ps("ff_pa%d" % i, [128, 512]) for i in range(2)]
    pb = [k.ps("ff_pb%d" % i, [128, 512]) for i in range(2)]
    po = [k.ps("ff_po%d" % i, [128, 512]) for i in range(2)]
    ht = [k.sb("ff_h%d" % i, [128, 512]) for i in range(2)]
    hn = [k.sb("ff_hn%d" % i, [128, 512]) for i in range(2)]
    ne = 1 if experts is None else experts
    if experts is not None:
        acc = k.sb("ff_acc", [128, 8, maxtok])
        GE = [k.sb("ff_GE%d" % i, [128, maxtok]) for i in range(2)]
        gbc = [k.sb("ff_gbc%d" % i, [128, 128]) for i in range(2)]
        tmpg = [k.sb("ff_tg%d" % i, [128, 512]) for i in range(2)]
        pg = k.ps("ff_pg", [128, 512])
    if xn2 is None:
        xloc = k.sb("ff_xloc", [128, 8, maxtok], BF16)
    wi = 0
    w2i = 0
    si = 0
    hi_ = 0
    gi_ = 0
    for st in super_tiles(tiles, maxtok):
        offs = []
        o = 0
        for t in st:
            offs.append(o)
            o += t[2]
        if xn2 is None:
            for ti, (b, t0, n, is_ctx) in enumerate(st):
                k.dma("sp", xloc[:, :, offs[ti]:offs[ti] + n], xn2_dram[:, :, b * T + t0:b * T + t0 + n])
        for e in range(ne):
            W1 = w1 if experts is None else w1[e]
            W3 = w3 if experts is None else w3[e]
            W2 = w2 if experts is None else w2[e]
            if experts is not None:
                ge = GE[e % 2]
                for ti, (b, t0, n, is_ctx) in enumerate(st):
                    for blk in range(n // 128):
                        gblk = (b * TL + (t0 - TC)) // 128 + blk
                        gb_ = gbc[gi_ % 2]
                        gi_ += 1
                        k.cp(gb_, gate_tm[:, gblk, e:e + 1].bc([128, 128]), x="pool")
                        k.mm(pg[:, blk * 128:(blk + 1) * 128], gb_, I)
                    k.cp(ge[:, offs[ti]:offs[ti] + n], pg[:, :n], x="act")
            for ffc in range(nff):
                ws = []
                for wsrc in (W1, W3):
                    s_, b_ = wst[wi % NW], wbf[wi % NW]
                    k.dma("sp", s_, wsrc[:, ffc * 128:(ffc + 1) * 128].rr("(kc p) n -> p kc n", p=128))
                    k.cp(b_, s_, x=("dve" if wi % 2 == 0 else "act"))
                    wi += 1
                    ws.append(b_)
                for ti, (b, t0, n, is_ctx) in enumerate(st):
                    if xn2 is None:
                        xs = xloc[:, :, offs[ti]:offs[ti] + n]
                    else:
                        xs = xn2[:, :, b * T + t0:b * T + t0 + n]
                    p1, p3 = pa[si % 2], pb[si % 2]
                    s1 = st1[si % 2]
                    si += 1
                    for kc in range(8):
                        k.mm(p1[:, :n], ws[0][:, kc, :], xs[:, kc, :], start=(kc == 0), stop=(kc == 7))
                    for kc in range(8):
                        k.mm(p3[:, :n], ws[1][:, kc, :], xs[:, kc, :], start=(kc == 0), stop=(kc == 7))
                    k.act(s1[:, :n], p1[:, :n], AF.Silu)
                    k.tt(actb[:, ffc, offs[ti]:offs[ti] + n], s1[:, :n], p3[:, :n], ALU.mult)
            for oc in range(8):
                s_, b_ = w2st[0], w2bf[w2i % 2]
                k.dma("sp", s_, W2[:, oc * 128:(oc + 1) * 128].rr("(kc p) n -> p kc n", p=128))
                k.cp(b_, s_, x=("act" if w2i % 2 == 0 else "dve"))
                w2i += 1
                for ti, (b, t0, n, is_ctx) in enumerate(st):
                    pp = po[hi_ % 2]
                    for kc in range(nff):
                        k.mm(pp[:, :n], b_[:, kc, :], actb[:, kc, offs[ti]:offs[ti] + n], start=(kc == 0), stop=(kc == nff - 1))
                    j = 2 if is_ctx else b
                    h_, hn_ = ht[hi_ % 2], hn[hi_ % 2]
                    hi_ += 1
                    hv = pr.S["hbuf"][b, oc * 128:(oc + 1) * 128, t0:t0 + n]
                    if experts is not None:
                        a_ = acc[:, oc, offs[ti]:offs[ti] + n]
                        g_ = GE[e % 2][:, offs[ti]:offs[ti] + n]
                        if e == 0:
                            k.tt(a_, pp[:, :n], g_, ALU.mult)
                        else:
                            tg = tmpg[hi_ % 2]
                            k.tt(tg[:, :n], pp[:, :n], g_, ALU.mult)
                            k.tt(a_, a_, tg[:, :n], ALU.add, x="pool")
                        if e < ne - 1:
                            continue
                        k.dma("sp", h_[:, :n], hv)
                        k.stt(hn_[:, :n], a_, pr.mod[l][:, 40 + oc, j:j + 1], h_[:, :n], ALU.mult, ALU.add)
                    else:
                        k.dma("sp", h_[:, :n], hv)
                        k.stt(hn_[:, :n], pp[:, :n], pr.mod[l][:, 40 + oc, j:j + 1], h_[:, :n], ALU.mult, ALU.add)
                    k.dma("sp", hv, hn_[:, :n])
    k.pop()


def final_stage(pr):
    k = pr.k
    k.push()
    fg = k.sb("fin_g", [128, 8])
    k.dma("sp", fg, pr.IN["fin_gT"])
    hts = [k.sb("fin_ht%d" % i, [128, 8, 512]) for i in range(2)]
    sq = k.sb("fin_sq", [128, 8, 512])
    ot = [k.sb("fin_o%d" % i, [128, 8, 512]) for i in range(2)]
    rs = k.sb("fin_rs", [128, 512])
    pss = k.ps("fin_pss", [128, 512])
    i = 0
    for (b, t0, n, is_ctx) in tok_tiles():
        if is_ctx:
            continue
        ht = hts[i % 2]
        o = ot[i % 2]
        i += 1
        k.dma("sp", ht, pr.S["hbuf"][b, :, t0:t0 + n].rr("(kc p) t -> p kc t", p=128))
        k.act(sq, ht, AF.Square)
        for kc in range(8):
            k.mm(pss, pr.ones, sq[:, kc, :], start=(kc == 0), stop=(kc == 7))
        k.ts(rs, pss, 1.0 / D, ALU.mult, RMS_EPS, ALU.add)
        k.act(rs, rs, AF.Sqrt)
        k.recip(rs, rs)
        for kc in range(8):
            k.stt(o[:, kc, :], ht[:, kc, :], fg[:, kc:kc + 1], rs, ALU.mult, ALU.mult)
        k.dma("pool", pr.out[b, :, t0 - TC:t0 - TC + n].rr("(kc p) t -> p kc t", p=128), o)
    k.pop()


_PROG = {}


def kernel(**inputs):
    if "nc" not in _PROG:
        nc = bass.Bass("TRN2", target_bir_lowering=False)
        pr = Prog(nc)
        pr.build()
        _PROG["nc"] = nc
        _PROG["names"] = set(pr.IN.keys())
    nc = _PROG["nc"]
    sh = shared_inputs(inputs)
    maps = []
    for c in range(NCORE):
        m = dict(sh)
        m.update(core_inputs(inputs, c))
        maps.append({k_: v for k_, v in m.items() if k_ in _PROG["names"]})
    res = run_bass_kernel_spmd(nc, maps, core_ids=list(range(NCORE)))
    outs = [np.asarray(r["out"]) for r in res.results]
    full = np.concatenate(outs, axis=0)
    return np.ascontiguousarray(full.transpose(0, 2, 1)).astype(np.float32)
```

```python
import math
from contextlib import ExitStack
import numpy as np
import concourse.bass as bass
import concourse.mybir as mybir
from concourse.bass_utils import run_bass_kernel_spmd

F32 = mybir.dt.float32
BF16 = mybir.dt.bfloat16
I32 = mybir.dt.int32
ALU = mybir.AluOpType
AF = mybir.ActivationFunctionType
AX = mybir.AxisListType

D = 1024
TC = 256
TL = 2048
T = TC + TL
NB = 2
NCORE = 8
DEPTH = 2
MIXW = 512
IN_TOTAL = 9632
OFF_HY, OFF_RW, OFF_S5, OFF_GLA, OFF_GATE = 0, 1536, 3456, 3968, 5536
D_FF = 2816
D_FFE = 3584
NEXP = 8
RMS_EPS = 1e-6


class Res:
    __slots__ = ("name", "lw", "rd", "psum", "dram", "ws")

    def __init__(self, name, psum=False, dram=False):
        self.name = name
        self.lw = None
        self.rd = {}
        self.psum = psum
        self.dram = dram
        self.ws = {}


class V:
    __slots__ = ("ap", "res")

    def __init__(self, ap, res):
        self.ap = ap
        self.res = res

    def __getitem__(self, idx):
        return V(self.ap[idx], self.res)

    def bc(self, shape):
        return V(self.ap.to_broadcast(shape), self.res)

    def rr(self, pat, **kw):
        return V(self.ap.rearrange(pat, **kw), self.res)


class KB:
    ENG = ("pe", "act", "dve", "pool", "sp")
    NS = 16

    def __init__(self, nc):
        self.nc = nc
        self.es = ExitStack()
        self.e = dict(pe=nc.tensor, act=nc.scalar, dve=nc.vector, pool=nc.gpsimd, sp=nc.sync)
        self.cnt = {k: 0 for k in self.ENG}
        self.sem = {k: self.es.enter_context(nc.semaphore("s_" + k)) for k in self.ENG}
        self.waited = {k: {} for k in self.ENG}
        self.dq = {}
        for q in ("sp", "act", "pool"):
            self.dq[q] = dict(i=0, sems=[self.es.enter_context(nc.semaphore("d_%s%d" % (q, s))) for s in range(self.NS)],
                              vals=[0] * self.NS)
        self.scopes = []
        self.dram_res = []
        self.marks = []
        self.uid = 0
        self.n_ins = 0

    def _nm(self, name):
        self.uid += 1
        return "%s_%d" % (name, self.uid)

    def _stack(self):
        return self.scopes[-1] if self.scopes else self.es

    def sb(self, name, shape, dtype=F32):
        t = self._stack().enter_context(self.nc.sbuf_tensor(self._nm(name), list(shape), dtype))
        return V(t[tuple(slice(None) for _ in shape)], Res(name))

    def ps(self, name, shape, dtype=F32):
        t = self._stack().enter_context(self.nc.psum_tensor(self._nm(name), list(shape), dtype))
        return V(t[tuple(slice(None) for _ in shape)], Res(name, psum=True))

    def ps_split(self, name, nbanks, width):
        out = []
        for bk in range(nbanks):
            t = self.ps("%s_b%d" % (name, bk), [128, 512])
            for j in range(512 // width):
                out.append(V(t.ap[:, j * width:(j + 1) * width], Res("%s_%d_%d" % (name, bk, j))))
        return out

    def dram(self, name, shape, dtype=F32, kind="Internal"):
        t = self.nc.dram_tensor(name, list(shape), dtype, kind=kind)
        r = Res(name, dram=True)
        self.dram_res.append(r)
        return V(t.ap(), r)

    def push(self):
        st = ExitStack()
        self.scopes.append(st)
        return st

    def pop(self):
        if len(self.scopes) == 1:
            self.marks.append(dict(self.cnt))
        self.barrier()
        st = self.scopes.pop()
        st.close()

    def _semof(self, key):
        if isinstance(key, str):
            return self.sem[key]
        _, q, s = key
        return self.dq[q]["sems"][s]

    def _wait(self, x, ev):
        key, val = ev
        if x == "pe" and key == "pe":
            return
        if self.waited[x].get(key, 0) >= val:
            return
        self.e[x].wait_ge(self._semof(key), val)
        self.waited[x][key] = val
        self.n_ins += 1

    def _deps(self, reads, writes):
        evs = {}

        def add(ev):
            if ev is None:
                return
            k, v = ev
            if evs.get(k, 0) < v:
                evs[k] = v
        for r in reads:
            add(r.lw)
            for k, v in r.ws.items():
                add((k, v))
            if r.psum:
                for k, v in r.rd.items():
                    add((k, v))
        for w in writes:
            if not w.dram:
                add(w.lw)
            for k, v in w.rd.items():
                add((k, v))
        return list(evs.items())

    def _commit(self, ev, reads, writes):
        k, v = ev
        for r in reads:
            if r.rd.get(k, 0) < v:
                r.rd[k] = v
        for w in writes:
            w.lw = ev
            if w.dram:
                if w.ws.get(k, 0) < v:
                    w.ws[k] = v
            else:
                w.rd = {}

    def op(self, x, fn, reads, writes, *args, **kw):
        for ev in self._deps(reads, writes):
            self._wait(x, ev)
        ins = getattr(self.e[x], fn)(*args, **kw)
        self.cnt[x] += 1
        ev = (x, self.cnt[x])
        ins.then_inc(self.sem[x], 1)
        self._commit(ev, reads, writes)
        self.n_ins += 1
        return ins

    def dma(self, q, out, in_, **kw):
        d = self.dq[q]
        s = d["i"] % self.NS
        d["i"] += 1
        key = ("d", q, s)
        if d["vals"][s] > 0:
            self._wait(q, (key, d["vals"][s]))
        for ev in self._deps([in_.res], [out.res]):
            self._wait(q, ev)
        ins = self.e[q].dma_start(out=out.ap, in_=in_.ap, **kw)
        d["vals"][s] += 16
        ins.then_inc(d["sems"][s], 16)
        self._commit((key, d["vals"][s]), [in_.res], [out.res])
        self.n_ins += 1

    def barrier(self):
        evs = [(k, self.cnt[k]) for k in self.ENG if self.cnt[k] > 0]
        for q, d in self.dq.items():
            for s in range(self.NS):
                if d["vals"][s] > 0:
                    evs.append((("d", q, s), d["vals"][s]))
        for x in self.ENG:
            for ev in evs:
                if ev[0] == x:
                    continue
                self._wait(x, ev)
        for r in self.dram_res:
            r.ws = {}
            r.rd = {}

    R32 = False

    def mm(self, out, lhsT, rhs, start=True, stop=True, r32=None):
        la, ra = lhsT.ap, rhs.ap
        if (self.R32 if r32 is None else r32) and la.dtype == F32 and ra.dtype == F32:
            la = la.bitcast(mybir.dt.float32r)
            ra = ra.bitcast(mybir.dt.float32r)
        self.op("pe", "matmul", [lhsT.res, rhs.res], [out.res], out.ap, la, ra, start=start, stop=stop)

    def tr(self, out, in_, ident):
        self.op("pe", "transpose", [in_.res, ident.res], [out.res], out.ap, in_.ap, ident.ap)

    def act(self, out, in_, func, bias=None, scale=None, x="act"):
        rd = [in_.res]
        kw = {}
        if bias is not None:
            if isinstance(bias, V):
                rd.append(bias.res)
                kw["bias"] = bias.ap
            else:
                kw["bias"] = float(bias)
        if scale is not None:
            if isinstance(scale, V):
                rd.append(scale.res)
                kw["scale"] = scale.ap
            else:
                kw["scale"] = float(scale)
        self.op("act", "activation", rd, [out.res], out.ap, in_.ap, func, **kw)

    def tt(self, out, in0, in1, op, x="dve"):
        self.op(x, "tensor_tensor", [in0.res, in1.res], [out.res], out.ap, in0.ap, in1.ap, op)

    def ts(self, out, in0, s1, op0, s2=None, op1=None, x="dve"):
        rd = [in0.res]
        a1 = s1
        if isinstance(s1, V):
            rd.append(s1.res)
            a1 = s1.ap
        a2 = s2
        if isinstance(s2, V):
            rd.append(s2.res)
            a2 = s2.ap
        if op1 is None:
            self.op(x, "tensor_scalar", rd, [out.res], out.ap, in0.ap, a1, None, op0)
        else:
            self.op(x, "tensor_scalar", rd, [out.res], out.ap, in0.ap, a1, a2, op0, op1)

    def stt(self, out, in0, scalar, in1, op0, op1):
        rd = [in0.res, in1.res]
        a = scalar
        if isinstance(scalar, V):
            rd.append(scalar.res)
            a = scalar.ap
        self.op("dve", "scalar_tensor_tensor", rd, [out.res], out.ap, in0.ap, a, in1.ap, op0, op1)

    def cp(self, out, in_, x="dve"):
        if x == "act":
            self.op("act", "copy", [in_.res], [out.res], out.ap, in_.ap)
        else:
            self.op(x, "tensor_copy", [in_.res], [out.res], out.ap, in_.ap)

    def memset(self, out, val, x="dve"):
        self.op(x, "memset", [], [out.res], out.ap, val)

    def scan(self, out, d0, d1, init, op0=ALU.mult, op1=ALU.add):
        rd = [d0.res, d1.res]
        a = init
        if isinstance(init, V):
            rd.append(init.res)
            a = init.ap
        self.op("dve", "tensor_tensor_scan", rd, [out.res], out.ap, d0.ap, d1.ap, a, op0, op1)

    def recip(self, out, in_):
        self.op("dve", "reciprocal", [in_.res], [out.res], out.ap, in_.ap)

    def reduce(self, out, in_, op, axis=AX.X):
        self.op("dve", "tensor_reduce", [in_.res], [out.res], out.ap, in_.ap, axis, op)

    def iota(self, out, pattern, base=0, cm=0):
        self.op("pool", "iota", [], [out.res], out.ap, pattern, base=base, channel_multiplier=cm,
                allow_small_or_imprecise_dtypes=True)

    def finish(self):
        self.barrier()
        self.es.close()


def w_in_chunks():
    ch = []
    c = 0
    while c < IN_TOTAL:
        if c == OFF_GLA + 1536:
            ch.append((c, 32))
            c += 32
        else:
            ch.append((c, 128))
            c += 128
    assert c == IN_TOTAL
    return ch


def tok_tiles():
    tl = []
    for b in range(NB):
        tl.append((b, 0, TC, True))
        for i in range(TL // 512):
            tl.append((b, TC + i * 512, 512, False))
    return tl


class Prog:
    def __init__(self, nc, dbg=(), upto="all", p_input=False, only=None):
        self.p_input = p_input
        self.only = only
        self.nc = nc
        self.k = KB(nc)
        self.dbg = set(dbg)
        self.upto = upto
        self.IN = {}
        self.S = {}

    def inp(self, name, shape, dtype=F32):
        self.IN[name] = self.k.dram(name, shape, dtype, kind="ExternalInput")
        return self.IN[name]

    def scratch(self, name, shape, dtype=F32):
        kind = "ExternalOutput" if name in self.dbg else "Internal"
        self.S[name] = self.k.dram(name, shape, dtype, kind=kind)
        return self.S[name]

    def declare(self):
        inp = self.inp
        inp("h0", [NB, D, T])
        inp("cT", [128, 8, 3])
        inp("ident", [128, 128])
        inp("cmask", [4, 128, 128])
        inp("rmask", [128, 256])
        inp("ada_w", [DEPTH, D, 6 * D])
        inp("ada_bT", [DEPTH, 128, 48])
        inp("gmixT", [DEPTH, 128, 8])
        inp("gffnT", [DEPTH, 128, 8])
        inp("w_in", [DEPTH, D, IN_TOTAL])
        sc = self.scratch
        if self.only is not None and any(o in ("ffn0", "moe", "final") for o in self.only):
            self.S["hbuf"] = self.k.dram("hbuf", [NB, D, T], F32, kind="ExternalOutput")
            inp("hbuf_in", [NB, D, T])
        else:
            sc("hbuf", [NB, D, T])
        if self.p_input:
            inp("P", [NB, IN_TOTAL, T])
            self.S["P"] = self.IN["P"]
        else:
            sc("P", [NB, IN_TOTAL, T])
        sc("YBR", [4, NB, MIXW, T])
        rwkv_declare(self)
        gla_declare(self)
        s5_declare(self)
        hyena_declare(self)
        ffn_declare(self)
        sc("modout", [DEPTH, 128, 48 * 3])
        sc("xnout", [128, 8 * NB * T], BF16)

    def consts(self):
        k = self.k
        self.ident = k.sb("ident", [128, 128])
        k.dma("sp", self.ident, self.IN["ident"])
        self.ones = k.sb("ones", [128, 128])
        k.memset(self.ones, 1.0)
        self.cmask = k.sb("cmask", [128, 4, 128])
        k.dma("sp", self.cmask, self.IN["cmask"].rr("i p t -> p i t"))
        self.m_up, self.m_lo, self.m_upi, self.ones_bd = (self.cmask[:, i, :] for i in range(4))
        self.rmask = k.sb("rmask", [128, SEG])
        k.dma("sp", self.rmask, self.IN["rmask"])
        self.mod = [k.sb("mod%d" % l, [128, 48, 3]) for l in range(DEPTH)]
        self.A1 = [k.sb("A1_%d" % l, [128, 8, 3]) for l in range(DEPTH)]
        self.A2 = [k.sb("A2_%d" % l, [128, 8, 3]) for l in range(DEPTH)]

    def stage_mods(self):
        k = self.k
        k.push()
        sT = k.sb("sT", [128, 8, 3])
        k.dma("sp", sT, self.IN["cT"])
        k.act(sT, sT, AF.Silu)
        wb = [k.sb("adaw%d" % i, [128, 8, 512]) for i in range(2)]
        pm = [k.ps("pmod%d" % i, [128, 4, 3]) for i in range(2)]
        adab = k.sb("adab", [128, 48])
        gm = k.sb("gm", [128, 8])
        gf = k.sb("gf", [128, 8])
        it = 0
        for l in range(DEPTH):
            k.dma("sp", adab, self.IN["ada_bT"][l])
            k.dma("sp", gm, self.IN["gmixT"][l])
            k.dma("sp", gf, self.IN["gffnT"][l])
            for og in range(12):
                w = wb[it % 2]
                p = pm[it % 2]
                it += 1
                k.dma("sp" if og % 2 == 0 else "pool", w,
                      self.IN["ada_w"][l, :, og * 512:(og + 1) * 512].rr("(kc p) n -> p kc n", p=128))
                for j in range(4):
                    for kc in range(8):
                        k.mm(p[:, j, :], w[:, kc, j * 128:(j + 1) * 128], sT[:, kc, :], start=(kc == 0), stop=(kc == 7))
                for j in range(4):
                    oc = og * 4 + j
                    k.ts(self.mod[l][:, oc, :], p[:, j, :], adab[:, oc:oc + 1], ALU.add)
            k.stt(self.A1[l], self.mod[l][:, 8:16, :], 1.0, gm[:, :, None].bc([128, 8, 3]), ALU.add, ALU.mult)
            k.stt(self.A2[l], self.mod[l][:, 32:40, :], 1.0, gf[:, :, None].bc([128, 8, 3]), ALU.add, ALU.mult)
            if "modout" in self.dbg:
                k.dma("sp", self.S["modout"][l], self.mod[l].rr("p a b -> p (a b)"))
        k.pop()

    def norm_tile(self, l, which, ht, n, b, is_ctx, xn_out, sq, pss, rs, tmp):
        k = self.k
        j = 2 if is_ctx else b
        A = (self.A1 if which == 1 else self.A2)[l]
        sh0 = 0 if which == 1 else 24
        k.act(sq[:, :, :n], ht[:, :, :n], AF.Square)
        for kc in range(8):
            k.mm(pss[:, :n], self.ones, sq[:, kc, :n], start=(kc == 0), stop=(kc == 7))
        k.ts(rs[:, :n], pss[:, :n], 1.0 / D, ALU.mult, RMS_EPS, ALU.add)
        k.act(rs[:, :n], rs[:, :n], AF.Sqrt)
        k.recip(rs[:, :n], rs[:, :n])
        for kc in range(8):
            k.stt(tmp[:, kc, :n], ht[:, kc, :n], A[:, kc, j:j + 1], rs[:, :n], ALU.mult, ALU.mult)
            k.act(xn_out[:, kc, :n], tmp[:, kc, :n], AF.Identity, bias=self.mod[l][:, sh0 + kc, j:j + 1], scale=1.0)

    def stage_norm1(self, l, src):
        k = self.k
        hts = [k.sb("ht%d" % i, [128, 8, 512]) for i in range(2)]
        sq = k.sb("sq", [128, 8, 512])
        tmp = k.sb("ntmp", [128, 8, 512])
        rs = k.sb("rs", [128, 512])
        pss = k.ps("pss", [128, 512])
        for i, (b, t0, n, is_ctx) in enumerate(tok_tiles()):
            ht = hts[i % 2]
            k.dma("sp", ht[:, :, :n], src[b, :, t0:t0 + n].rr("(kc p) t -> p kc t", p=128))
            self.norm_tile(l, 1, ht, n, b, is_ctx, self.xn[:, :, b * T + t0: b * T + t0 + n], sq, pss, rs, tmp)

    def stage_win(self, l):
        k = self.k
        chunks = w_in_chunks()
        groups = []
        cur = []
        for c in chunks:
            if cur and (c[0] + c[1] - cur[0][0] > 512):
                groups.append(cur)
                cur = []
            cur.append(c)
        groups.append(cur)
        wst = [k.sb("wst%d" % i, [128, 8, 512]) for i in range(2)]
        wbf = [k.sb("wbf%d" % i, [128, 8, 512], BF16) for i in range(2)]
        pp = [k.ps("pwin%d" % i, [128, 512]) for i in range(4)]
        ost = [k.sb("ost%d" % i, [128, 512]) for i in range(4)]
        tiles = tok_tiles()
        it = 0
        P = self.S["P"]
        for gi, g in enumerate(groups):
            g0 = g[0][0]
            g1 = g[-1][0] + g[-1][1]
            ws = wst[gi % 2]
            wb = wbf[gi % 2]
            k.dma("sp", ws[:, :, :g1 - g0],
                  self.IN["w_in"][l, :, g0:g1].rr("(kc p) n -> p kc n", p=128))
            if gi % 2 == 0:
                k.cp(wb[:, :, :g1 - g0], ws[:, :, :g1 - g0], x="dve")
            else:
                k.cp(wb[:, :, :g1 - g0], ws[:, :, :g1 - g0], x="act")
            for (c0, m) in g:
                is_gate = c0 >= OFF_GATE
                for (b, t0, n, is_ctx) in tiles:
                    p = pp[it % 4]
                    o = ost[it % 4]
                    for kc in range(8):
                        k.mm(p[:m, :n], wb[:, kc, c0 - g0:c0 - g0 + m], self.xn[:, kc, b * T + t0:b * T + t0 + n],
                             start=(kc == 0), stop=(kc == 7))
                    if is_gate:
                        k.act(o[:m, :n], p[:m, :n], AF.Sigmoid)
                    elif it % 2 == 0:
                        k.cp(o[:m, :n], p[:m, :n], x="dve")
                    else:
                        k.cp(o[:m, :n], p[:m, :n], x="act")
                    k.dma("sp" if it % 4 != 3 else "pool", P[b, c0:c0 + m, t0:t0 + n], o[:m, :n])
                    it += 1

    def build(self):
        k = self.k
        self.declare()
        self.consts()
        if self.only is not None:
            for l in range(1):
                if "rwkv" in self.only or "rwkv_prep" in self.only:
                    rwkv_prep(self, l)
                if "rwkv" in self.only or "rwkv_scan" in self.only:
                    rwkv_scan(self, l)
                if "rwkv" in self.only or "rwkv_read" in self.only:
                    rwkv_readout(self, l, self.S["YBR"][1])
                if "rws5" in self.only:
                    rwkv_prep(self, l)
                    rwkv_s5_scan(self, l)
                    rwkv_readout(self, l, self.S["YBR"][1])
                    s5_out(self, l, self.S["YBR"][2])
                if "hyena" in self.only:
                    hyena_stage(self, l, TL, TC, self.S["YBR"][0])
                    hyena_stage(self, l, TC, 0, self.S["YBR"][0])
                if "s5" in self.only:
                    s5_scan(self, l)
                    s5_out(self, l, self.S["YBR"][2])
                if "gla" in self.only:
                    gla_prep(self, l)
                    gla_scan(self, l)
                    gla_readout(self, l, self.S["YBR"][3])
            if any(o in ("ffn0", "moe", "final") for o in self.only):
                self.stage_mods()
                k.dma("sp", self.S["hbuf"], self.IN["hbuf_in"])
                alltiles = tok_tiles()
                lat = [t for t in alltiles if not t[3]]
                k.push()
                if "ffn0" in self.only:
                    xn2 = k.sb("xn2", [128, 8, NB * T], BF16)
                    norm2_stage(self, 0, alltiles, xn2)
                    swiglu_stage(self, 0, alltiles, xn2, self.IN["ffn_w1"][0], self.IN["ffn_w3"][0],
                                 self.IN["ffn_w2"][0], D_FF, 1280)
                if "moe" in self.only:
                    l = 1
                    gate_tm = k.sb("gate_tm", [128, NB * TL // 128, NEXP])
                    rw_ = k.sb("router_w", [128, 8, NEXP])
                    rb_ = k.sb("router_b", [1, NEXP])
                    k.dma("sp", rw_, self.IN["moe_router_w"][0].rr("(kc p) e -> p kc e", p=128))
                    k.dma("sp", rb_, self.IN["moe_router_b"][0])
                    norm2_stage(self, l, lat, None, router=(gate_tm, rw_, rb_), xn2_dram=self.S["XN2"])
                    swiglu_stage(self, l, lat, None, self.IN["moe_w1"][0], self.IN["moe_w3"][0],
                                 self.IN["moe_w2"][0], D_FFE, 1024, experts=NEXP, gate_tm=gate_tm, xn2_dram=self.S["XN2"])
                k.pop()
                if "final" in self.only:
                    final_stage(self)
            return self.end()
        self.stage_mods()
        if self.upto == "mods":
            return self.end()
        S = self.S
        alltiles = tok_tiles()
        lat = [t for t in alltiles if not t[3]]
        for l in range(DEPTH):
            last = (l == DEPTH - 1)
            hsrc = self.IN["h0"] if l == 0 else S["hbuf"]
            if not self.p_input:
                k.push()
                self.xn = k.sb("xn", [128, 8, NB * T], BF16)
                self.stage_norm1(l, hsrc)
                self.stage_win(l)
                k.pop()
            if self.upto == "win":
                return self.end()
            hyena_stage(self, l, TL, TC, S["YBR"][0])
            if not last:
                hyena_stage(self, l, TC, 0, S["YBR"][0])
            rwkv_prep(self, l)
            rwkv_scan(self, l)
            rwkv_readout(self, l, S["YBR"][1])
            s5_scan(self, l)
            s5_out(self, l, S["YBR"][2])
            gla_prep(self, l)
            gla_scan(self, l)
            gla_readout(self, l, S["YBR"][3])
            if self.upto == "mixers":
                return self.end()
            merge_stage(self, l, hsrc, lat if last else alltiles)
            if self.upto == "merge":
                return self.end()
            k.push()
            if l % 2 == 0:
                xn2 = k.sb("xn2", [128, 8, NB * T], BF16)
                norm2_stage(self, l, alltiles, xn2)
                swiglu_stage(self, l, alltiles, xn2, self.IN["ffn_w1"][l // 2], self.IN["ffn_w3"][l // 2],
                             self.IN["ffn_w2"][l // 2], D_FF, 1280)
            else:
                gate_tm = k.sb("gate_tm", [128, NB * TL // 128, NEXP])
                rw_ = k.sb("router_w", [128, 8, NEXP])
                rb_ = k.sb("router_b", [1, NEXP])
                k.dma("sp", rw_, self.IN["moe_router_w"][l // 2].rr("(kc p) e -> p kc e", p=128))
                k.dma("sp", rb_, self.IN["moe_router_b"][l // 2])
                norm2_stage(self, l, lat, None, router=(gate_tm, rw_, rb_), xn2_dram=S["XN2"])
                swiglu_stage(self, l, lat, None, self.IN["moe_w1"][l // 2], self.IN["moe_w3"][l // 2],
                             self.IN["moe_w2"][l // 2], D_FFE, 1024, experts=NEXP, gate_tm=gate_tm, xn2_dram=S["XN2"])
            k.pop()
            if self.upto == "layer%d" % l:
                return self.end()
        final_stage(self)
        return self.end()

    def end(self):
        self.k.finish()
        return self.nc


def _fm(v, nch):
    return np.ascontiguousarray(np.asarray(v, np.float32).reshape(nch, 128).T)


def shared_inputs(inputs):
    sh = {}
    sh["ident"] = np.eye(128, dtype=np.float32)
    r = np.arange(128)
    same = (r[:, None] // 64) == (r[None, :] // 64)
    sh["cmask"] = np.stack([same & (r[:, None] < r[None, :]), same & (r[:, None] > r[None, :]),
                            same & (r[:, None] <= r[None, :]), same]).astype(np.float32)
    sh["rmask"] = np.ascontiguousarray(np.broadcast_to((np.arange(256) % 64 != 0).astype(np.float32), (128, 256)))
    rw = lambda n: np.asarray(inputs[n], np.float32)
    sh["rw_muT"] = np.stack([[_fm(rw("rw_mu")[l, i], 15) for i in range(2)] for l in range(DEPTH)])
    sh["rw_w0T"] = np.stack([[_fm(rw("rw_w0")[l, i], 4) for i in range(2)] for l in range(DEPTH)])
    sh["rw_a0T"] = np.stack([[_fm(rw("rw_a0")[l, i], 4) for i in range(2)] for l in range(DEPTH)])
    sh["rw_w_up"] = np.ascontiguousarray(rw("rw_w_up").reshape(DEPTH, 128, 512))
    sh["rw_a_up"] = np.ascontiguousarray(rw("rw_a_up").reshape(DEPTH, 128, 512))
    sh["rw_g_up"] = np.ascontiguousarray(rw("rw_g_up"))
    sh["gla_gate_up"] = np.ascontiguousarray(rw("gla_gate_up"))
    sh["gla_gate_bT"] = np.stack([[_fm(rw("gla_gate_b")[l, d], 2) for d in range(2)] for l in range(DEPTH)])
    sh["gla_norm_gT"] = np.ascontiguousarray(rw("gla_norm_g").reshape(DEPTH, 128, 1))
    for L in (TL, TC):
        n = 2 * L
        pos = np.arange(L, dtype=np.float64)
        t = pos / max(L - 1, 1)
        freqs = np.linspace(1e-4, 16 - 1, 16)
        ang = (2.0 * math.pi / L) * pos[:, None] * freqs[None, :]
        z = np.concatenate([t[:, None], np.cos(ang), -np.sin(ang)], axis=-1)
        sh["hy_zT%d" % L] = np.ascontiguousarray(z.T.astype(np.float32))
        rates = np.abs(np.linspace(math.log(1e-2) / 1.5, math.log(1e-2) / 0.3, MIXW))
        sh["hy_win%d" % L] = np.exp(-t[:, None] * rates[None, :]).astype(np.float32)
        om = 2.0 * math.pi * (np.arange(L, dtype=np.float64) + 0.5) / n
        ph = pos[:, None] * om[None, :]
        sh["dft_c%d" % L] = np.cos(ph).astype(np.float32)
        sh["dft_s%d" % L] = np.sin(ph).astype(np.float32)
        sh["idft_c%d" % L] = np.ascontiguousarray((np.cos(ph).T / L).astype(np.float32))
        sh["idft_s%d" % L] = np.ascontiguousarray((-np.sin(ph).T / L).astype(np.float32))
    sh["hy_cwT"] = np.stack([[_fm(rw("hy_conv_w")[l, i], 12) for i in range(3)] for l in range(DEPTH)])
    sh["hy_cbT"] = np.stack([_fm(rw("hy_conv_b")[l], 12) for l in range(DEPTH)])
    sh["hy_biasT"] = np.stack([_fm(rw("hy_bias")[l], 4) for l in range(DEPTH)])
    sh["hy_f_w1"] = rw("hy_f_w1"); sh["hy_f_w2"] = rw("hy_f_w2"); sh["hy_f_w3"] = rw("hy_f_w3")
    sh["hy_f_b1T"] = np.ascontiguousarray(rw("hy_f_b1")[:, :, None]); sh["hy_f_b2T"] = np.ascontiguousarray(rw("hy_f_b2")[:, :, None])
    sh["hy_f_b3"] = np.ascontiguousarray(rw("hy_f_b3")[:, None, :])
    for n in ("w_branch", "w_out", "ffn_w1", "ffn_w3", "ffn_w2", "moe_router_w", "moe_w1", "moe_w3", "moe_w2"):
        sh[n] = np.ascontiguousarray(rw(n))
    sh["moe_router_b"] = np.ascontiguousarray(rw("moe_router_b")[:, None, :])
    sh["fin_gT"] = _fm(rw("final_norm_g"), 8)
    sh["tau"] = np.ascontiguousarray(np.broadcast_to(np.arange(513, dtype=np.float32), (128, 513)))
    def pairT(a):
        a = np.asarray(a, np.float32)
        rest = a.shape[2:]
        return np.ascontiguousarray(np.moveaxis(a.reshape((16, 128) + rest), 0, 1))
    sh["s5_aT"] = np.stack([[[pairT(rw(n)[l, d]) for n in ("s5_a_re", "s5_a_im")] for d in range(2)] for l in range(DEPTH)])
    sh["s5_ldtT"] = np.stack([[pairT(np.repeat(rw("s5_log_dt")[l, d][:, None], 64, axis=1)) for d in range(2)] for l in range(DEPTH)])
    sh["s5_bT"] = np.stack([[pairT(rw(n)[l]) for n in ("s5_b_re", "s5_b_im")] for l in range(DEPTH)])
    cT = np.zeros((DEPTH, 2, 128, 16, 32), np.float32)
    for l in range(DEPTH):
        for x, n in enumerate(("s5_c_re", "s5_c_im")):
            c = rw(n)[l]
            for g in range(32):
                j, gl = g // 2, g % 2
                cT[l, x, gl * 64:(gl + 1) * 64, j, gl * 16:(gl + 1) * 16] = c[g].T
    sh["s5_cT"] = cT
    sh["s5_dT"] = np.stack([_fm(rw("s5_d")[l], 4) for l in range(DEPTH)])
    sh["s5_glu_w"] = np.ascontiguousarray(rw("s5_glu_w"))
    sh["s5_glu_bT"] = np.stack([_fm(rw("s5_glu_b")[l], 4) for l in range(DEPTH)])
    sh["rw_vecT"] = np.stack([[_fm(rw(n)[l].reshape(-1), 4) for n in ("rw_k_k", "rw_k_a", "rw_r_k", "rw_ln_g", "rw_ln_b")]
                              for l in range(DEPTH)])
    sh["ada_w"] = np.ascontiguousarray(inputs["ada_w"], dtype=np.float32)
    sh["ada_bT"] = np.stack([_fm(inputs["ada_b"][l], 48) for l in range(DEPTH)])
    sh["gmixT"] = np.stack([_fm(inputs["norm_mix_g"][l], 8) for l in range(DEPTH)])
    sh["gffnT"] = np.stack([_fm(inputs["norm_ffn_g"][l], 8) for l in range(DEPTH)])
    sh["w_in"] = np.ascontiguousarray(inputs["w_in"], dtype=np.float32)
    return sh


def core_inputs(inputs, core):
    b0 = core * NB
    x = np.asarray(inputs["x"][b0:b0 + NB], np.float32)
    ctx = np.asarray(inputs["ctx"][b0:b0 + NB], np.float32)
    h0 = np.concatenate([ctx.transpose(0, 2, 1), x.transpose(0, 2, 1)], axis=2)
    cm = {"h0": np.ascontiguousarray(h0)}
    cols = np.stack([inputs["c"][b0], inputs["c"][b0 + 1], inputs["c_ctx"]], axis=1).astype(np.float32)
    cm["cT"] = np.ascontiguousarray(cols.reshape(8, 128, 3).transpose(1, 0, 2))
    return cm


CH = 64
SEGC = 4
SEG = CH * SEGC


class CLA:
    MMDT = BF16

    def __init__(self, prog, delta, nvb, nbanks=8, nbuf=2):
        self.nbuf = nbuf
        dt = self.dt = CLA.MMDT
        self.pr = prog
        k = self.k = prog.k
        self.delta = delta
        self.nvb = nvb
        self.nop = 5 if delta else 3
        nv = nvb * 128
        self.nv = nv
        self.opin = [k.sb("cla_in%d" % i, [128, self.nop, SEG]) for i in range(nbuf)]
        self.vin = [k.sb("cla_vin%d" % i, [128, nvb, SEG]) for i in range(nbuf)]
        self.cum = k.sb("cla_cum", [128, SEG])
        self.tmp = k.sb("cla_tmp", [128, SEG])
        self.E = {n: k.sb("cla_E" + n, [128, SEG]) for n in (["p", "m", "l", "v"] if delta else ["p", "m", "l"])}
        self.WC = k.sb("cla_WC", [128, SEGC])
        self.mkt = [k.sb("cla_mkt%d" % i, [128, SEGC, CH]) for i in range(2)]
        self.mki = 0
        names = ["Rq", "Kd", "Kc"] + (["Kq", "Bd", "Bc"] if delta else [])
        self.bd = {n: k.sb("cla_bd" + n, [128, SEGC, 2, CH], dt) for n in names}
        for n in names:
            k.memset(self.bd[n], 0.0, x="pool")
        self.vpad = [k.sb("cla_vpad%d" % v, [128, SEGC, 2, CH]) for v in range(nvb)]
        for v in range(nvb):
            k.memset(self.vpad[v], 0.0, x="pool")
        nslot = 14 if delta else 5
        self.SL = [k.sb("cla_s%d" % s, [128, SEGC, 128], dt) for s in range(nslot)]
        self.slots = [[self.SL[s][:, c, :] for s in range(nslot)] for c in range(SEGC)]
        self.VBT = k.sb("cla_VB", [128, SEGC, nv], dt)
        self.OtT = k.sb("cla_Ot", [128, SEGC, nv], dt)
        self.HT = k.sb("cla_H", [128, SEGC, nv], dt)
        self.VB = [self.VBT[:, c, :] for c in range(SEGC)]
        self.Ot = [self.OtT[:, c, :] for c in range(SEGC)]
        self.H = [self.HT[:, c, :] for c in range(SEGC)]
        self.Tst = [k.sb("cla_T%d" % i, [128, nv]) for i in range(2)]
        self.Tsh = self.Tst if dt == F32 else [k.sb("cla_Tb%d" % i, [128, nv], dt) for i in range(2)]
        if dt == F32:
            self.Ib = prog.ident
        else:
            self.Ib = k.sb("cla_Ib", [128, 128], dt)
            k.cp(self.Ib, prog.ident, x="act")
        self.oseg = [k.sb("cla_oseg%d" % i, [128, nvb, SEG]) for i in range(2)]
        self.pst = [k.ps("cla_ps%d" % i, [128, 512]) for i in range(nbanks)]
        self.psn = nbanks
        self.pi = 0
        self.ei = 0

    def P(self):
        p = self.pst[self.pi % self.psn]
        self.pi += 1
        return p

    def evac(self, out, in_, neg=False):
        k = self.k
        if neg:
            k.act(out, in_, AF.Copy, scale=-1.0)
        elif self.ei % 3 == 0:
            k.cp(out, in_, x="dve")
        else:
            k.cp(out, in_, x="act")

    def run_gen(self, ops_dram, v_dram, o_dram):
        k = self.k
        pr = self.pr
        delta = self.delta
        nvb = self.nvb
        nv = self.nv
        I = pr.ident
        Ib = self.Ib
        f32 = (self.dt == F32)

        def trn(p, x):
            if f32:
                k.tr(p, x, I)
            else:
                k.mm(p, x, Ib)
        k.memset(self.Tst[0], 0.0)
        if not f32:
            k.memset(self.Tsh[0], 0.0)
        tcur = 0
        nseg = T // SEG
        for sg in range(nseg):
            s0 = sg * SEG
            oin = self.opin[sg % self.nbuf]
            vin = self.vin[sg % self.nbuf]
            k.dma("sp", oin, ops_dram[:, :, s0:s0 + SEG].rr("i p t -> p i t"))
            k.dma("pool", vin, v_dram[:, :, s0:s0 + SEG].rr("i p t -> p i t"))
            Rr, Kr, LW = oin[:, 0, :], oin[:, 1, :], oin[:, 2, :]
            k.scan(self.cum, pr.rmask, LW, 0.0)
            cum3 = self.cum.rr("p (c t) -> p c t", t=CH)
            cl = cum3[:, :, CH - 1:CH]
            k.act(self.E["p"], self.cum, AF.Exp)
            k.act(self.E["m"], self.cum, AF.Exp, scale=-1.0)
            k.tt(self.tmp.rr("p (c t) -> p c t", t=CH), cl.bc([128, SEGC, CH]), cum3, ALU.subtract, x="pool")
            k.act(self.E["l"], self.tmp, AF.Exp)
            k.act(self.WC.rr("p (c o) -> p c o", o=1), cl, AF.Exp)
            if delta:
                k.tt(self.tmp, self.cum, LW, ALU.subtract, x="pool")
                k.act(self.E["v"], self.tmp, AF.Exp)
            def mk(name, src, e):
                e3 = self.E[e].rr("p (c t) -> p c t", t=CH)
                s3 = src.rr("p (c t) -> p c t", t=CH)
                t3 = self.mkt[self.mki % 2]
                self.mki += 1
                k.tt(t3, s3, e3, ALU.mult, x="dve")
                k.cp(self.bd[name][0:64, :, 0, :], t3[0:64], x="act")
                k.cp(self.bd[name][64:128, :, 1, :], t3[64:128], x="act")
            mk("Rq", Rr, "p")
            mk("Kd", Kr, "m")
            mk("Kc", Kr, "l")
            if delta:
                KKr, Br = oin[:, 3, :], oin[:, 4, :]
                mk("Kq", KKr, "v")
                mk("Bd", Br, "m")
                mk("Bc", Br, "l")
            for v in range(nvb):
                v3 = vin[:, v, :].rr("p (c t) -> p c t", t=CH)
                if nvb == 1:
                    k.cp(self.vpad[0][0:64, :, 0, :], v3[0:64], x="act")
                    k.cp(self.vpad[0][64:128, :, 1, :], v3[64:128], x="act")
                else:
                    k.cp(self.vpad[v][:, :, v, :], v3, x="act")
            B = {n: [self.bd[n][:, c, :, :].rr("p h t -> p (h t)") for c in range(SEGC)] for n in self.bd}
            S = self.slots
            SL = self.SL
            CC = range(SEGC)

            def step(mmf, evall, w=128):
                rpb = 512 // w
                self.ei += 1
                regs = []
                banks = []
                for c in CC:
                    if c % rpb == 0:
                        bank = self.P()
                        banks.append((bank, c))
                    regs.append(bank[:, (c % rpb) * w:(c % rpb + 1) * w])
                for c in CC:
                    mmf(c, regs[c])
                yield
                for bank, c0 in banks:
                    ncb = min(rpb, SEGC - c0)
                    pv = bank[:, 0:ncb * w].rr("p (c x) -> p c x", x=w)
                    evall(pv, slice(c0, c0 + ncb), ncb)

            def m3(m, n_):
                return m[:, None, :].bc([128, n_, 128])
            bd3 = {n_: self.bd[n_].rr("p c h t -> p c (h t)") for n_ in self.bd}
            for v in range(nvb):
                yield from step(lambda c, p: k.tr(p, self.vpad[v][:, c, :, :].rr("p h t -> p (h t)"), I),
                                lambda pv, cs, n_: self.evac(self.VBT[:, cs, v * 128:(v + 1) * 128], pv))
            if delta:
                yield from step(lambda c, p: k.mm(p, B["Bd"][c], B["Kq"][c]),
                                lambda pv, cs, n_: k.stt(SL[1][:, cs], pv, -1.0, m3(pr.m_up, n_), ALU.mult, ALU.mult))
                yield from step(lambda c, p: k.mm(p, B["Kq"][c], B["Bd"][c]),
                                lambda pv, cs, n_: k.stt(SL[0][:, cs], pv, -1.0, m3(pr.m_lo, n_), ALU.mult, ALU.mult))
                yield from step(lambda c, p: k.mm(p, B["Kd"][c], B["Kq"][c]),
                                lambda pv, cs, n_: k.tt(SL[6][:, cs], pv, m3(pr.m_up, n_), ALU.mult))
                yield from step(lambda c, p: k.mm(p, B["Kd"][c], B["Rq"][c]),
                                lambda pv, cs, n_: k.tt(SL[7][:, cs], pv, m3(pr.m_upi, n_), ALU.mult))
                yield from step(lambda c, p: k.mm(p, B["Bd"][c], B["Rq"][c]),
                                lambda pv, cs, n_: k.stt(SL[8][:, cs], pv, -1.0, m3(pr.m_upi, n_), ALU.mult, ALU.mult))
                k.tt(SL[4], SL[1], m3(I, SEGC), ALU.add, x="dve")
                pc, pt, yc = 0, 1, 4
                for j in range(1, 6):
                    pn, ptn, yn = 2 - pc, 4 - pt, 9 - yc
                    yield from step(lambda c, p: k.mm(p, S[c][pt], S[c][pc]),
                                    lambda pv, cs, n_: self.evac(SL[pn][:, cs], pv))
                    if j < 5:
                        yield from step(lambda c, p: k.mm(p, S[c][pc], S[c][pt]),
                                        lambda pv, cs, n_: self.evac(SL[ptn][:, cs], pv))
                    yield from step(lambda c, p: k.mm(p, S[c][pn], S[c][yc]),
                                    lambda pv, cs, n_: k.tt(SL[yn][:, cs], pv, SL[yc][:, cs], ALU.add))
                    pc, pt, yc = pn, ptn, yn
                XT = yc
                yield from step(lambda c, p: trn(p, B["Kq"][c]), lambda pv, cs, n_: self.evac(SL[9][:, cs], pv))
                yield from step(lambda c, p: trn(p, B["Kc"][c]), lambda pv, cs, n_: self.evac(SL[10][:, cs], pv))
                yield from step(lambda c, p: trn(p, B["Bc"][c]), lambda pv, cs, n_: self.evac(SL[11][:, cs], pv, neg=True))
                free = [s_ for s_ in (0, 1, 2, 3, 4, 5) if s_ != XT]
                sAV, sUt, sKqt, sRt, sGT = free[0], free[1], free[2], free[3], free[4]
                yield from step(lambda c, p: k.mm(p, S[c][6], self.VB[c]), lambda pv, cs, n_: self.evac(SL[sAV][:, cs], pv))
                yield from step(lambda c, p: k.mm(p, S[c][XT], S[c][sAV]), lambda pv, cs, n_: self.evac(SL[sUt][:, cs], pv))
                yield from step(lambda c, p: k.mm(p, S[c][XT], S[c][9]), lambda pv, cs, n_: self.evac(SL[sKqt][:, cs], pv))
                yield from step(lambda c, p: k.mm(p, S[c][sKqt], S[c][8]),
                                lambda pv, cs, n_: k.tt(SL[sRt][:, cs], pv, bd3["Rq"][:, cs], ALU.add))

                def mm_ot(c, p):
                    k.mm(p, S[c][7], self.VB[c], start=True, stop=False)
                    k.mm(p, S[c][8], S[c][sUt], start=False, stop=True)
                yield from step(mm_ot, lambda pv, cs, n_: self.evac(self.OtT[:, cs], pv))

                def mm_h(c, p):
                    k.mm(p, S[c][10], self.VB[c], start=True, stop=False)
                    k.mm(p, S[c][11], S[c][sUt], start=False, stop=True)
                yield from step(mm_h, lambda pv, cs, n_: self.evac(self.HT[:, cs], pv))
                yield from step(lambda c, p: k.mm(p, S[c][sKqt], S[c][11]),
                                lambda pv, cs, n_: self.evac(SL[sGT][:, cs], pv))
                RtT = [S[c][sRt] for c in CC]
                GT = [S[c][sGT] for c in CC]
            else:
                yield from step(lambda c, p: k.mm(p, B["Kd"][c], B["Rq"][c]),
                                lambda pv, cs, n_: k.tt(SL[0][:, cs], pv, m3(pr.m_upi, n_), ALU.mult))
                yield from step(lambda c, p: trn(p, B["Kc"][c]), lambda pv, cs, n_: self.evac(SL[1][:, cs], pv))
                yield from step(lambda c, p: k.mm(p, S[c][0], self.VB[c]), lambda pv, cs, n_: self.evac(self.OtT[:, cs], pv), w=nv)
                yield from step(lambda c, p: k.mm(p, S[c][1], self.VB[c]), lambda pv, cs, n_: self.evac(self.HT[:, cs], pv), w=nv)
                RtT = [B["Rq"][c] for c in CC]
            osg = self.oseg[sg % 2]
            for c in CC:
                Tc = self.Tst[tcur]
                Tn = self.Tst[1 - tcur]
                Tcb = self.Tsh[tcur]
                Tnb = self.Tsh[1 - tcur]
                for v in range(nvb):
                    p = self.P()[:, 0:128]
                    k.mm(p, Tcb[:, v * 128:(v + 1) * 128], RtT[c], start=True, stop=False)
                    k.mm(p, self.Ot[c][:, v * 128:(v + 1) * 128], Ib, start=False, stop=True)
                    yield
                    if nvb == 1:
                        k.cp(osg[0:64, 0, c * CH:(c + 1) * CH], p[0:64, 0:64], x="dve")
                        k.cp(osg[64:128, 0, c * CH:(c + 1) * CH], p[64:128, 64:128], x="dve")
                    else:
                        k.cp(osg[:, v, c * CH:(c + 1) * CH], p[:, v * CH:(v + 1) * CH], x="act")
                if delta:
                    p = self.P()[:, 0:nv]
                    k.mm(p, GT[c], Tcb, start=True, stop=False)
                    k.mm(p, Ib, self.H[c], start=False, stop=True)
                    yield
                    k.stt(Tn, Tc, self.WC[:, c:c + 1], p, ALU.mult, ALU.add)
                else:
                    k.stt(Tn, Tc, self.WC[:, c:c + 1], self.H[c], ALU.mult, ALU.add)
                if not f32:
                    k.cp(Tnb, Tn, x="act")
                tcur = 1 - tcur
            k.dma("sp", o_dram[:, :, s0:s0 + SEG].rr("i p t -> p i t"), osg)


    def run(self, ops_dram, v_dram, o_dram):
        for _ in self.run_gen(ops_dram, v_dram, o_dram):
            pass


def cla_run_many(clas, jobs):
    pending = list(jobs)
    active = [None] * len(clas)
    while pending or any(a is not None for a in active):
        for i, cla in enumerate(clas):
            if active[i] is None and pending:
                active[i] = cla.run_gen(*pending.pop(0))
            if active[i] is not None:
                try:
                    next(active[i])
                except StopIteration:
                    active[i] = None


RW_DSCALE = 0.606531
RW_GN_EPS = 64e-5


def nat_tiles():
    tl = [(0, TC, 0, TC)]
    for i in range(TL // 512):
        tl.append((TC + i * 512, 512, TC, T))
    return tl


def rwkv_declare(pr):
    pr.inp("rw_muT", [DEPTH, 2, 128, 15])
    pr.inp("rw_w0T", [DEPTH, 2, 128, 4])
    pr.inp("rw_a0T", [DEPTH, 2, 128, 4])
    pr.inp("rw_w_up", [DEPTH, 128, 512])
    pr.inp("rw_a_up", [DEPTH, 128, 512])
    pr.inp("rw_g_up", [DEPTH, 128, 512])
    pr.inp("rw_vecT", [DEPTH, 5, 128, 4])
    pr.scratch("RWop", [NB, 2, 4, 5, 128, T])
    pr.scratch("RWv", [NB, 2, 4, 1, 128, T])
    pr.scratch("RWo", [NB, 2, 4, 1, 128, T])
    pr.scratch("RWg", [NB, 4, 128, T])
    pr.scratch("RWcb", [NB, 4, 128, T])


def rwkv_prep(pr, l):
    k = pr.k
    k.push()
    IN = pr.IN
    mu = [k.sb("rw_mu%d" % i, [128, 15]) for i in range(2)]
    c0 = k.sb("rw_c0", [128, 15])
    for i in range(2):
        k.dma("sp", mu[i], IN["rw_muT"][l, i])
    k.tt(c0, mu[0], mu[1], ALU.add)
    k.ts(c0, c0, -1.0, ALU.mult, 1.0, ALU.add)
    w0 = k.sb("rw_w0", [128, 2, 4])
    a0 = k.sb("rw_a0", [128, 2, 4])
    k.dma("sp", w0, IN["rw_w0T"][l].rr("d p c -> p d c"))
    k.dma("sp", a0, IN["rw_a0T"][l].rr("d p c -> p d c"))
    wup = k.sb("rw_wup", [128, 512])
    aup = k.sb("rw_aup", [128, 512])
    gup = k.sb("rw_gup", [128, 512])
    k.dma("sp", wup, IN["rw_w_up"][l])
    k.dma("sp", aup, IN["rw_a_up"][l])
    k.dma("sp", gup, IN["rw_g_up"][l])
    vec = k.sb("rw_vec", [128, 5, 4])
    k.dma("sp", vec, IN["rw_vecT"][l].rr("i p c -> p i c"))
    omka = k.sb("rw_omka", [128, 4])
    k.ts(omka, vec[:, 1, :], -1.0, ALU.mult, 1.0, ALU.add)
    raw = [k.sb("rw_raw%d" % i, [128, 15, 514]) for i in range(1)]
    sh = k.sb("rw_sh", [128, 15, 512])
    t15 = k.sb("rw_t15", [128, 15, 512])
    TW = k.sb("rw_TW", [128, 512])
    SG = k.sb("rw_SG", [128, 512])
    kkr = k.sb("rw_kkr", [128, 512])
    sq = k.sb("rw_sq", [128, 512])
    nrm = k.sb("rw_nrm", [128, 512])
    kk = k.sb("rw_kk", [128, 512])
    NO = 3
    ost = [[k.sb("rw_o%d_%d" % (j, i), [128, 512]) for i in range(7)] for j in range(NO)]
    A = k.sb("rw_A", [128, 512])
    tA = k.sb("rw_tA", [128, 512])
    sgw = k.sb("rw_sgw", [128, 512])
    cbin = k.sb("rw_cbin", [128, 512])
    pss = [k.ps("rw_ps%d" % i, [128, 512]) for i in range(5)]
    pcb = k.ps("rw_pcb", [128, 512])
    P = pr.S["P"]
    it = 0
    oi = 0
    pi = 0
    for b in range(NB):
        for (t0, n, ra, rb) in nat_tiles():
            rw = raw[0]
            it += 1
            lo = max(ra, t0 - 1)
            hi = min(rb, t0 + n + 1)
            if lo == t0:
                k.memset(rw[:, :, 0:1], 0.0, x="pool")
            if hi == t0 + n:
                k.memset(rw[:, :, n + 1:n + 2], 0.0, x="pool")
            k.dma("sp", rw[:, :, lo - (t0 - 1):hi - (t0 - 1)],
                  P[b, OFF_RW:OFF_RW + 1920, lo:hi].rr("(j p) t -> p j t", p=128))
            k.tt(sh[:, :, :n], rw[:, :, 1:n + 1], c0[:, :, None].bc([128, 15, n]), ALU.mult, x="dve")
            for jj in range(15):
                k.act(t15[:, jj, :n], rw[:, jj, 0:n], AF.Identity, scale=mu[0][:, jj:jj + 1])
            k.tt(sh[:, :, :n], sh[:, :, :n], t15[:, :, :n], ALU.add, x="dve")
            for jj in range(15):
                k.act(t15[:, jj, :n], rw[:, jj, 2:n + 2], AF.Identity, scale=mu[1][:, jj:jj + 1])
            k.tt(sh[:, :, :n], sh[:, :, :n], t15[:, :, :n], ALU.add, x="dve")
            k.act(TW[:, :n], sh[:, 12, :n], AF.Tanh)
            k.act(SG[:, :n], sh[:, 14, :n], AF.Sigmoid)
            AD = sh[:, 13, :]
            m0 = ra + rb - t0 - n
            for p in range(4):
                rS, kS, vS = sh[:, p, :], sh[:, 4 + p, :], sh[:, 8 + p, :]
                cs = slice(p * 128, (p + 1) * 128)
                k.act(kkr[:, :n], kS[:, :n], AF.Identity, scale=vec[:, 0, p:p + 1])
                k.act(sq[:, :n], kkr[:, :n], AF.Square)
                ps = pss[pi % 5]
                pi += 1
                k.mm(ps[:, :n], pr.ones_bd, sq[:, :n])
                k.act(nrm[:, :n], ps[:, :n], AF.Sqrt)
                k.ts(nrm[:, :n], nrm[:, :n], 1e-12, ALU.max)
                k.recip(nrm[:, :n], nrm[:, :n])
                k.tt(kk[:, :n], kkr[:, :n], nrm[:, :n], ALU.mult)
                ps = pss[pi % 5]
                pi += 1
                k.mm(ps[:, :n], gup[:, cs], SG[:, :n])
                o = ost[oi % NO]
                oi += 1
                k.cp(o[6][:, :n], ps[:, :n], x="act")
                k.dma("pool", pr.S["RWg"][b, p, :, t0:t0 + n], o[6][:, :n])
                for d in range(2):
                    ds = slice(d * 64, (d + 1) * 64)
                    if d == 1:
                        o = ost[oi % NO]
                        oi += 1

                    def ov(tile):
                        return tile[:, :n] if d == 0 else tile[:, :n][:, ::-1]
                    ps = pss[pi % 5]
                    pi += 1
                    k.mm(ps[:, :n], wup[ds, cs], TW[ds, :n])
                    k.act(sgw[:, :n], ps[:, :n], AF.Sigmoid, bias=w0[:, d, p:p + 1], scale=1.0)
                    k.ts(ov(o[2]), sgw[:, :n], -RW_DSCALE, ALU.mult)
                    ps = pss[pi % 5]
                    pi += 1
                    k.mm(ps[:, :n], aup[ds, cs], AD[ds, :n])
                    k.act(A[:, :n], ps[:, :n], AF.Sigmoid, bias=a0[:, d, p:p + 1], scale=1.0)
                    k.ts(tA[:, :n], A[:, :n], vec[:, 1, p:p + 1], ALU.mult, omka[:, p:p + 1], ALU.add)
                    k.tt(ov(o[1]), kS[:, :n], tA[:, :n], ALU.mult)
                    k.tt(ov(o[4]), A[:, :n], kk[:, :n], ALU.mult, x="dve")
                    k.stt(cbin[:, :n], rS[:, :n], vec[:, 2, p:p + 1], o[1][:, :n] if d == 0 else o[1][:, :n][:, ::-1],
                          ALU.mult, ALU.mult)
                    k.mm(pcb[:, :n], pr.ones_bd, cbin[:, :n], start=(d == 0), stop=(d == 1))
                    k.cp(ov(o[0]), rS[:, :n], x="act")
                    k.cp(ov(o[3]), kk[:, :n], x="act")
                    k.cp(ov(o[5]), vS[:, :n], x="act")
                    s0 = t0 if d == 0 else m0
                    for i in range(5):
                        k.dma("sp" if i % 2 == 0 else "pool", pr.S["RWop"][b, d, p, i, :, s0:s0 + n], o[i][:, :n])
                    k.dma("sp", pr.S["RWv"][b, d, p, 0, :, s0:s0 + n], o[5][:, :n])
                k.cp(cbin[:, :n], pcb[:, :n], x="dve")
                k.dma("pool", pr.S["RWcb"][b, p, :, t0:t0 + n], cbin[:, :n])
    k.pop()


def rwkv_scan(pr, l):
    k = pr.k
    k.push()
    clas = [CLA(pr, True, 1, nbanks=4) for _ in range(2)]
    jobs = [(pr.S["RWop"][b, d, p], pr.S["RWv"][b, d, p], pr.S["RWo"][b, d, p])
            for b in range(NB) for d in range(2) for p in range(4)]
    cla_run_many(clas, jobs)
    k.pop()


def rwkv_s5_scan(pr, l):
    k = pr.k
    k.push()
    clas = [CLA(pr, True, 1, nbanks=3, nbuf=1)]
    jobs = [(pr.S["RWop"][b, d, p], pr.S["RWv"][b, d, p], pr.S["RWo"][b, d, p])
            for b in range(NB) for d in range(2) for p in range(4)]
    s5g = s5_scan_gen(pr, l, npy=1, nwork=1)
    pending = list(jobs)
    active = None
    s5_alive = True
    n = 0
    while pending or active is not None or s5_alive:
        if active is None and pending:
            active = clas[0].run_gen(*pending.pop(0))
        if active is not None:
            try:
                next(active)
            except StopIteration:
                active = None
        n += 1
        if s5_alive and (n % 3 == 0 or (active is None and not pending)):
            try:
                next(s5g)
            except StopIteration:
                s5_alive = False
    k.pop()


def rwkv_readout(pr, l, ybr):
    k = pr.k
    k.push()
    vec = k.sb("rwr_vec", [128, 5, 4])
    k.dma("sp", vec, pr.IN["rw_vecT"][l].rr("i p c -> p i c"))
    NBUF = 2
    o0 = [k.sb("rwr_o0_%d" % i, [128, 512]) for i in range(NBUF)]
    o1 = [k.sb("rwr_o1_%d" % i, [128, 512]) for i in range(NBUF)]
    vv = [k.sb("rwr_v_%d" % i, [128, 512]) for i in range(NBUF)]
    gg = [k.sb("rwr_g_%d" % i, [128, 512]) for i in range(NBUF)]
    cb = [k.sb("rwr_cb_%d" % i, [128, 512]) for i in range(NBUF)]
    yo = [k.sb("rwr_y_%d" % i, [128, 512]) for i in range(NBUF)]
    sq = k.sb("rwr_sq", [128, 512])
    mean = k.sb("rwr_mean", [128, 512])
    var = k.sb("rwr_var", [128, 512])
    t1 = k.sb("rwr_t1", [128, 512])
    ps1 = k.ps("rwr_ps1", [128, 512])
    ps2 = k.ps("rwr_ps2", [128, 512])
    it = 0
    for b in range(NB):
        for p in range(4):
            for (t0, n, ra, rb) in nat_tiles():
                i = it % NBUF
                it += 1
                m0 = ra + rb - t0 - n
                k.dma("sp", o0[i][:, :n], pr.S["RWo"][b, 0, p, 0, :, t0:t0 + n])
                k.dma("pool", o1[i][:, :n], pr.S["RWo"][b, 1, p, 0, :, m0:m0 + n])
                k.dma("sp", vv[i][:, :n], pr.S["RWv"][b, 0, p, 0, :, t0:t0 + n])
                k.dma("pool", gg[i][:, :n], pr.S["RWg"][b, p, :, t0:t0 + n])
                k.dma("sp", cb[i][:, :n], pr.S["RWcb"][b, p, :, t0:t0 + n])
                o = o0[i]
                k.tt(o[:, :n], o[:, :n], o1[i][:, :n][:, ::-1], ALU.add)
                k.mm(ps1[:, :n], pr.ones_bd, o[:, :n])
                k.act(sq[:, :n], o[:, :n], AF.Square)
                k.mm(ps2[:, :n], pr.ones_bd, sq[:, :n])
                k.ts(mean[:, :n], ps1[:, :n], 1.0 / 64, ALU.mult)
                k.tt(t1[:, :n], mean[:, :n], mean[:, :n], ALU.mult, x="pool")
                k.stt(var[:, :n], ps2[:, :n], 1.0 / 64, t1[:, :n], ALU.mult, ALU.subtract)
                k.ts(var[:, :n], var[:, :n], RW_GN_EPS, ALU.add)
                k.act(var[:, :n], var[:, :n], AF.Sqrt)
                k.recip(var[:, :n], var[:, :n])
                k.tt(t1[:, :n], o[:, :n], mean[:, :n], ALU.subtract, x="pool")
                k.stt(t1[:, :n], t1[:, :n], vec[:, 3, p:p + 1], var[:, :n], ALU.mult, ALU.mult)
                k.tt(sq[:, :n], cb[i][:, :n], vv[i][:, :n], ALU.mult, x="pool")
                k.stt(t1[:, :n], t1[:, :n], vec[:, 4, p:p + 1], sq[:, :n], ALU.add, ALU.add)
                k.tt(yo[i][:, :n], t1[:, :n], gg[i][:, :n], ALU.mult)
                k.dma("sp", ybr[b, p * 128:(p + 1) * 128, t0:t0 + n], yo[i][:, :n])
    k.pop()


def col_tiles():
    return [(0, 512), (512, 512), (1024, 512), (1536, 512), (2048, 256)]


def seq_copy(k, dst, src, colmajor, rev, x, scale=None):
    def emit(o, i):
        if scale is not None:
            k.act(o, i, AF.Copy, scale=scale)
        else:
            k.cp(o, i, x=x)
    c_src = src[:, 0:TC]
    emit(dst[:, 0:TC], c_src[:, ::-1] if rev else c_src)
    if colmajor:
        l_src = src[:, TC:T].rr("p (r c) -> p c r", c=64)
        if rev:
            l_src = l_src[:, ::-1, ::-1]
        emit(dst[:, TC:T].rr("p (c r) -> p c r", r=32), l_src)
    else:
        l_src = src[:, TC:T]
        emit(dst[:, TC:T], l_src[:, ::-1] if rev else l_src)


def gla_declare(pr):
    pr.inp("gla_gate_up", [DEPTH, 2, 16, 256])
    pr.inp("gla_gate_bT", [DEPTH, 2, 128, 2])
    pr.inp("gla_norm_gT", [DEPTH, 128, 1])
    pr.scratch("GLop", [NB, 2, 2, 3, 128, T])
    pr.scratch("GLv", [NB, 2, 2, 2, 128, T])
    pr.scratch("GLo", [NB, 2, 2, 2, 128, T])


def gla_prep(pr, l):
    k = pr.k
    k.push()
    IN = pr.IN
    P = pr.S["P"]
    gup = [k.sb("gl_gup%d" % d, [16, 256]) for d in range(2)]
    nb = k.sb("gl_nb", [128, 2, 2])
    for d in range(2):
        k.dma("sp", gup[d], IN["gla_gate_up"][l, d])
    k.dma("sp", nb, IN["gla_gate_bT"][l].rr("d p c -> p d c"))
    k.ts(nb, nb, -1.0, ALU.mult)
    gd = [k.sb("gl_gd%d" % d, [16, T]) for d in range(2)]
    nat = [k.sb("gl_nat%d" % i, [128, T]) for i in range(2)]
    sq_ = [k.sb("gl_seq%d" % i, [128, T]) for i in range(3)]
    lwn = k.sb("gl_lwn", [128, T])
    ps = [k.ps("gl_ps%d" % i, [128, 512]) for i in range(2)]
    ni = 0
    si = 0
    for b in range(NB):
        for d in range(2):
            k.dma("sp", gd[d], P[b, OFF_GLA + 1536 + d * 16:OFF_GLA + 1536 + (d + 1) * 16, :])
        for p in range(2):
            for i, (row0, scale) in enumerate(((p * 128, 0.125), (256 + p * 128, None))):
                nt = nat[ni % 2]
                ni += 1
                k.dma("sp", nt, P[b, OFF_GLA + row0:OFF_GLA + row0 + 128, :])
                for d in range(2):
                    st = sq_[si % 3]
                    si += 1
                    seq_copy(k, st, nt, True, d == 1, "act", scale=scale)
                    k.dma("pool", pr.S["GLop"][b, d, p, i], st)
            for d in range(2):
                for j, (c0, n) in enumerate(col_tiles()):
                    pp = ps[j % 2]
                    k.mm(pp[:, :n], gup[d][:, p * 128:(p + 1) * 128], gd[d][:, c0:c0 + n])
                    k.act(lwn[:, c0:c0 + n], pp[:, :n], AF.Exp, bias=nb[:, d, p:p + 1], scale=-1.0)
                k.act(lwn, lwn, AF.Ln, bias=1.0, scale=1.0)
                st = sq_[si % 3]
                si += 1
                seq_copy(k, st, lwn, True, d == 1, "pool", scale=-1.0 / 16.0)
                k.dma("pool", pr.S["GLop"][b, d, p, 2], st)
            for vb in range(2):
                h = 2 * p + vb
                nt = nat[ni % 2]
                ni += 1
                k.dma("sp", nt, P[b, OFF_GLA + 512 + h * 128:OFF_GLA + 512 + (h + 1) * 128, :])
                for d in range(2):
                    st = sq_[si % 3]
                    si += 1
                    seq_copy(k, st, nt, True, d == 1, "act" if d == 0 else "dve")
                    k.dma("pool", pr.S["GLv"][b, d, p, vb], st)
    k.pop()


def gla_scan(pr, l):
    k = pr.k
    k.push()
    clas = [CLA(pr, False, 2, nbanks=4) for _ in range(2)]
    jobs = [(pr.S["GLop"][b, d, p], pr.S["GLv"][b, d, p], pr.S["GLo"][b, d, p])
            for b in range(NB) for d in range(2) for p in range(2)]
    cla_run_many(clas, jobs)
    k.pop()


def gla_readout(pr, l, ybr):
    k = pr.k
    k.push()
    P = pr.S["P"]
    ng = k.sb("glr_ng", [128, 1])
    k.dma("sp", ng, pr.IN["gla_norm_gT"][l])
    o0 = k.sb("glr_o0", [128, T])
    o1 = k.sb("glr_o1", [128, T])
    gn = k.sb("glr_gn", [128, T])
    gs = k.sb("glr_gs", [128, T])
    sq = k.sb("glr_sq", [128, T])
    rs = k.sb("glr_rs", [128, T])
    yn = k.sb("glr_yn", [128, T])
    ps = [k.ps("glr_ps%d" % i, [128, 512]) for i in range(2)]
    for b in range(NB):
        for h in range(4):
            p, vb = h // 2, h % 2
            k.dma("sp", o0, pr.S["GLo"][b, 0, p, vb])
            k.dma("pool", o1, pr.S["GLo"][b, 1, p, vb])
            k.dma("sp", gn, P[b, OFF_GLA + 1024 + h * 128:OFF_GLA + 1024 + (h + 1) * 128, :])
            k.tt(o0[:, 0:TC], o0[:, 0:TC], o1[:, 0:TC][:, ::-1], ALU.add)
            k.tt(o0[:, TC:T], o0[:, TC:T], o1[:, TC:T][:, ::-1], ALU.add)
            k.act(sq, o0, AF.Square)
            for j, (c0, n) in enumerate(col_tiles()):
                pp = ps[j % 2]
                k.mm(pp[:, :n], pr.ones, sq[:, c0:c0 + n])
                k.ts(rs[:, c0:c0 + n], pp[:, :n], 1.0 / 128, ALU.mult, RMS_EPS, ALU.add)
            k.act(rs, rs, AF.Sqrt)
            k.recip(rs, rs)
            k.stt(o0, o0, ng[:, 0:1], rs, ALU.mult, ALU.mult)
            k.act(gn, gn, AF.Silu)
            seq_copy(k, gs, gn, True, False, "pool")
            k.tt(o0, o0, gs, ALU.mult)
            k.cp(yn[:, 0:TC], o0[:, 0:TC], x="act")
            k.cp(yn[:, TC:T].rr("p (r c) -> p c r", c=64), o0[:, TC:T].rr("p (c r) -> p c r", r=32), x="pool")
            k.dma("sp", ybr[b, h * 128:(h + 1) * 128, :], yn)
    k.pop()


MAGIC = 12582912.0
TWO_PI_LO = 6.2831850


def sincos_turns(k, out_s, out_c, turns, t1, t2):
    for (dst, off) in ((out_s, 0.0), (out_c, 0.25)):
        if off != 0.0:
            k.ts(t2, turns, off, ALU.add)
            src = t2
        else:
            src = turns
        k.ts(t1, src, MAGIC, ALU.add)
        k.ts(t1, t1, -MAGIC, ALU.add)
        k.tt(t1, src, t1, ALU.subtract)
        k.ts(dst, t1, 0.5, ALU.is_gt)
        k.tt(t1, t1, dst, ALU.subtract)
        k.ts(dst, t1, -0.5, ALU.is_lt)
        k.tt(t1, t1, dst, ALU.add)
        k.act(dst, t1, AF.Sin, scale=TWO_PI_LO)


def s5_declare(pr):
    pr.inp("tau", [128, 513])
    pr.inp("s5_aT", [DEPTH, 2, 2, 128, 16])
    pr.inp("s5_ldtT", [DEPTH, 2, 128, 16])
    pr.inp("s5_bT", [DEPTH, 2, 128, 16, 16])
    pr.inp("s5_cT", [DEPTH, 2, 128, 16, 32])
    pr.inp("s5_dT", [DEPTH, 128, 4])
    pr.inp("s5_glu_w", [DEPTH, 512, 512])
    pr.inp("s5_glu_bT", [DEPTH, 128, 4])
    pr.scratch("S5y", [NB, MIXW, T])


def s5_scan(pr, l):
    k = pr.k
    k.push()
    for _ in s5_scan_gen(pr, l):
        pass
    k.pop()


def s5_scan_gen(pr, l, npy=2, nwork=2):
    k = pr.k
    IN = pr.IN
    P = pr.S["P"]
    I = pr.ident
    tau = k.sb("s5_tau", [128, 513])
    k.dma("sp", tau, IN["tau"])
    cbd32 = [k.sb("s5_c32_%d" % x, [128, 16, 32]) for x in range(2)]
    cbd = [k.sb("s5_c%d" % x, [128, 16, 32], BF16) for x in range(2)]
    for x in range(2):
        k.dma("sp", cbd32[x], IN["s5_cT"][l, x])
    k.cp(cbd[0], cbd32[0], x="act")
    k.ts(cbd[1], cbd32[1], -1.0, ALU.mult)
    bt = [k.sb("s5_b%d" % x, [128, 16, 16]) for x in range(2)]
    for x in range(2):
        k.dma("sp", bt[x], IN["s5_bT"][l, x])
    names = ["are", "aim", "dt", "mag", "tht", "s", "c", "nr", "ni", "den", "fre", "fim", "t1", "t2", "t3"]
    drv = [[k.sb("s5_drv%d%d" % (d, x), [32, 16, 128], BF16) for x in range(2)] for d in range(2)]
    rho = [k.sb("s5_rho%d" % d, [128, 16]) for d in range(2)]
    tht = [k.sb("s5_tht%d" % d, [128, 16]) for d in range(2)]
    cth = [k.sb("s5_cth%d" % d, [128, 16]) for d in range(2)]
    sth = [k.sb("s5_sth%d" % d, [128, 16]) for d in range(2)]
    pAs = [k.ps("s5_pA%d" % q, [128, 512]) for q in range(2)]
    pBs = [k.ps("s5_pB%d" % q, [128, 512]) for q in range(2)]
    pYs = [k.ps("s5_pY%d" % q, [128, 512]) for q in range(npy)]
    pst = pYs[0]
    fbd = [k.sb("s5_fbd%d" % x, [128, 16, 2, 16]) for x in range(2)]
    for x in range(2):
        k.memset(fbd[x], 0.0)
    ft = [k.sb("s5_ft%d" % x, [128, 16, 16]) for x in range(2)]
    ftmp = k.sb("s5_ftmp", [128, 16, 16])
    w = {n: k.sb("s5w_" + n, [128, 16]) for n in names}
    for d in range(2):
        k.dma("sp", w["are"], IN["s5_aT"][l, d, 0])
        k.dma("sp", w["aim"], IN["s5_aT"][l, d, 1])
        k.dma("sp", w["dt"], IN["s5_ldtT"][l, d])
        k.act(w["dt"], w["dt"], AF.Exp)
        k.tt(w["t1"], w["are"], w["dt"], ALU.mult)
        k.act(rho[d], w["t1"], AF.Exp)
        k.tt(w["t1"], w["aim"], w["dt"], ALU.mult)
        k.ts(tht[d], w["t1"], 1.0 / (2.0 * math.pi), ALU.mult)
        sincos_turns(k, sth[d], cth[d], tht[d], w["t2"], w["t3"])
        k.tt(w["nr"], rho[d], cth[d], ALU.mult)
        k.ts(w["nr"], w["nr"], -1.0, ALU.add)
        k.tt(w["ni"], rho[d], sth[d], ALU.mult)
        k.tt(w["den"], w["are"], w["are"], ALU.mult)
        k.tt(w["t1"], w["aim"], w["aim"], ALU.mult)
        k.tt(w["den"], w["den"], w["t1"], ALU.add)
        k.recip(w["den"], w["den"])
        k.tt(w["fre"], w["nr"], w["are"], ALU.mult)
        k.tt(w["t1"], w["ni"], w["aim"], ALU.mult)
        k.tt(w["fre"], w["fre"], w["t1"], ALU.add)
        k.tt(w["fre"], w["fre"], w["den"], ALU.mult)
        k.tt(w["fim"], w["ni"], w["are"], ALU.mult)
        k.tt(w["t1"], w["nr"], w["aim"], ALU.mult)
        k.tt(w["fim"], w["fim"], w["t1"], ALU.subtract)
        k.tt(w["fim"], w["fim"], w["den"], ALU.mult)
        fre3 = w["fre"][:, :, None].bc([128, 16, 16])
        fim3 = w["fim"][:, :, None].bc([128, 16, 16])
        k.tt(ft[0], bt[0], fre3, ALU.mult)
        k.tt(ftmp, bt[1], fim3, ALU.mult)
        k.tt(ft[0], ft[0], ftmp, ALU.subtract)
        k.tt(ft[1], bt[1], fre3, ALU.mult)
        k.tt(ftmp, bt[0], fim3, ALU.mult)
        k.tt(ft[1], ft[1], ftmp, ALU.add)
        for x in range(2):
            k.cp(fbd[x][0:64, :, 0, :], ft[x][0:64], x="dve")
            k.cp(fbd[x][64:128, :, 1, :], ft[x][64:128], x="dve")
            for j0 in range(0, 16, 4):
                for jj in range(4):
                    j = j0 + jj
                    k.tr(pst[0:32, jj * 128:(jj + 1) * 128], fbd[x][:, j, :, :].rr("p a b -> p (a b)"), I)
                k.cp(drv[d][x][:, j0:j0 + 4, :], pst[0:32, :].rr("p (a b) -> p a b", a=4), x="act")
        yield
    Ct = k.sb("s5_Ct", [128, 513])
    St = k.sb("s5_St", [128, 513])
    turns = k.sb("s5_turns", [128, 513])
    ta = k.sb("s5_ta", [128, 513])
    tb = k.sb("s5_tb", [128, 513])
    un32 = [k.sb("s5_un32_%d" % b, [32, T]) for b in range(NB)]
    un = [k.sb("s5_un%d" % b, [32, T], BF16) for b in range(NB)]
    ur = [k.sb("s5_ur%d" % b, [32, T], BF16) for b in range(NB)]
    yacc = [k.sb("s5_yacc%d" % b, [32, T]) for b in range(NB)]
    W = []
    for q in range(nwork):
        W.append({n_: k.sb("s5_%s%d" % (n_, q), [128, 512], BF16 if n_ in ("hr", "hi") else F32)
                  for n_ in ("m1", "m2", "m3", "m4", "xmr", "xmi", "hr", "hi")})
    ini = k.sb("s5_ini", [128, 2])
    it1 = k.sb("s5_it1", [128, 2])
    G2 = [(k.sb("s5_gr%d" % q, [128, 512]), k.sb("s5_gi%d" % q, [128, 512])) for q in range(2)]
    ytmp = k.sb("s5_ytmp", [32, 512])
    tcount = 0
    for j in range(16):
        for b in range(NB):
            k.dma("sp", un32[b], P[b, OFF_S5 + j * 32:OFF_S5 + (j + 1) * 32, :])
            k.cp(un[b], un32[b], x="act")
            seq_copy(k, ur[b], un32[b], False, True, "act")
        for d in range(2):
            k.ts(turns, tau, tht[d][:, j:j + 1], ALU.mult)
            sincos_turns(k, St, Ct, turns, ta, tb)
            yield
            for b in range(NB):
                u = un[b] if d == 0 else ur[b]
                for ti, (s0, n, _ra, _rb) in enumerate(nat_tiles()):
                    q = tcount % 2
                    tcount += 1
                    w_ = W[q % nwork]
                    pA, pB, pY = pAs[q], pBs[q], pYs[q % npy]
                    m1, m2, m3, m4, xmr, xmi, hr, hi = (w_[n_] for n_ in ("m1", "m2", "m3", "m4", "xmr", "xmi", "hr", "hi"))
                    gr, gi = G2[q]
                    k.mm(pA[:, :n], drv[d][0][:, j, :], u[:, s0:s0 + n])
                    k.mm(pB[:, :n], drv[d][1][:, j, :], u[:, s0:s0 + n])
                    k.tt(m1[:, :n], pA[:, :n], Ct[:, :n], ALU.mult, x="dve")
                    k.tt(m2[:, :n], pB[:, :n], St[:, :n], ALU.mult, x="dve")
                    k.tt(xmr[:, :n], m1[:, :n], m2[:, :n], ALU.add, x="dve")
                    k.tt(m3[:, :n], pB[:, :n], Ct[:, :n], ALU.mult, x="dve")
                    k.tt(m4[:, :n], pA[:, :n], St[:, :n], ALU.mult, x="dve")
                    k.tt(xmi[:, :n], m3[:, :n], m4[:, :n], ALU.subtract, x="dve")
                    yield
                    if ti == 0:
                        k.memset(ini, 0.0)
                    else:
                        grp, gip = G2[1 - q]
                        cc, ss = Ct[:, pn:pn + 1], St[:, pn:pn + 1]
                        k.ts(it1[:, 0:1], gip[:, pn - 1:pn], ss, ALU.mult)
                        k.stt(ini[:, 0:1], grp[:, pn - 1:pn], cc, it1[:, 0:1], ALU.mult, ALU.subtract)
                        k.ts(it1[:, 1:2], gip[:, pn - 1:pn], cc, ALU.mult)
                        k.stt(ini[:, 1:2], grp[:, pn - 1:pn], ss, it1[:, 1:2], ALU.mult, ALU.add)
                    k.scan(gr[:, :n], rho[d][:, j:j + 1].bc([128, n]), xmr[:, :n], ini[:, 0:1])
                    k.scan(gi[:, :n], rho[d][:, j:j + 1].bc([128, n]), xmi[:, :n], ini[:, 1:2])
                    yield
                    k.tt(m1[:, :n], gr[:, :n], Ct[:, :n], ALU.mult, x="dve")
                    k.tt(m2[:, :n], gi[:, :n], St[:, :n], ALU.mult, x="dve")
                    k.tt(hr[:, :n], m1[:, :n], m2[:, :n], ALU.subtract, x="dve")
                    k.tt(m3[:, :n], gr[:, :n], St[:, :n], ALU.mult, x="dve")
                    k.tt(m4[:, :n], gi[:, :n], Ct[:, :n], ALU.mult, x="dve")
                    k.tt(hi[:, :n], m3[:, :n], m4[:, :n], ALU.add, x="dve")
                    yield
                    pn = n
                    k.mm(pY[0:32, :n], cbd[0][:, j, :], hr[:, :n], start=True, stop=False)
                    k.mm(pY[0:32, :n], cbd[1][:, j, :], hi[:, :n], start=False, stop=True)
                    if d == 0:
                        k.cp(yacc[b][:, s0:s0 + n], pY[0:32, :n], x="act")
                    else:
                        if s0 < TC:
                            dst = yacc[b][:, 0:TC][:, ::-1]
                        else:
                            dst = yacc[b][:, TC:T][:, ::-1][:, s0 - TC:s0 - TC + n]
                        k.cp(ytmp[:, :n], pY[0:32, :n], x="act")
                        k.tt(dst, dst, ytmp[:, :n], ALU.add, x="pool")
        for b in range(NB):
            k.dma("sp", pr.S["S5y"][b, j * 32:(j + 1) * 32, :], yacc[b])
        yield


def s5_out(pr, l, ybr):
    k = pr.k
    k.push()
    IN = pr.IN
    P = pr.S["P"]
    dT = k.sb("s5o_d", [128, 4])
    gb = k.sb("s5o_gb", [128, 4])
    gw = k.sb("s5o_gw", [128, 4, 512])
    k.dma("sp", dT, IN["s5_dT"][l])
    k.dma("sp", gb, IN["s5_glu_bT"][l])
    k.dma("sp", gw, IN["s5_glu_w"][l].rr("(kc p) n -> p kc n", p=128))
    yt = [k.sb("s5o_y%d" % i, [128, 4, 512]) for i in range(2)]
    ut = [k.sb("s5o_u%d" % i, [128, 4, 512]) for i in range(2)]
    z = k.sb("s5o_z", [128, 4, 512])
    t1 = k.sb("s5o_t1", [128, 4, 512])
    gl = k.sb("s5o_gl", [128, 4, 512])
    ot = [k.sb("s5o_o%d" % i, [128, 512]) for i in range(2)]
    ps = [k.ps("s5o_ps%d" % i, [128, 512]) for i in range(2)]
    it = 0
    oi = 0
    for b in range(NB):
        for (t0, n, ra, rb) in nat_tiles():
            y = yt[it % 2]
            u = ut[it % 2]
            it += 1
            k.dma("sp", y[:, :, :n], pr.S["S5y"][b, :, t0:t0 + n].rr("(c p) t -> p c t", p=128))
            k.dma("pool", u[:, :, :n], P[b, OFF_S5:OFF_S5 + MIXW, t0:t0 + n].rr("(c p) t -> p c t", p=128))
            for c in range(4):
                k.stt(z[:, c, :n], u[:, c, :n], dT[:, c:c + 1], y[:, c, :n], ALU.mult, ALU.add)
            k.tt(t1[:, :, :n], z[:, :, :n], z[:, :, :n], ALU.mult, x="pool")
            k.ts(t1[:, :, :n], t1[:, :, :n], 0.044715, ALU.mult, 1.0, ALU.add)
            k.tt(t1[:, :, :n], t1[:, :, :n], z[:, :, :n], ALU.mult, x="pool")
            k.act(t1[:, :, :n], t1[:, :, :n], AF.Sigmoid, scale=2.0 * math.sqrt(2.0 / math.pi))
            k.tt(gl[:, :, :n], z[:, :, :n], t1[:, :, :n], ALU.mult)
            for oc in range(4):
                pp = ps[oi % 2]
                o = ot[oi % 2]
                oi += 1
                for kc in range(4):
                    k.mm(pp[:, :n], gw[:, kc, oc * 128:(oc + 1) * 128], gl[:, kc, :n], start=(kc == 0), stop=(kc == 3))
                k.act(o[:, :n], pp[:, :n], AF.Sigmoid, bias=gb[:, oc:oc + 1], scale=1.0)
                k.tt(o[:, :n], o[:, :n], gl[:, oc, :n], ALU.mult)
                k.dma("sp", ybr[b, oc * 128:(oc + 1) * 128, t0:t0 + n], o[:, :n])
    k.pop()


def sin_turns(k, dst, turns, t1):
    k.ts(t1, turns, MAGIC, ALU.add)
    k.ts(t1, t1, -MAGIC, ALU.add)
    k.tt(t1, turns, t1, ALU.subtract)
    k.ts(dst, t1, 0.5, ALU.is_gt)
    k.tt(t1, t1, dst, ALU.subtract)
    k.ts(dst, t1, -0.5, ALU.is_lt)
    k.tt(t1, t1, dst, ALU.add)
    k.act(dst, t1, AF.Sin, scale=TWO_PI_LO)


def hyena_declare(pr):
    for L in (TL, TC):
        pr.inp("hy_zT%d" % L, [33, L])
        pr.inp("hy_win%d" % L, [L, MIXW])
        pr.inp("dft_c%d" % L, [L, L])
        pr.inp("dft_s%d" % L, [L, L])
        pr.inp("idft_c%d" % L, [L, L])
        pr.inp("idft_s%d" % L, [L, L])
    pr.inp("hy_cwT", [DEPTH, 3, 128, 12])
    pr.inp("hy_cbT", [DEPTH, 128, 12])
    pr.inp("hy_biasT", [DEPTH, 128, 4])
    pr.inp("hy_f_w1", [DEPTH, 33, 64])
    pr.inp("hy_f_b1T", [DEPTH, 64, 1])
    pr.inp("hy_f_w2", [DEPTH, 64, 64])
    pr.inp("hy_f_b2T", [DEPTH, 64, 1])
    pr.inp("hy_f_w3", [DEPTH, 64, 1024])
    pr.inp("hy_f_b3", [DEPTH, 1, 1024])
    pr.scratch("HYx0", [NB, MIXW, TL])
    pr.scratch("HYz", [NB, MIXW, TL])
    pr.scratch("HYY", [NB, 2, TL, MIXW])


def hyena_stage(pr, l, L, toff, ybr):
    k = pr.k
    IN = pr.IN
    P = pr.S["P"]
    I = pr.ident
    NT = L // 128
    TW = min(L, 512)
    NTT = L // TW
    sfx = "%d" % L
    k.push()
    KC = k.sb("hy_KC", [128, NT, MIXW])
    KS = k.sb("hy_KS", [128, NT, MIXW])
    tabs_cur = [None]
    pacc = [k.ps("hy_pacc%d" % i, [128, 512]) for i in range(4)]
    pw = [k.ps("hy_pw%d" % i, [128, 512]) for i in range(2)]
    tabi = [0]

    def load_tabs(fc):
        tb = tabs_cur[0][tabi[0] % 3]
        tabi[0] += 1
        k.dma("sp", tb[0], IN["dft_c" + sfx][:, fc * 128:(fc + 1) * 128].rr("(tc p) f -> p tc f", p=128))
        k.dma("sp", tb[1], IN["dft_s" + sfx][:, fc * 128:(fc + 1) * 128].rr("(tc p) f -> p tc f", p=128))
        return tb

    k.push()
    tabs_cur[0] = [[k.sb("hy_tabA%d%d" % (i, x), [128, NT, 128]) for x in range(2)] for i in range(3)]
    fw1 = k.sb("hy_fw1", [33, 64])
    fw2 = k.sb("hy_fw2", [64, 64])
    fw3 = k.sb("hy_fw3", [64, 1024])
    fb = k.sb("hy_fb", [64, 2])
    fb3 = k.sb("hy_fb3", [1, 1024])
    h2 = k.sb("hy_h2", [64, L])
    k.push()
    zT = k.sb("hy_zT", [33, L])
    k.dma("sp", fw1, IN["hy_f_w1"][l])
    k.dma("sp", fw2, IN["hy_f_w2"][l])
    k.dma("sp", fw3, IN["hy_f_w3"][l])
    k.dma("sp", fb[:, 0:1], IN["hy_f_b1T"][l])
    k.dma("sp", fb[:, 1:2], IN["hy_f_b2T"][l])
    k.dma("sp", fb3, IN["hy_f_b3"][l])
    k.dma("sp", zT, IN["hy_zT" + sfx])
    h1 = k.sb("hy_h1", [64, L])
    tu = k.sb("hy_tu", [64, 512])
    tv = k.sb("hy_tv", [64, 512])
    for (src, w, bcol, dst) in ((zT, fw1, 0, h1), (h1, fw2, 1, h2)):
        for i in range(NTT):
            pp = pw[i % 2]
            k.mm(pp[0:64, :TW], w, src[:, i * TW:(i + 1) * TW])
            k.ts(tu[:, :TW], pp[0:64, :TW], fb[:, bcol:bcol + 1], ALU.add, 1.0 / (2.0 * math.pi), ALU.mult)
            sin_turns(k, dst[:, i * TW:(i + 1) * TW], tu[:, :TW], tv[:, :TW])
    k.pop()
    hsum = k.sb("hy_hsum", [128, NT, MIXW])
    hdif = k.sb("hy_hdif", [128, NT, MIXW])
    win = [k.sb("hy_win%d" % i, [128, MIXW]) for i in range(2)]
    hf = k.sb("hy_hf", [128, MIXW])
    hb = k.sb("hy_hb", [128, MIXW])
    for tc in range(NT):
        wt = win[tc % 2]
        k.dma("sp", wt, IN["hy_win" + sfx][tc * 128:(tc + 1) * 128, :])
        for half, dst in ((0, hf), (1, hb)):
            pp = pw[half]
            k.mm(pp, h2[:, tc * 128:(tc + 1) * 128], fw3[:, half * 512:(half + 1) * 512], start=True, stop=False)
            k.mm(pp, pr.ones[0:1, :], fb3[:, half * 512:(half + 1) * 512], start=False, stop=True)
            k.tt(dst, pp, wt, ALU.mult)
        k.tt(hdif[:, tc, :], hb, hf, ALU.subtract, x="pool")
        if tc == 0:
            k.memset(hb[0:1, :], 0.0)
        k.tt(hsum[:, tc, :], hf, hb, ALU.add, x="pool")
    for fc in range(NT):
        tb = load_tabs(fc)
        for x, src, dst in ((0, hsum, KC), (1, hdif, KS)):
            pp = pw[x]
            for tc in range(NT):
                k.mm(pp, tb[x][:, tc, :], src[:, tc, :], start=(tc == 0), stop=(tc == NT - 1))
            k.cp(dst[:, fc, :], pp, x=("act" if x == 0 else "dve"))
    k.pop()
    cw = k.sb("hy_cw", [128, 3, 12])
    cb = k.sb("hy_cb", [128, 12])
    hbias = k.sb("hy_bias", [128, 4])
    k.dma("sp", cw, IN["hy_cwT"][l].rr("i p c -> p i c"))
    k.dma("sp", cb, IN["hy_cbT"][l])
    k.dma("sp", hbias, IN["hy_biasT"][l])
    for b in range(NB):
        k.push()
        tabs_cur[0] = [[k.sb("hy_tabB%d%d" % (i, x), [128, NT, 128]) for x in range(2)] for i in range(3)]
        zTm = k.sb("hy_zTm", [128, NT, MIXW])
        raw = [k.sb("hy_raw%d" % i, [128, L + 2]) for i in range(2)]
        uu = [k.sb("hy_u%d" % i, [128, L]) for i in range(3)]
        ri = 0
        for i in range(4):
            for s in range(3):
                ch = s * 4 + i
                r = raw[ri % 2]
                ri += 1
                k.memset(r[:, 0:1], 0.0, x="pool")
                k.memset(r[:, L + 1:L + 2], 0.0, x="pool")
                k.dma("sp", r[:, 1:L + 1], P[b, OFF_HY + ch * 128:OFF_HY + (ch + 1) * 128, toff:toff + L])
                u = uu[s]
                k.act(u, r[:, 1:L + 1], AF.Identity, bias=cb[:, ch:ch + 1], scale=cw[:, 1, ch:ch + 1])
                k.stt(u, r[:, 0:L], cw[:, 0, ch:ch + 1], u, ALU.mult, ALU.add)
                k.stt(u, r[:, 2:L + 2], cw[:, 2, ch:ch + 1], u, ALU.mult, ALU.add)
            k.dma("pool", pr.S["HYx0"][b, i * 128:(i + 1) * 128, 0:L], uu[0])
            k.tt(uu[2], uu[2], uu[1], ALU.mult, x="pool")
            k.dma("pool", pr.S["HYz"][b, i * 128:(i + 1) * 128, 0:L], uu[2])
            for t4 in range(0, NT, 4):
                nn = min(4, NT - t4)
                pp = pw[(t4 // 4) % 2]
                for j in range(nn):
                    k.tr(pp[:, j * 128:(j + 1) * 128], uu[2][:, (t4 + j) * 128:(t4 + j + 1) * 128], I)
                k.cp(zTm[:, t4:t4 + nn, i * 128:(i + 1) * 128], pp[:, 0:nn * 128].rr("p (a b) -> p a b", b=128), x="act")
        uc = k.sb("hy_uc", [128, MIXW])
        us = k.sb("hy_us", [128, MIXW])
        yy = [[k.sb("hy_yy%d%d" % (i, x), [128, MIXW]) for x in range(2)] for i in range(2)]
        m1 = k.sb("hy_m1", [128, MIXW])
        for fc in range(NT):
            tb = load_tabs(fc)
            for x in range(2):
                pp = pw[x]
                for tc in range(NT):
                    k.mm(pp, tb[x][:, tc, :], zTm[:, tc, :], start=(tc == 0), stop=(tc == NT - 1))
            k.cp(uc, pw[0], x="act")
            k.cp(us, pw[1], x="act")
            y = yy[fc % 2]
            k.tt(y[0], uc, KC[:, fc, :], ALU.mult, x="dve")
            k.tt(m1, us, KS[:, fc, :], ALU.mult, x="pool")
            k.tt(y[0], y[0], m1, ALU.add, x="dve")
            k.tt(y[1], uc, KS[:, fc, :], ALU.mult, x="pool")
            k.tt(m1, us, KC[:, fc, :], ALU.mult, x="dve")
            k.tt(y[1], y[1], m1, ALU.subtract, x="pool")
            for x in range(2):
                k.dma("sp" if x == 0 else "pool", pr.S["HYY"][b, x, fc * 128:(fc + 1) * 128, :], y[x])
        k.pop()
        k.push()
        Y = [k.sb("hy_Y%d" % x, [128, NT, MIXW]) for x in range(2)]
        for x in range(2):
            k.dma("sp", Y[x], pr.S["HYY"][b, x, 0:L, :].rr("(fc p) c -> p fc c", p=128))
        itab = [[k.sb("hy_itab%d%d" % (i, x), [128, 512]) for x in range(2)] for i in range(6)]
        zt = [k.sb("hy_zt%d" % i, [128, 512]) for i in range(2)]
        x0t = [k.sb("hy_x0t%d" % i, [128, 512]) for i in range(2)]
        ot = [k.sb("hy_ot%d" % i, [128, 512]) for i in range(2)]
        ii = 0
        oi = 0
        for tt in range(NTT):
            for fc in range(NT):
                tb = itab[ii % 6]
                ii += 1
                k.dma("sp", tb[0][:, :TW], IN["idft_c" + sfx][fc * 128:(fc + 1) * 128, tt * TW:(tt + 1) * TW])
                k.dma("sp", tb[1][:, :TW], IN["idft_s" + sfx][fc * 128:(fc + 1) * 128, tt * TW:(tt + 1) * TW])
                for ci in range(4):
                    k.mm(pacc[ci][:, :TW], Y[0][:, fc, ci * 128:(ci + 1) * 128], tb[0][:, :TW],
                         start=(fc == 0), stop=False)
                    k.mm(pacc[ci][:, :TW], Y[1][:, fc, ci * 128:(ci + 1) * 128], tb[1][:, :TW],
                         start=False, stop=(fc == NT - 1))
            for ci in range(4):
                z = zt[oi % 2]
                x0 = x0t[oi % 2]
                o = ot[oi % 2]
                oi += 1
                k.dma("sp", z[:, :TW], pr.S["HYz"][b, ci * 128:(ci + 1) * 128, tt * TW:(tt + 1) * TW])
                k.dma("pool", x0[:, :TW], pr.S["HYx0"][b, ci * 128:(ci + 1) * 128, tt * TW:(tt + 1) * TW])
                k.stt(o[:, :TW], z[:, :TW], hbias[:, ci:ci + 1], pacc[ci][:, :TW], ALU.mult, ALU.add)
                k.tt(o[:, :TW], o[:, :TW], x0[:, :TW], ALU.mult, x="pool")
                k.dma("sp", ybr[b, ci * 128:(ci + 1) * 128, toff + tt * TW:toff + (tt + 1) * TW], o[:, :TW])
        k.pop()
    k.pop()


def ffn_declare(pr):
    pr.inp("w_branch", [DEPTH, 4, MIXW, D])
    pr.inp("w_out", [DEPTH, D, D])
    pr.inp("ffn_w1", [1, D, D_FF])
    pr.inp("ffn_w3", [1, D, D_FF])
    pr.inp("ffn_w2", [1, D_FF, D])
    pr.inp("moe_router_w", [1, D, NEXP])
    pr.inp("moe_router_b", [1, 1, NEXP])
    pr.inp("moe_w1", [1, NEXP, D, D_FFE])
    pr.inp("moe_w3", [1, NEXP, D, D_FFE])
    pr.inp("moe_w2", [1, NEXP, D_FFE, D])
    pr.inp("fin_gT", [128, 8])
    pr.out = pr.k.dram("out", [NB, D, TL], F32, kind="ExternalOutput")
    pr.scratch("XN2", [128, 8, NB * T], BF16)


def merge_stage(pr, l, hsrc, tiles):
    k = pr.k
    k.push()
    IN = pr.IN
    P = pr.S["P"]
    YBR = pr.S["YBR"]
    stg = k.sb("mg_stg", [128, 4, 1024])
    wbr = k.sb("mg_wbr", [128, 4, 4, 1024], BF16)
    wout = k.sb("mg_wout", [128, 8, 1024], BF16)
    for m in range(4):
        k.dma("sp", stg, IN["w_branch"][l, m].rr("(kc p) n -> p kc n", p=128))
        k.cp(wbr[:, m], stg, x=("act" if m % 2 == 0 else "pool"))
    for hf in range(2):
        k.dma("sp", stg, IN["w_out"][l, hf * 512:(hf + 1) * 512, :].rr("(kc p) n -> p kc n", p=128))
        k.cp(wout[:, hf * 4:(hf + 1) * 4], stg, x=("act" if hf == 0 else "pool"))
    acc = k.sb("mg_acc", [128, 8, 512])
    accb = k.sb("mg_accb", [128, 8, 512], BF16)
    yt = [k.sb("mg_y%d" % i, [128, 4, 512]) for i in range(1)]
    ybf = [k.sb("mg_ybf%d" % i, [128, 4, 512], BF16) for i in range(2)]
    gt = [k.sb("mg_g%d" % i, [128, 8, 512]) for i in range(2)]
    tmp = [k.sb("mg_tmp%d" % i, [128, 512]) for i in range(2)]
    ht = k.sb("mg_h", [128, 8, 512])
    hn = k.sb("mg_hn", [128, 8, 512])
    ps = [k.ps("mg_ps%d" % i, [128, 512]) for i in range(4)]
    it = 0
    pi = 0
    for (b, t0, n, is_ctx) in tiles:
        j = 2 if is_ctx else b
        k.dma("sp", ht[:, :, :n], hsrc[b, :, t0:t0 + n].rr("(kc p) t -> p kc t", p=128))
        for m in range(4):
            y = yt[0]
            yb = ybf[it % 2]
            g = gt[it % 2]
            it += 1
            k.dma("sp", y[:, :, :n], YBR[m, b, :, t0:t0 + n].rr("(kc p) t -> p kc t", p=128))
            k.dma("pool", g[:, :, :n], P[b, OFF_GATE + m * D:OFF_GATE + (m + 1) * D, t0:t0 + n].rr("(oc p) t -> p oc t", p=128))
            k.cp(yb[:, :, :n], y[:, :, :n], x="act")
            for oc in range(8):
                pp = ps[pi % 4]
                pi += 1
                for kc in range(4):
                    k.mm(pp[:, :n], wbr[:, m, kc, oc * 128:(oc + 1) * 128], yb[:, kc, :n], start=(kc == 0), stop=(kc == 3))
                if m == 0:
                    k.tt(acc[:, oc, :n], pp[:, :n], g[:, oc, :n], ALU.mult)
                else:
                    tp = tmp[pi % 2]
                    k.tt(tp[:, :n], pp[:, :n], g[:, oc, :n], ALU.mult)
                    k.tt(acc[:, oc, :n], acc[:, oc, :n], tp[:, :n], ALU.add, x="dve")
        k.cp(accb[:, :, :n], acc[:, :, :n], x="act")
        for oc in range(8):
            pp = ps[pi % 4]
            pi += 1
            for kc in range(8):
                k.mm(pp[:, :n], wout[:, kc, oc * 128:(oc + 1) * 128], accb[:, kc, :n], start=(kc == 0), stop=(kc == 7))
            k.stt(hn[:, oc, :n], pp[:, :n], pr.mod[l][:, 16 + oc, j:j + 1], ht[:, oc, :n], ALU.mult, ALU.add)
        k.dma("pool", pr.S["hbuf"][b, :, t0:t0 + n].rr("(kc p) t -> p kc t", p=128), hn[:, :, :n])
    k.pop()


def norm2_stage(pr, l, tiles, xn2, router=None, xn2_dram=None):
    k = pr.k
    k.push()
    hts = [k.sb("n2_ht%d" % i, [128, 8, 512]) for i in range(2)]
    sq = k.sb("n2_sq", [128, 8, 512])
    tmp = k.sb("n2_tmp", [128, 8, 512])
    rs = k.sb("n2_rs", [128, 512])
    pss = k.ps("n2_pss", [128, 512])
    if xn2 is None:
        xob = [k.sb("n2_xo%d" % i, [128, 8, 512], BF16) for i in range(2)]
    if router is not None:
        gate_tm, rw_, rb_ = router
        xf = k.sb("n2_xf", [128, 8, 512])
        plg = k.ps("n2_plg", [128, 512])
        lg = k.sb("n2_lg", [128, 8])
        e1 = k.sb("n2_e1", [128, 8])
        e2 = k.sb("n2_e2", [128, 8])
        l2 = k.sb("n2_l2", [128, 8])
        sc = k.sb("n2_sc", [128, 8])
    for i, (b, t0, n, is_ctx) in enumerate(tiles):
        ht = hts[i % 2]
        k.dma("sp", ht[:, :, :n], pr.S["hbuf"][b, :, t0:t0 + n].rr("(kc p) t -> p kc t", p=128))
        if xn2 is None:
            xo = xob[i % 2]
            pr.norm_tile(l, 2, ht, n, b, is_ctx, xo[:, :, :n], sq, pss, rs, tmp)
            k.dma("sp", xn2_dram[:, :, b * T + t0:b * T + t0 + n], xo[:, :, :n])
        else:
            pr.norm_tile(l, 2, ht, n, b, is_ctx, xn2[:, :, b * T + t0: b * T + t0 + n], sq, pss, rs, tmp)
        if router is not None:
            j = b
            for kc in range(8):
                k.act(xf[:, kc, :n], tmp[:, kc, :n], AF.Identity, bias=pr.mod[l][:, 24 + kc, j:j + 1], scale=1.0)
            for blk in range(n // 128):
                gblk = (b * TL + (t0 - TC)) // 128 + blk
                pl = plg[:, blk * 8:(blk + 1) * 8]
                for kc in range(8):
                    k.mm(pl, xf[:, kc, blk * 128:(blk + 1) * 128], rw_[:, kc, :], start=(kc == 0), stop=False)
                k.mm(pl, pr.ones[0:1, :], rb_, start=False, stop=True)
            for blk in range(n // 128):
                gblk = (b * TL + (t0 - TC)) // 128 + blk
                k.cp(lg, plg[:, blk * 8:(blk + 1) * 8])
                k.reduce(sc[:, 0:1], lg, ALU.max)
                k.ts(e1, lg, sc[:, 0:1], ALU.is_equal)
                k.stt(l2, e1, -1e30, lg, ALU.mult, ALU.add)
                k.reduce(sc[:, 1:2], l2, ALU.max)
                k.ts(e2, l2, sc[:, 1:2], ALU.is_equal)
                k.tt(sc[:, 2:3], sc[:, 1:2], sc[:, 0:1], ALU.subtract)
                k.act(sc[:, 3:4], sc[:, 2:3], AF.Exp)
                k.ts(sc[:, 4:5], sc[:, 3:4], 1.0, ALU.add)
                k.recip(sc[:, 5:6], sc[:, 4:5])
                k.tt(sc[:, 6:7], sc[:, 3:4], sc[:, 5:6], ALU.mult)
                k.ts(gate_tm[:, gblk, :], e1, sc[:, 5:6], ALU.mult)
                k.stt(gate_tm[:, gblk, :], e2, sc[:, 6:7], gate_tm[:, gblk, :], ALU.mult, ALU.add)
    k.pop()


def super_tiles(tiles, maxtok):
    sts = []
    cur = []
    tot = 0
    for t in tiles:
        if cur and (tot + t[2] > maxtok or cur[0][0] != t[0]):
            sts.append(cur)
            cur, tot = [], 0
        cur.append(t)
        tot += t[2]
    sts.append(cur)
    return sts


def swiglu_stage(pr, l, tiles, xn2, w1, w3, w2, dff, maxtok, experts=None, gate_tm=None, xn2_dram=None):
    k = pr.k
    k.push()
    nff = dff // 128
    I = pr.ident
    actb = k.sb("ff_act", [128, nff, maxtok], BF16)
    NW = 4
    wst = [k.sb("ff_wst%d" % i, [128, 8, 128]) for i in range(NW)]
    wbf = [k.sb("ff_wbf%d" % i, [128, 8, 128], BF16) for i in range(NW)]
    w2st = [k.sb("ff_w2st%d" % i, [128, nff, 128]) for i in range(1)]
    w2bf = [k.sb("ff_w2bf%d" % i, [128, nff, 128], BF16) for i in range(2)]
    st1 = [k.sb("ff_s1_%d" % i, [128, 512]) for i in range(2)]
    pa = [k.ps("ff_pa%d" % i, [128, 512]) for i in range(2)]
    pb = [k.ps("ff_pb%d" % i, [128, 512]) for i in range(2)]
    po = [k.ps("ff_po%d" % i, [128, 512]) for i in range(2)]
    ht = [k.sb("ff_h%d" % i, [128, 512]) for i in range(2)]
    hn = [k.sb("ff_hn%d" % i, [128, 512]) for i in range(2)]
    ne = 1 if experts is None else experts
    if experts is not None:
        acc = k.sb("ff_acc", [128, 8, maxtok])
        GE = [k.sb("ff_GE%d" % i, [128, maxtok]) for i in range(2)]
        gbc = [k.sb("ff_gbc%d" % i, [128, 128]) for i in range(2)]
        tmpg = [k.sb("ff_tg%d" % i, [128, 512]) for i in range(2)]
        pg = k.ps("ff_pg", [128, 512])
    if xn2 is None:
        xloc = k.sb("ff_xloc", [128, 8, maxtok], BF16)
    wi = 0
    w2i = 0
    si = 0
    hi_ = 0
    gi_ = 0
    for st in super_tiles(tiles, maxtok):
        offs = []
        o = 0
        for t in st:
            offs.append(o)
            o += t[2]
        if xn2 is None:
            for ti, (b, t0, n, is_ctx) in enumerate(st):
                k.dma("sp", xloc[:, :, offs[ti]:offs[ti] + n], xn2_dram[:, :, b * T + t0:b * T + t0 + n])
        for e in range(ne):
            W1 = w1 if experts is None else w1[e]
            W3 = w3 if experts is None else w3[e]
            W2 = w2 if experts is None else w2[e]
            if experts is not None:
                ge = GE[e % 2]
                for ti, (b, t0, n, is_ctx) in enumerate(st):
                    for blk in range(n // 128):
                        gblk = (b * TL + (t0 - TC)) // 128 + blk
                        gb_ = gbc[gi_ % 2]
                        gi_ += 1
                        k.cp(gb_, gate_tm[:, gblk, e:e + 1].bc([128, 128]), x="pool")
                        k.mm(pg[:, blk * 128:(blk + 1) * 128], gb_, I)
                    k.cp(ge[:, offs[ti]:offs[ti] + n], pg[:, :n], x="act")
            for ffc in range(nff):
                ws = []
                for wsrc in (W1, W3):
                    s_, b_ = wst[wi % NW], wbf[wi % NW]
                    k.dma("sp", s_, wsrc[:, ffc * 128:(ffc + 1) * 128].rr("(kc p) n -> p kc n", p=128))
                    k.cp(b_, s_, x=("dve" if wi % 2 == 0 else "act"))
                    wi += 1
                    ws.append(b_)
                for ti, (b, t0, n, is_ctx) in enumerate(st):
                    if xn2 is None:
                        xs = xloc[:, :, offs[ti]:offs[ti] + n]
                    else:
                        xs = xn2[:, :, b * T + t0:b * T + t0 + n]
                    p1, p3 = pa[si % 2], pb[si % 2]
                    s1 = st1[si % 2]
                    si += 1
                    for kc in range(8):
                        k.mm(p1[:, :n], ws[0][:, kc, :], xs[:, kc, :], start=(kc == 0), stop=(kc == 7))
                    for kc in range(8):
                        k.mm(p3[:, :n], ws[1][:, kc, :], xs[:, kc, :], start=(kc == 0), stop=(kc == 7))
                    k.act(s1[:, :n], p1[:, :n], AF.Silu)
                    k.tt(actb[:, ffc, offs[ti]:offs[ti] + n], s1[:, :n], p3[:, :n], ALU.mult)
            for oc in range(8):
                s_, b_ = w2st[0], w2bf[w2i % 2]
                k.dma("sp", s_, W2[:, oc * 128:(oc + 1) * 128].rr("(kc p) n -> p kc n", p=128))
                k.cp(b_, s_, x=("act" if w2i % 2 == 0 else "dve"))
                w2i += 1
                for ti, (b, t0, n, is_ctx) in enumerate(st):
                    pp = po[hi_ % 2]
                    for kc in range(nff):
                        k.mm(pp[:, :n], b_[:, kc, :], actb[:, kc, offs[ti]:offs[ti] + n], start=(kc == 0), stop=(kc == nff - 1))
                    j = 2 if is_ctx else b
                    h_, hn_ = ht[hi_ % 2], hn[hi_ % 2]
                    hi_ += 1
                    hv = pr.S["hbuf"][b, oc * 128:(oc + 1) * 128, t0:t0 + n]
                    if experts is not None:
                        a_ = acc[:, oc, offs[ti]:offs[ti] + n]
                        g_ = GE[e % 2][:, offs[ti]:offs[ti] + n]
                        if e == 0:
                            k.tt(a_, pp[:, :n], g_, ALU.mult)
                        else:
                            tg = tmpg[hi_ % 2]
                            k.tt(tg[:, :n], pp[:, :n], g_, ALU.mult)
                            k.tt(a_, a_, tg[:, :n], ALU.add, x="pool")
                        if e < ne - 1:
                            continue
                        k.dma("sp", h_[:, :n], hv)
                        k.stt(hn_[:, :n], a_, pr.mod[l][:, 40 + oc, j:j + 1], h_[:, :n], ALU.mult, ALU.add)
                    else:
                        k.dma("sp", h_[:, :n], hv)
                        k.stt(hn_[:, :n], pp[:, :n], pr.mod[l][:, 40 + oc, j:j + 1], h_[:, :n], ALU.mult, ALU.add)
                    k.dma("sp", hv, hn_[:, :n])
    k.pop()


def final_stage(pr):
    k = pr.k
    k.push()
    fg = k.sb("fin_g", [128, 8])
    k.dma("sp", fg, pr.IN["fin_gT"])
    hts = [k.sb("fin_ht%d" % i, [128, 8, 512]) for i in range(2)]
    sq = k.sb("fin_sq", [128, 8, 512])
    ot = [k.sb("fin_o%d" % i, [128, 8, 512]) for i in range(2)]
    rs = k.sb("fin_rs", [128, 512])
    pss = k.ps("fin_pss", [128, 512])
    i = 0
    for (b, t0, n, is_ctx) in tok_tiles():
        if is_ctx:
            continue
        ht = hts[i % 2]
        o = ot[i % 2]
        i += 1
        k.dma("sp", ht, pr.S["hbuf"][b, :, t0:t0 + n].rr("(kc p) t -> p kc t", p=128))
        k.act(sq, ht, AF.Square)
        for kc in range(8):
            k.mm(pss, pr.ones, sq[:, kc, :], start=(kc == 0), stop=(kc == 7))
        k.ts(rs, pss, 1.0 / D, ALU.mult, RMS_EPS, ALU.add)
        k.act(rs, rs, AF.Sqrt)
        k.recip(rs, rs)
        for kc in range(8):
            k.stt(o[:, kc, :], ht[:, kc, :], fg[:, kc:kc + 1], rs, ALU.mult, ALU.mult)
        k.dma("pool", pr.out[b, :, t0 - TC:t0 - TC + n].rr("(kc p) t -> p kc t", p=128), o)
    k.pop()


_PROG = {}


def kernel(**inputs):
    if "nc" not in _PROG:
        nc = bass.Bass("TRN2", target_bir_lowering=False)
        pr = Prog(nc)
        pr.build()
        _PROG["nc"] = nc
        _PROG["names"] = set(pr.IN.keys())
    nc = _PROG["nc"]
    sh = shared_inputs(inputs)
    maps = []
    for c in range(NCORE):
        m = dict(sh)
        m.update(core_inputs(inputs, c))
        maps.append({k_: v for k_, v in m.items() if k_ in _PROG["names"]})
    res = run_bass_kernel_spmd(nc, maps, core_ids=list(range(NCORE)))
    outs = [np.asarray(r["out"]) for r in res.results]
    full = np.concatenate(outs, axis=0)
    return np.ascontiguousarray(full.transpose(0, 2, 1)).astype(np.float32)
```

```python
import math
from contextlib import ExitStack
import numpy as np
import concourse.bass as bass
import concourse.mybir as mybir
from concourse.bass_utils import run_bass_kernel_spmd

F32 = mybir.dt.float32
BF16 = mybir.dt.bfloat16
I32 = mybir.dt.int32
ALU = mybir.AluOpType
AF = mybir.ActivationFunctionType
AX = mybir.AxisListType

D = 1024
TC = 256
TL = 2048
T = TC + TL
NB = 2
NCORE = 8
DEPTH = 2
MIXW = 512
IN_TOTAL = 9632
OFF_HY, OFF_RW, OFF_S5, OFF_GLA, OFF_GATE = 0, 1536, 3456, 3968, 5536
D_FF = 2816
D_FFE = 3584
NEXP = 8
RMS_EPS = 1e-6


class Res:
    __slots__ = ("name", "lw", "rd", "psum", "dram", "ws")

    def __init__(self, name, psum=False, dram=False):
        self.name = name
        self.lw = None
        self.rd = {}
        self.psum = psum
        self.dram = dram
        self.ws = {}


class V:
    __slots__ = ("ap", "res")

    def __init__(self, ap, res):
        self.ap = ap
        self.res = res

    def __getitem__(self, idx):
        return V(self.ap[idx], self.res)

    def bc(self, shape):
        return V(self.ap.to_broadcast(shape), self.res)

    def rr(self, pat, **kw):
        return V(self.ap.rearrange(pat, **kw), self.res)


class KB:
    ENG = ("pe", "act", "dve", "pool", "sp")
    NS = 8

    def __init__(self, nc):
        self.nc = nc
        self.es = ExitStack()
        self.e = dict(pe=nc.tensor, act=nc.scalar, dve=nc.vector, pool=nc.gpsimd, sp=nc.sync)
        self.cnt = {k: 0 for k in self.ENG}
        self.sem = {k: self.es.enter_context(nc.semaphore("s_" + k)) for k in self.ENG}
        self.waited = {k: {} for k in self.ENG}
        self.dq = {}
        for q in ("sp", "act", "pool"):
            self.dq[q] = dict(i=0, sems=[self.es.enter_context(nc.semaphore("d_%s%d" % (q, s))) for s in range(self.NS)],
                              vals=[0] * self.NS)
        self.scopes = []
        self.dram_res = []
        self.marks = []
        self.uid = 0
        self.n_ins = 0

    def _nm(self, name):
        self.uid += 1
        return "%s_%d" % (name, self.uid)

    def _stack(self):
        return self.scopes[-1] if self.scopes else self.es

    def sb(self, name, shape, dtype=F32):
        t = self._stack().enter_context(self.nc.sbuf_tensor(self._nm(name), list(shape), dtype))
        return V(t[tuple(slice(None) for _ in shape)], Res(name))

    def ps(self, name, shape, dtype=F32):
        t = self._stack().enter_context(self.nc.psum_tensor(self._nm(name), list(shape), dtype))
        return V(t[tuple(slice(None) for _ in shape)], Res(name, psum=True))

    def ps_split(self, name, nbanks, width):
        out = []
        for bk in range(nbanks):
            t = self.ps("%s_b%d" % (name, bk), [128, 512])
            for j in range(512 // width):
                out.append(V(t.ap[:, j * width:(j + 1) * width], Res("%s_%d_%d" % (name, bk, j))))
        return out

    def dram(self, name, shape, dtype=F32, kind="Internal"):
        t = self.nc.dram_tensor(name, list(shape), dtype, kind=kind)
        r = Res(name, dram=True)
        self.dram_res.append(r)
        return V(t.ap(), r)

    def push(self):
        st = ExitStack()
        self.scopes.append(st)
        return st

    def pop(self):
        if len(self.scopes) == 1:
            self.marks.append(dict(self.cnt))
        self.barrier()
        st = self.scopes.pop()
        st.close()

    def _semof(self, key):
        if isinstance(key, str):
            return self.sem[key]
        _, q, s = key
        return self.dq[q]["sems"][s]

    def _wait(self, x, ev):
        key, val = ev
        if x == "pe" and key == "pe":
            return
        if self.waited[x].get(key, 0) >= val:
            return
        self.e[x].wait_ge(self._semof(key), val)
        self.waited[x][key] = val
        self.n_ins += 1

    def _deps(self, reads, writes):
        evs = {}

        def add(ev):
            if ev is None:
                return
            k, v = ev
            if evs.get(k, 0) < v:
                evs[k] = v
        for r in reads:
            add(r.lw)
            for k, v in r.ws.items():
                add((k, v))
            if r.psum:
                for k, v in r.rd.items():
                    add((k, v))
        for w in writes:
            if not w.dram:
                add(w.lw)
            for k, v in w.rd.items():
                add((k, v))
        return list(evs.items())

    def _commit(self, ev, reads, writes):
        k, v = ev
        for r in reads:
            if r.rd.get(k, 0) < v:
                r.rd[k] = v
        for w in writes:
            w.lw = ev
            if w.dram:
                if w.ws.get(k, 0) < v:
                    w.ws[k] = v
            else:
                w.rd = {}

    def op(self, x, fn, reads, writes, *args, **kw):
        for ev in self._deps(reads, writes):
            self._wait(x, ev)
        ins = getattr(self.e[x], fn)(*args, **kw)
        self.cnt[x] += 1
        ev = (x, self.cnt[x])
        ins.then_inc(self.sem[x], 1)
        self._commit(ev, reads, writes)
        self.n_ins += 1
        return ins

    def dma(self, q, out, in_, **kw):
        d = self.dq[q]
        s = d["i"] % self.NS
        d["i"] += 1
        key = ("d", q, s)
        if d["vals"][s] > 0:
            self._wait(q, (key, d["vals"][s]))
        for ev in self._deps([in_.res], [out.res]):
            self._wait(q, ev)
        ins = self.e[q].dma_start(out=out.ap, in_=in_.ap, **kw)
        d["vals"][s] += 16
        ins.then_inc(d["sems"][s], 16)
        self._commit((key, d["vals"][s]), [in_.res], [out.res])
        self.n_ins += 1

    def barrier(self):
        evs = [(k, self.cnt[k]) for k in self.ENG if self.cnt[k] > 0]
        for q, d in self.dq.items():
            for s in range(self.NS):
                if d["vals"][s] > 0:
                    evs.append((("d", q, s), d["vals"][s]))
        for x in self.ENG:
            for ev in evs:
                if ev[0] == x:
                    continue
                self._wait(x, ev)
        for r in self.dram_res:
            r.ws = {}
            r.rd = {}

    R32 = False

    def mm(self, out, lhsT, rhs, start=True, stop=True, r32=None):
        la, ra = lhsT.ap, rhs.ap
        if (self.R32 if r32 is None else r32) and la.dtype == F32 and ra.dtype == F32:
            la = la.bitcast(mybir.dt.float32r)
            ra = ra.bitcast(mybir.dt.float32r)
        self.op("pe", "matmul", [lhsT.res, rhs.res], [out.res], out.ap, la, ra, start=start, stop=stop)

    def tr(self, out, in_, ident):
        self.op("pe", "transpose", [in_.res, ident.res], [out.res], out.ap, in_.ap, ident.ap)

    def act(self, out, in_, func, bias=None, scale=None, x="act"):
        rd = [in_.res]
        kw = {}
        if bias is not None:
            if isinstance(bias, V):
                rd.append(bias.res)
                kw["bias"] = bias.ap
            else:
                kw["bias"] = float(bias)
        if scale is not None:
            if isinstance(scale, V):
                rd.append(scale.res)
                kw["scale"] = scale.ap
            else:
                kw["scale"] = float(scale)
        self.op("act", "activation", rd, [out.res], out.ap, in_.ap, func, **kw)

    def tt(self, out, in0, in1, op, x="dve"):
        self.op(x, "tensor_tensor", [in0.res, in1.res], [out.res], out.ap, in0.ap, in1.ap, op)

    def ts(self, out, in0, s1, op0, s2=None, op1=None, x="dve"):
        rd = [in0.res]
        a1 = s1
        if isinstance(s1, V):
            rd.append(s1.res)
            a1 = s1.ap
        a2 = s2
        if isinstance(s2, V):
            rd.append(s2.res)
            a2 = s2.ap
        if op1 is None:
            self.op(x, "tensor_scalar", rd, [out.res], out.ap, in0.ap, a1, None, op0)
        else:
            self.op(x, "tensor_scalar", rd, [out.res], out.ap, in0.ap, a1, a2, op0, op1)

    def stt(self, out, in0, scalar, in1, op0, op1):
        rd = [in0.res, in1.res]
        a = scalar
        if isinstance(scalar, V):
            rd.append(scalar.res)
            a = scalar.ap
        self.op("dve", "scalar_tensor_tensor", rd, [out.res], out.ap, in0.ap, a, in1.ap, op0, op1)

    def cp(self, out, in_, x="dve"):
        if x == "act":
            self.op("act", "copy", [in_.res], [out.res], out.ap, in_.ap)
        else:
            self.op(x, "tensor_copy", [in_.res], [out.res], out.ap, in_.ap)

    def memset(self, out, val, x="dve"):
        self.op(x, "memset", [], [out.res], out.ap, val)

    def scan(self, out, d0, d1, init, op0=ALU.mult, op1=ALU.add):
        rd = [d0.res, d1.res]
        a = init
        if isinstance(init, V):
            rd.append(init.res)
            a = init.ap
        self.op("dve", "tensor_tensor_scan", rd, [out.res], out.ap, d0.ap, d1.ap, a, op0, op1)

    def recip(self, out, in_):
        self.op("dve", "reciprocal", [in_.res], [out.res], out.ap, in_.ap)

    def reduce(self, out, in_, op, axis=AX.X):
        self.op("dve", "tensor_reduce", [in_.res], [out.res], out.ap, in_.ap, axis, op)

    def iota(self, out, pattern, base=0, cm=0):
        self.op("pool", "iota", [], [out.res], out.ap, pattern, base=base, channel_multiplier=cm,
                allow_small_or_imprecise_dtypes=True)

    def finish(self):
        self.barrier()
        self.es.close()


def w_in_chunks():
    ch = []
    c = 0
    while c < IN_TOTAL:
        if c == OFF_GLA + 1536:
            ch.append((c, 32))
            c += 32
        else:
            ch.append((c, 128))
            c += 128
    assert c == IN_TOTAL
    return ch


def tok_tiles():
    tl = []
    for b in range(NB):
        tl.append((b, 0, TC, True))
        for i in range(TL // 512):
            tl.append((b, TC + i * 512, 512, False))
    return tl


class Prog:
    def __init__(self, nc, dbg=(), upto="all", p_input=False, only=None):
        self.p_input = p_input
        self.only = only
        self.nc = nc
        self.k = KB(nc)
        self.dbg = set(dbg)
        self.upto = upto
        self.IN = {}
        self.S = {}

    def inp(self, name, shape, dtype=F32):
        self.IN[name] = self.k.dram(name, shape, dtype, kind="ExternalInput")
        return self.IN[name]

    def scratch(self, name, shape, dtype=F32):
        kind = "ExternalOutput" if name in self.dbg else "Internal"
        self.S[name] = self.k.dram(name, shape, dtype, kind=kind)
        return self.S[name]

    def declare(self):
        inp = self.inp
        inp("h0", [NB, D, T])
        inp("cT", [128, 8, 3])
        inp("ident", [128, 128])
        inp("cmask", [4, 128, 128])
        inp("rmask", [128, 256])
        inp("ada_w", [DEPTH, D, 6 * D])
        inp("ada_bT", [DEPTH, 128, 48])
        inp("gmixT", [DEPTH, 128, 8])
        inp("gffnT", [DEPTH, 128, 8])
        inp("w_in", [DEPTH, D, IN_TOTAL])
        sc = self.scratch
        if self.only is not None and any(o in ("ffn0", "moe", "final") for o in self.only):
            self.S["hbuf"] = self.k.dram("hbuf", [NB, D, T], F32, kind="ExternalOutput")
            inp("hbuf_in", [NB, D, T])
        else:
            sc("hbuf", [NB, D, T])
        if self.p_input:
            inp("P", [NB, IN_TOTAL, T])
            self.S["P"] = self.IN["P"]
        else:
            sc("P", [NB, IN_TOTAL, T])
        sc("YBR", [4, NB, MIXW, T])
        rwkv_declare(self)
        gla_declare(self)
        s5_declare(self)
        hyena_declare(self)
        ffn_declare(self)
        sc("modout", [DEPTH, 128, 48 * 3])
        sc("xnout", [128, 8 * NB * T], BF16)

    def consts(self):
        k = self.k
        self.ident = k.sb("ident", [128, 128])
        k.dma("sp", self.ident, self.IN["ident"])
        self.ones = k.sb("ones", [128, 128])
        k.memset(self.ones, 1.0)
        self.cmask = k.sb("cmask", [128, 4, 128])
        k.dma("sp", self.cmask, self.IN["cmask"].rr("i p t -> p i t"))
        self.m_up, self.m_lo, self.m_upi, self.ones_bd = (self.cmask[:, i, :] for i in range(4))
        self.rmask = k.sb("rmask", [128, SEG])
        k.dma("sp", self.rmask, self.IN["rmask"])
        self.mod = [k.sb("mod%d" % l, [128, 48, 3]) for l in range(DEPTH)]
        self.A1 = [k.sb("A1_%d" % l, [128, 8, 3]) for l in range(DEPTH)]
        self.A2 = [k.sb("A2_%d" % l, [128, 8, 3]) for l in range(DEPTH)]

    def stage_mods(self):
        k = self.k
        k.push()
        sT = k.sb("sT", [128, 8, 3])
        k.dma("sp", sT, self.IN["cT"])
        k.act(sT, sT, AF.Silu)
        wb = [k.sb("adaw%d" % i, [128, 8, 512]) for i in range(2)]
        pm = [k.ps("pmod%d" % i, [128, 4, 3]) for i in range(2)]
        adab = k.sb("adab", [128, 48])
        gm = k.sb("gm", [128, 8])
        gf = k.sb("gf", [128, 8])
        it = 0
        for l in range(DEPTH):
            k.dma("sp", adab, self.IN["ada_bT"][l])
            k.dma("sp", gm, self.IN["gmixT"][l])
            k.dma("sp", gf, self.IN["gffnT"][l])
            for og in range(12):
                w = wb[it % 2]
                p = pm[it % 2]
                it += 1
                k.dma("sp" if og % 2 == 0 else "pool", w,
                      self.IN["ada_w"][l, :, og * 512:(og + 1) * 512].rr("(kc p) n -> p kc n", p=128))
                for j in range(4):
                    for kc in range(8):
                        k.mm(p[:, j, :], w[:, kc, j * 128:(j + 1) * 128], sT[:, kc, :], start=(kc == 0), stop=(kc == 7))
                for j in range(4):
                    oc = og * 4 + j
                    k.ts(self.mod[l][:, oc, :], p[:, j, :], adab[:, oc:oc + 1], ALU.add)
            k.stt(self.A1[l], self.mod[l][:, 8:16, :], 1.0, gm[:, :, None].bc([128, 8, 3]), ALU.add, ALU.mult)
            k.stt(self.A2[l], self.mod[l][:, 32:40, :], 1.0, gf[:, :, None].bc([128, 8, 3]), ALU.add, ALU.mult)
            if "modout" in self.dbg:
                k.dma("sp", self.S["modout"][l], self.mod[l].rr("p a b -> p (a b)"))
        k.pop()

    def norm_tile(self, l, which, ht, n, b, is_ctx, xn_out, sq, pss, rs, tmp):
        k = self.k
        j = 2 if is_ctx else b
        A = (self.A1 if which == 1 else self.A2)[l]
        sh0 = 0 if which == 1 else 24
        k.act(sq[:, :, :n], ht[:, :, :n], AF.Square)
        for kc in range(8):
            k.mm(pss[:, :n], self.ones, sq[:, kc, :n], start=(kc == 0), stop=(kc == 7))
        k.ts(rs[:, :n], pss[:, :n], 1.0 / D, ALU.mult, RMS_EPS, ALU.add)
        k.act(rs[:, :n], rs[:, :n], AF.Sqrt)
        k.recip(rs[:, :n], rs[:, :n])
        for kc in range(8):
            k.stt(tmp[:, kc, :n], ht[:, kc, :n], A[:, kc, j:j + 1], rs[:, :n], ALU.mult, ALU.mult)
            k.act(xn_out[:, kc, :n], tmp[:, kc, :n], AF.Identity, bias=self.mod[l][:, sh0 + kc, j:j + 1], scale=1.0)

    def stage_norm1(self, l, src):
        k = self.k
        hts = [k.sb("ht%d" % i, [128, 8, 512]) for i in range(2)]
        sq = k.sb("sq", [128, 8, 512])
        tmp = k.sb("ntmp", [128, 8, 512])
        rs = k.sb("rs", [128, 512])
        pss = k.ps("pss", [128, 512])
        for i, (b, t0, n, is_ctx) in enumerate(tok_tiles()):
            ht = hts[i % 2]
            k.dma("sp", ht[:, :, :n], src[b, :, t0:t0 + n].rr("(kc p) t -> p kc t", p=128))
            self.norm_tile(l, 1, ht, n, b, is_ctx, self.xn[:, :, b * T + t0: b * T + t0 + n], sq, pss, rs, tmp)

    def stage_win(self, l):
        k = self.k
        chunks = w_in_chunks()
        groups = []
        cur = []
        for c in chunks:
            if cur and (c[0] + c[1] - cur[0][0] > 512):
                groups.append(cur)
                cur = []
            cur.append(c)
        groups.append(cur)
        wst = [k.sb("wst%d" % i, [128, 8, 512]) for i in range(2)]
        wbf = [k.sb("wbf%d" % i, [128, 8, 512], BF16) for i in range(2)]
        pp = [k.ps("pwin%d" % i, [128, 512]) for i in range(4)]
        ost = [k.sb("ost%d" % i, [128, 512]) for i in range(4)]
        tiles = tok_tiles()
        it = 0
        P = self.S["P"]
        for gi, g in enumerate(groups):
            g0 = g[0][0]
            g1 = g[-1][0] + g[-1][1]
            ws = wst[gi % 2]
            wb = wbf[gi % 2]
            k.dma("sp", ws[:, :, :g1 - g0],
                  self.IN["w_in"][l, :, g0:g1].rr("(kc p) n -> p kc n", p=128))
            if gi % 2 == 0:
                k.cp(wb[:, :, :g1 - g0], ws[:, :, :g1 - g0], x="dve")
            else:
                k.cp(wb[:, :, :g1 - g0], ws[:, :, :g1 - g0], x="act")
            for (c0, m) in g:
                is_gate = c0 >= OFF_GATE
                for (b, t0, n, is_ctx) in tiles:
                    p = pp[it % 4]
                    o = ost[it % 4]
                    for kc in range(8):
                        k.mm(p[:m, :n], wb[:, kc, c0 - g0:c0 - g0 + m], self.xn[:, kc, b * T + t0:b * T + t0 + n],
                             start=(kc == 0), stop=(kc == 7))
                    if is_gate:
                        k.act(o[:m, :n], p[:m, :n], AF.Sigmoid)
                    elif it % 2 == 0:
                        k.cp(o[:m, :n], p[:m, :n], x="dve")
                    else:
                        k.cp(o[:m, :n], p[:m, :n], x="act")
                    k.dma("sp" if it % 4 != 3 else "pool", P[b, c0:c0 + m, t0:t0 + n], o[:m, :n])
                    it += 1

    def build(self):
        k = self.k
        self.declare()
        self.consts()
        if self.only is not None:
            for l in range(1):
                if "rwkv" in self.only or "rwkv_prep" in self.only:
                    rwkv_prep(self, l)
                if "rwkv" in self.only or "rwkv_scan" in self.only:
                    rwkv_scan(self, l)
                if "rwkv" in self.only or "rwkv_read" in self.only:
                    rwkv_readout(self, l, self.S["YBR"][1])
                if "rws5" in self.only:
                    rwkv_prep(self, l)
                    rwkv_s5_scan(self, l)
                    rwkv_readout(self, l, self.S["YBR"][1])
                    s5_out(self, l, self.S["YBR"][2])
                if "hyena" in self.only:
                    hyena_stage(self, l, TL, TC, self.S["YBR"][0])
                    hyena_stage(self, l, TC, 0, self.S["YBR"][0])
                if "s5" in self.only:
                    s5_scan(self, l)
                    s5_out(self, l, self.S["YBR"][2])
                if "gla" in self.only:
                    gla_prep(self, l)
                    gla_scan(self, l)
                    gla_readout(self, l, self.S["YBR"][3])
            if any(o in ("ffn0", "moe", "final") for o in self.only):
                self.stage_mods()
                k.dma("sp", self.S["hbuf"], self.IN["hbuf_in"])
                alltiles = tok_tiles()
                lat = [t for t in alltiles if not t[3]]
                k.push()
                if "ffn0" in self.only:
                    xn2 = k.sb("xn2", [128, 8, NB * T], BF16)
                    norm2_stage(self, 0, alltiles, xn2)
                    swiglu_stage(self, 0, alltiles, xn2, self.IN["ffn_w1"][0], self.IN["ffn_w3"][0],
                                 self.IN["ffn_w2"][0], D_FF, 1280)
                if "moe" in self.only:
                    l = 1
                    gate_tm = k.sb("gate_tm", [128, NB * TL // 128, NEXP])
                    rw_ = k.sb("router_w", [128, 8, NEXP])
                    rb_ = k.sb("router_b", [1, NEXP])
                    k.dma("sp", rw_, self.IN["moe_router_w"][0].rr("(kc p) e -> p kc e", p=128))
                    k.dma("sp", rb_, self.IN["moe_router_b"][0])
                    norm2_stage(self, l, lat, None, router=(gate_tm, rw_, rb_), xn2_dram=self.S["XN2"])
                    swiglu_stage(self, l, lat, None, self.IN["moe_w1"][0], self.IN["moe_w3"][0],
                                 self.IN["moe_w2"][0], D_FFE, 1024, experts=NEXP, gate_tm=gate_tm, xn2_dram=self.S["XN2"])
                k.pop()
                if "final" in self.only:
                    final_stage(self)
            return self.end()
        self.stage_mods()
        if self.upto == "mods":
            return self.end()
        S = self.S
        alltiles = tok_tiles()
        lat = [t for t in alltiles if not t[3]]
        for l in range(DEPTH):
            last = (l == DEPTH - 1)
            hsrc = self.IN["h0"] if l == 0 else S["hbuf"]
            if not self.p_input:
                k.push()
                self.xn = k.sb("xn", [128, 8, NB * T], BF16)
                self.stage_norm1(l, hsrc)
                self.stage_win(l)
                k.pop()
            if self.upto == "win":
                return self.end()
            hyena_stage(self, l, TL, TC, S["YBR"][0])
            if not last:
                hyena_stage(self, l, TC, 0, S["YBR"][0])
            rwkv_prep(self, l)
            rwkv_scan(self, l)
            rwkv_readout(self, l, S["YBR"][1])
            s5_scan(self, l)
            s5_out(self, l, S["YBR"][2])
            gla_prep(self, l)
            gla_scan(self, l)
            gla_readout(self, l, S["YBR"][3])
            if self.upto == "mixers":
                return self.end()
            merge_stage(self, l, hsrc, lat if last else alltiles)
            if self.upto == "merge":
                return self.end()
            k.push()
            if l % 2 == 0:
                xn2 = k.sb("xn2", [128, 8, NB * T], BF16)
                norm2_stage(self, l, alltiles, xn2)
                swiglu_stage(self, l, alltiles, xn2, self.IN["ffn_w1"][l // 2], self.IN["ffn_w3"][l // 2],
                             self.IN["ffn_w2"][l // 2], D_FF, 1280)
            else:
                gate_tm = k.sb("gate_tm", [128, NB * TL // 128, NEXP])
                rw_ = k.sb("router_w", [128, 8, NEXP])
                rb_ = k.sb("router_b", [1, NEXP])
                k.dma("sp", rw_, self.IN["moe_router_w"][l // 2].rr("(kc p) e -> p kc e", p=128))
                k.dma("sp", rb_, self.IN["moe_router_b"][l // 2])
                norm2_stage(self, l, lat, None, router=(gate_tm, rw_, rb_), xn2_dram=S["XN2"])
                swiglu_stage(self, l, lat, None, self.IN["moe_w1"][l // 2], self.IN["moe_w3"][l // 2],
                             self.IN["moe_w2"][l // 2], D_FFE, 1024, experts=NEXP, gate_tm=gate_tm, xn2_dram=S["XN2"])
            k.pop()
            if self.upto == "layer%d" % l:
                return self.end()
        final_stage(self)
        return self.end()

    def end(self):
        self.k.finish()
        return self.nc


def _fm(v, nch):
    return np.ascontiguousarray(np.asarray(v, np.float32).reshape(nch, 128).T)


def shared_inputs(inputs):
    sh = {}
    sh["ident"] = np.eye(128, dtype=np.float32)
    r = np.arange(128)
    same = (r[:, None] // 64) == (r[None, :] // 64)
    sh["cmask"] = np.stack([same & (r[:, None] < r[None, :]), same & (r[:, None] > r[None, :]),
                            same & (r[:, None] <= r[None, :]), same]).astype(np.float32)
    sh["rmask"] = np.ascontiguousarray(np.broadcast_to((np.arange(256) % 64 != 0).astype(np.float32), (128, 256)))
    rw = lambda n: np.asarray(inputs[n], np.float32)
    sh["rw_muT"] = np.stack([[_fm(rw("rw_mu")[l, i], 15) for i in range(2)] for l in range(DEPTH)])
    sh["rw_w0T"] = np.stack([[_fm(rw("rw_w0")[l, i], 4) for i in range(2)] for l in range(DEPTH)])
    sh["rw_a0T"] = np.stack([[_fm(rw("rw_a0")[l, i], 4) for i in range(2)] for l in range(DEPTH)])
    sh["rw_w_up"] = np.ascontiguousarray(rw("rw_w_up").reshape(DEPTH, 128, 512))
    sh["rw_a_up"] = np.ascontiguousarray(rw("rw_a_up").reshape(DEPTH, 128, 512))
    sh["rw_g_up"] = np.ascontiguousarray(rw("rw_g_up"))
    sh["gla_gate_up"] = np.ascontiguousarray(rw("gla_gate_up"))
    sh["gla_gate_bT"] = np.stack([[_fm(rw("gla_gate_b")[l, d], 2) for d in range(2)] for l in range(DEPTH)])
    sh["gla_norm_gT"] = np.ascontiguousarray(rw("gla_norm_g").reshape(DEPTH, 128, 1))
    for L in (TL, TC):
        n = 2 * L
        pos = np.arange(L, dtype=np.float64)
        t = pos / max(L - 1, 1)
        freqs = np.linspace(1e-4, 16 - 1, 16)
        ang = (2.0 * math.pi / L) * pos[:, None] * freqs[None, :]
        z = np.concatenate([t[:, None], np.cos(ang), -np.sin(ang)], axis=-1)
        sh["hy_zT%d" % L] = np.ascontiguousarray(z.T.astype(np.float32))
        rates = np.abs(np.linspace(math.log(1e-2) / 1.5, math.log(1e-2) / 0.3, MIXW))
        sh["hy_win%d" % L] = np.exp(-t[:, None] * rates[None, :]).astype(np.float32)
        om = 2.0 * math.pi * (np.arange(L, dtype=np.float64) + 0.5) / n
        ph = pos[:, None] * om[None, :]
        sh["dft_c%d" % L] = np.cos(ph).astype(np.float32)
        sh["dft_s%d" % L] = np.sin(ph).astype(np.float32)
        sh["idft_c%d" % L] = np.ascontiguousarray((np.cos(ph).T / L).astype(np.float32))
        sh["idft_s%d" % L] = np.ascontiguousarray((-np.sin(ph).T / L).astype(np.float32))
    sh["hy_cwT"] = np.stack([[_fm(rw("hy_conv_w")[l, i], 12) for i in range(3)] for l in range(DEPTH)])
    sh["hy_cbT"] = np.stack([_fm(rw("hy_conv_b")[l], 12) for l in range(DEPTH)])
    sh["hy_biasT"] = np.stack([_fm(rw("hy_bias")[l], 4) for l in range(DEPTH)])
    sh["hy_f_w1"] = rw("hy_f_w1"); sh["hy_f_w2"] = rw("hy_f_w2"); sh["hy_f_w3"] = rw("hy_f_w3")
    sh["hy_f_b1T"] = np.ascontiguousarray(rw("hy_f_b1")[:, :, None]); sh["hy_f_b2T"] = np.ascontiguousarray(rw("hy_f_b2")[:, :, None])
    sh["hy_f_b3"] = np.ascontiguousarray(rw("hy_f_b3")[:, None, :])
    for n in ("w_branch", "w_out", "ffn_w1", "ffn_w3", "ffn_w2", "moe_router_w", "moe_w1", "moe_w3", "moe_w2"):
        sh[n] = np.ascontiguousarray(rw(n))
    sh["moe_router_b"] = np.ascontiguousarray(rw("moe_router_b")[:, None, :])
    sh["fin_gT"] = _fm(rw("final_norm_g"), 8)
    sh["tau"] = np.ascontiguousarray(np.broadcast_to(np.arange(513, dtype=np.float32), (128, 513)))
    def pairT(a):
        a = np.asarray(a, np.float32)
        rest = a.shape[2:]
        return np.ascontiguousarray(np.moveaxis(a.reshape((16, 128) + rest), 0, 1))
    sh["s5_aT"] = np.stack([[[pairT(rw(n)[l, d]) for n in ("s5_a_re", "s5_a_im")] for d in range(2)] for l in range(DEPTH)])
    sh["s5_ldtT"] = np.stack([[pairT(np.repeat(rw("s5_log_dt")[l, d][:, None], 64, axis=1)) for d in range(2)] for l in range(DEPTH)])
    sh["s5_bT"] = np.stack([[pairT(rw(n)[l]) for n in ("s5_b_re", "s5_b_im")] for l in range(DEPTH)])
    cT = np.zeros((DEPTH, 2, 128, 16, 32), np.float32)
    for l in range(DEPTH):
        for x, n in enumerate(("s5_c_re", "s5_c_im")):
            c = rw(n)[l]
            for g in range(32):
                j, gl = g // 2, g % 2
                cT[l, x, gl * 64:(gl + 1) * 64, j, gl * 16:(gl + 1) * 16] = c[g].T
    sh["s5_cT"] = cT
    sh["s5_dT"] = np.stack([_fm(rw("s5_d")[l], 4) for l in range(DEPTH)])
    sh["s5_glu_w"] = np.ascontiguousarray(rw("s5_glu_w"))
    sh["s5_glu_bT"] = np.stack([_fm(rw("s5_glu_b")[l], 4) for l in range(DEPTH)])
    sh["rw_vecT"] = np.stack([[_fm(rw(n)[l].reshape(-1), 4) for n in ("rw_k_k", "rw_k_a", "rw_r_k", "rw_ln_g", "rw_ln_b")]
                              for l in range(DEPTH)])
    sh["ada_w"] = np.ascontiguousarray(inputs["ada_w"], dtype=np.float32)
    sh["ada_bT"] = np.stack([_fm(inputs["ada_b"][l], 48) for l in range(DEPTH)])
    sh["gmixT"] = np.stack([_fm(inputs["norm_mix_g"][l], 8) for l in range(DEPTH)])
    sh["gffnT"] = np.stack([_fm(inputs["norm_ffn_g"][l], 8) for l in range(DEPTH)])
    sh["w_in"] = np.ascontiguousarray(inputs["w_in"], dtype=np.float32)
    return sh


def core_inputs(inputs, core):
    b0 = core * NB
    x = np.asarray(inputs["x"][b0:b0 + NB], np.float32)
    ctx = np.asarray(inputs["ctx"][b0:b0 + NB], np.float32)
    h0 = np.concatenate([ctx.transpose(0, 2, 1), x.transpose(0, 2, 1)], axis=2)
    cm = {"h0": np.ascontiguousarray(h0)}
    cols = np.stack([inputs["c"][b0], inputs["c"][b0 + 1], inputs["c_ctx"]], axis=1).astype(np.float32)
    cm["cT"] = np.ascontiguousarray(cols.reshape(8, 128, 3).transpose(1, 0, 2))
    return cm


CH = 64
SEGC = 4
SEG = CH * SEGC


class CLA:
    MMDT = BF16

    def __init__(self, prog, delta, nvb, nbanks=8, nbuf=2):
        self.nbuf = nbuf
        dt = self.dt = CLA.MMDT
        self.pr = prog
        k = self.k = prog.k
        self.delta = delta
        self.nvb = nvb
        self.nop = 5 if delta else 3
        nv = nvb * 128
        self.nv = nv
        self.opin = [k.sb("cla_in%d" % i, [128, self.nop, SEG]) for i in range(nbuf)]
        self.vin = [k.sb("cla_vin%d" % i, [128, nvb, SEG]) for i in range(nbuf)]
        self.cum = k.sb("cla_cum", [128, SEG])
        self.tmp = k.sb("cla_tmp", [128, SEG])
        self.E = {n: k.sb("cla_E" + n, [128, SEG]) for n in (["p", "m", "l", "v"] if delta else ["p", "m", "l"])}
        self.WC = k.sb("cla_WC", [128, SEGC])
        self.mkt = [k.sb("cla_mkt%d" % i, [128, SEGC, CH]) for i in range(2)]
        self.mki = 0
        names = ["Rq", "Kd", "Kc"] + (["Kq", "Bd", "Bc"] if delta else [])
        self.bd = {n: k.sb("cla_bd" + n, [128, SEGC, 2, CH], dt) for n in names}
        for n in names:
            k.memset(self.bd[n], 0.0, x="pool")
        self.vpad = [k.sb("cla_vpad%d" % v, [128, SEGC, 2, CH]) for v in range(nvb)]
        for v in range(nvb):
            k.memset(self.vpad[v], 0.0, x="pool")
        nslot = 14 if delta else 5
        self.SL = [k.sb("cla_s%d" % s, [128, SEGC, 128], dt) for s in range(nslot)]
        self.slots = [[self.SL[s][:, c, :] for s in range(nslot)] for c in range(SEGC)]
        self.VBT = k.sb("cla_VB", [128, SEGC, nv], dt)
        self.OtT = k.sb("cla_Ot", [128, SEGC, nv], dt)
        self.HT = k.sb("cla_H", [128, SEGC, nv], dt)
        self.VB = [self.VBT[:, c, :] for c in range(SEGC)]
        self.Ot = [self.OtT[:, c, :] for c in range(SEGC)]
        self.H = [self.HT[:, c, :] for c in range(SEGC)]
        self.Tst = [k.sb("cla_T%d" % i, [128, nv]) for i in range(2)]
        self.Tsh = self.Tst if dt == F32 else [k.sb("cla_Tb%d" % i, [128, nv], dt) for i in range(2)]
        if dt == F32:
            self.Ib = prog.ident
        else:
            self.Ib = k.sb("cla_Ib", [128, 128], dt)
            k.cp(self.Ib, prog.ident, x="act")
        self.oseg = [k.sb("cla_oseg%d" % i, [128, nvb, SEG]) for i in range(2)]
        self.pst = [k.ps("cla_ps%d" % i, [128, 512]) for i in range(nbanks)]
        self.psn = nbanks
        self.pi = 0
        self.ei = 0

    def P(self):
        p = self.pst[self.pi % self.psn]
        self.pi += 1
        return p

    def evac(self, out, in_, neg=False):
        k = self.k
        if neg:
            k.act(out, in_, AF.Copy, scale=-1.0)
        elif self.ei % 3 == 0:
            k.cp(out, in_, x="dve")
        else:
            k.cp(out, in_, x="act")

    def run_gen(self, ops_dram, v_dram, o_dram):
        k = self.k
        pr = self.pr
        delta = self.delta
        nvb = self.nvb
        nv = self.nv
        I = pr.ident
        Ib = self.Ib
        f32 = (self.dt == F32)

        def trn(p, x):
            if f32:
                k.tr(p, x, I)
            else:
                k.mm(p, x, Ib)
        k.memset(self.Tst[0], 0.0)
        if not f32:
            k.memset(self.Tsh[0], 0.0)
        tcur = 0
        nseg = T // SEG
        for sg in range(nseg):
            s0 = sg * SEG
            oin = self.opin[sg % self.nbuf]
            vin = self.vin[sg % self.nbuf]
            k.dma("sp", oin, ops_dram[:, :, s0:s0 + SEG].rr("i p t -> p i t"))
            k.dma("pool", vin, v_dram[:, :, s0:s0 + SEG].rr("i p t -> p i t"))
            Rr, Kr, LW = oin[:, 0, :], oin[:, 1, :], oin[:, 2, :]
            k.scan(self.cum, pr.rmask, LW, 0.0)
            cum3 = self.cum.rr("p (c t) -> p c t", t=CH)
            cl = cum3[:, :, CH - 1:CH]
            k.act(self.E["p"], self.cum, AF.Exp)
            k.act(self.E["m"], self.cum, AF.Exp, scale=-1.0)
            k.tt(self.tmp.rr("p (c t) -> p c t", t=CH), cl.bc([128, SEGC, CH]), cum3, ALU.subtract, x="pool")
            k.act(self.E["l"], self.tmp, AF.Exp)
            k.act(self.WC.rr("p (c o) -> p c o", o=1), cl, AF.Exp)
            if delta:
                k.tt(self.tmp, self.cum, LW, ALU.subtract, x="pool")
                k.act(self.E["v"], self.tmp, AF.Exp)
            def mk(name, src, e):
                e3 = self.E[e].rr("p (c t) -> p c t", t=CH)
                s3 = src.rr("p (c t) -> p c t", t=CH)
                t3 = self.mkt[self.mki % 2]
                self.mki += 1
                k.tt(t3, s3, e3, ALU.mult, x="dve")
                k.cp(self.bd[name][0:64, :, 0, :], t3[0:64], x="act")
                k.cp(self.bd[name][64:128, :, 1, :], t3[64:128], x="act")
            mk("Rq", Rr, "p")
            mk("Kd", Kr, "m")
            mk("Kc", Kr, "l")
            if delta:
                KKr, Br = oin[:, 3, :], oin[:, 4, :]
                mk("Kq", KKr, "v")
                mk("Bd", Br, "m")
                mk("Bc", Br, "l")
            for v in range(nvb):
                v3 = vin[:, v, :].rr("p (c t) -> p c t", t=CH)
                if nvb == 1:
                    k.cp(self.vpad[0][0:64, :, 0, :], v3[0:64], x="act")
                    k.cp(self.vpad[0][64:128, :, 1, :], v3[64:128], x="act")
                else:
                    k.cp(self.vpad[v][:, :, v, :], v3, x="act")
            B = {n: [self.bd[n][:, c, :, :].rr("p h t -> p (h t)") for c in range(SEGC)] for n in self.bd}
            S = self.slots
            SL = self.SL
            CC = range(SEGC)

            def step(mmf, evall, w=128):
                rpb = 512 // w
                self.ei += 1
                regs = []
                banks = []
                for c in CC:
                    if c % rpb == 0:
                        bank = self.P()
                        banks.append((bank, c))
                    regs.append(bank[:, (c % rpb) * w:(c % rpb + 1) * w])
                for c in CC:
                    mmf(c, regs[c])
                yield
                for bank, c0 in banks:
                    ncb = min(rpb, SEGC - c0)
                    pv = bank[:, 0:ncb * w].rr("p (c x) -> p c x", x=w)
                    evall(pv, slice(c0, c0 + ncb), ncb)

            def m3(m, n_):
                return m[:, None, :].bc([128, n_, 128])
            bd3 = {n_: self.bd[n_].rr("p c h t -> p c (h t)") for n_ in self.bd}
            for v in range(nvb):
                yield from step(lambda c, p: k.tr(p, self.vpad[v][:, c, :, :].rr("p h t -> p (h t)"), I),
                                lambda pv, cs, n_: self.evac(self.VBT[:, cs, v * 128:(v + 1) * 128], pv))
            if delta:
                yield from step(lambda c, p: k.mm(p, B["Bd"][c], B["Kq"][c]),
                                lambda pv, cs, n_: k.stt(SL[1][:, cs], pv, -1.0, m3(pr.m_up, n_), ALU.mult, ALU.mult))
                yield from step(lambda c, p: k.mm(p, B["Kq"][c], B["Bd"][c]),
                                lambda pv, cs, n_: k.stt(SL[0][:, cs], pv, -1.0, m3(pr.m_lo, n_), ALU.mult, ALU.mult))
                yield from step(lambda c, p: k.mm(p, B["Kd"][c], B["Kq"][c]),
                                lambda pv, cs, n_: k.tt(SL[6][:, cs], pv, m3(pr.m_up, n_), ALU.mult))
                yield from step(lambda c, p: k.mm(p, B["Kd"][c], B["Rq"][c]),
                                lambda pv, cs, n_: k.tt(SL[7][:, cs], pv, m3(pr.m_upi, n_), ALU.mult))
                yield from step(lambda c, p: k.mm(p, B["Bd"][c], B["Rq"][c]),
                                lambda pv, cs, n_: k.stt(SL[8][:, cs], pv, -1.0, m3(pr.m_upi, n_), ALU.mult, ALU.mult))
                k.tt(SL[4], SL[1], m3(I, SEGC), ALU.add, x="dve")
                pc, pt, yc = 0, 1, 4
                for j in range(1, 6):
                    pn, ptn, yn = 2 - pc, 4 - pt, 9 - yc
                    yield from step(lambda c, p: k.mm(p, S[c][pt], S[c][pc]),
                                    lambda pv, cs, n_: self.evac(SL[pn][:, cs], pv))
                    if j < 5:
                        yield from step(lambda c, p: k.mm(p, S[c][pc], S[c][pt]),
                                        lambda pv, cs, n_: self.evac(SL[ptn][:, cs], pv))
                    yield from step(lambda c, p: k.mm(p, S[c][pn], S[c][yc]),
                                    lambda pv, cs, n_: k.tt(SL[yn][:, cs], pv, SL[yc][:, cs], ALU.add))
                    pc, pt, yc = pn, ptn, yn
                XT = yc
                yield from step(lambda c, p: trn(p, B["Kq"][c]), lambda pv, cs, n_: self.evac(SL[9][:, cs], pv))
                yield from step(lambda c, p: trn(p, B["Kc"][c]), lambda pv, cs, n_: self.evac(SL[10][:, cs], pv))
                yield from step(lambda c, p: trn(p, B["Bc"][c]), lambda pv, cs, n_: self.evac(SL[11][:, cs], pv, neg=True))
                free = [s_ for s_ in (0, 1, 2, 3, 4, 5) if s_ != XT]
                sAV, sUt, sKqt, sRt, sGT = free[0], free[1], free[2], free[3], free[4]
                yield from step(lambda c, p: k.mm(p, S[c][6], self.VB[c]), lambda pv, cs, n_: self.evac(SL[sAV][:, cs], pv))
                yield from step(lambda c, p: k.mm(p, S[c][XT], S[c][sAV]), lambda pv, cs, n_: self.evac(SL[sUt][:, cs], pv))
                yield from step(lambda c, p: k.mm(p, S[c][XT], S[c][9]), lambda pv, cs, n_: self.evac(SL[sKqt][:, cs], pv))
                yield from step(lambda c, p: k.mm(p, S[c][sKqt], S[c][8]),
                                lambda pv, cs, n_: k.tt(SL[sRt][:, cs], pv, bd3["Rq"][:, cs], ALU.add))

                def mm_ot(c, p):
                    k.mm(p, S[c][7], self.VB[c], start=True, stop=False)
                    k.mm(p, S[c][8], S[c][sUt], start=False, stop=True)
                yield from step(mm_ot, lambda pv, cs, n_: self.evac(self.OtT[:, cs], pv))

                def mm_h(c, p):
                    k.mm(p, S[c][10], self.VB[c], start=True, stop=False)
                    k.mm(p, S[c][11], S[c][sUt], start=False, stop=True)
                yield from step(mm_h, lambda pv, cs, n_: self.evac(self.HT[:, cs], pv))
                yield from step(lambda c, p: k.mm(p, S[c][sKqt], S[c][11]),
                                lambda pv, cs, n_: self.evac(SL[sGT][:, cs], pv))
                RtT = [S[c][sRt] for c in CC]
                GT = [S[c][sGT] for c in CC]
            else:
                yield from step(lambda c, p: k.mm(p, B["Kd"][c], B["Rq"][c]),
                                lambda pv, cs, n_: k.tt(SL[0][:, cs], pv, m3(pr.m_upi, n_), ALU.mult))
                yield from step(lambda c, p: trn(p, B["Kc"][c]), lambda pv, cs, n_: self.evac(SL[1][:, cs], pv))
                yield from step(lambda c, p: k.mm(p, S[c][0], self.VB[c]), lambda pv, cs, n_: self.evac(self.OtT[:, cs], pv), w=nv)
                yield from step(lambda c, p: k.mm(p, S[c][1], self.VB[c]), lambda pv, cs, n_: self.evac(self.HT[:, cs], pv), w=nv)
                RtT = [B["Rq"][c] for c in CC]
            osg = self.oseg[sg % 2]
            for c in CC:
                Tc = self.Tst[tcur]
                Tn = self.Tst[1 - tcur]
                Tcb = self.Tsh[tcur]
                Tnb = self.Tsh[1 - tcur]
                for v in range(nvb):
                    p = self.P()[:, 0:128]
                    k.mm(p, Tcb[:, v * 128:(v + 1) * 128], RtT[c], start=True, stop=False)
                    k.mm(p, self.Ot[c][:, v * 128:(v + 1) * 128], Ib, start=False, stop=True)
                    yield
                    if nvb == 1:
                        k.cp(osg[0:64, 0, c * CH:(c + 1) * CH], p[0:64, 0:64], x="dve")
                        k.cp(osg[64:128, 0, c * CH:(c + 1) * CH], p[64:128, 64:128], x="dve")
                    else:
                        k.cp(osg[:, v, c * CH:(c + 1) * CH], p[:, v * CH:(v + 1) * CH], x="act")
                if delta:
                    p = self.P()[:, 0:nv]
                    k.mm(p, GT[c], Tcb, start=True, stop=False)
                    k.mm(p, Ib, self.H[c], start=False, stop=True)
                    yield
                    k.stt(Tn, Tc, self.WC[:, c:c + 1], p, ALU.mult, ALU.add)
                else:
                    k.stt(Tn, Tc, self.WC[:, c:c + 1], self.H[c], ALU.mult, ALU.add)
                if not f32:
                    k.cp(Tnb, Tn, x="act")
                tcur = 1 - tcur
            k.dma("sp", o_dram[:, :, s0:s0 + SEG].rr("i p t -> p i t"), osg)


    def run(self, ops_dram, v_dram, o_dram):
        for _ in self.run_gen(ops_dram, v_dram, o_dram):
            pass


def cla_run_many(clas, jobs):
    pending = list(jobs)
    active = [None] * len(clas)
    while pending or any(a is not None for a in active):
        for i, cla in enumerate(clas):
            if active[i] is None and pending:
                active[i] = cla.run_gen(*pending.pop(0))
            if active[i] is not None:
                try:
                    next(active[i])
                except StopIteration:
                    active[i] = None


RW_DSCALE = 0.606531
RW_GN_EPS = 64e-5


def nat_tiles():
    tl = [(0, TC, 0, TC)]
    for i in range(TL // 512):
        tl.append((TC + i * 512, 512, TC, T))
    return tl


def rwkv_declare(pr):
    pr.inp("rw_muT", [DEPTH, 2, 128, 15])
    pr.inp("rw_w0T", [DEPTH, 2, 128, 4])
    pr.inp("rw_a0T", [DEPTH, 2, 128, 4])
    pr.inp("rw_w_up", [DEPTH, 128, 512])
    pr.inp("rw_a_up", [DEPTH, 128, 512])
    pr.inp("rw_g_up", [DEPTH, 128, 512])
    pr.inp("rw_vecT", [DEPTH, 5, 128, 4])
    pr.scratch("RWop", [NB, 2, 4, 5, 128, T])
    pr.scratch("RWv", [NB, 2, 4, 1, 128, T])
    pr.scratch("RWo", [NB, 2, 4, 1, 128, T])
    pr.scratch("RWg", [NB, 4, 128, T])
    pr.scratch("RWcb", [NB, 4, 128, T])


def rwkv_prep(pr, l):
    k = pr.k
    k.push()
    IN = pr.IN
    mu = [k.sb("rw_mu%d" % i, [128, 15]) for i in range(2)]
    c0 = k.sb("rw_c0", [128, 15])
    for i in range(2):
        k.dma("sp", mu[i], IN["rw_muT"][l, i])
    k.tt(c0, mu[0], mu[1], ALU.add)
    k.ts(c0, c0, -1.0, ALU.mult, 1.0, ALU.add)
    w0 = k.sb("rw_w0", [128, 2, 4])
    a0 = k.sb("rw_a0", [128, 2, 4])
    k.dma("sp", w0, IN["rw_w0T"][l].rr("d p c -> p d c"))
    k.dma("sp", a0, IN["rw_a0T"][l].rr("d p c -> p d c"))
    wup = k.sb("rw_wup", [128, 512])
    aup = k.sb("rw_aup", [128, 512])
    gup = k.sb("rw_gup", [128, 512])
    k.dma("sp", wup, IN["rw_w_up"][l])
    k.dma("sp", aup, IN["rw_a_up"][l])
    k.dma("sp", gup, IN["rw_g_up"][l])
    vec = k.sb("rw_vec", [128, 5, 4])
    k.dma("sp", vec, IN["rw_vecT"][l].rr("i p c -> p i c"))
    omka = k.sb("rw_omka", [128, 4])
    k.ts(omka, vec[:, 1, :], -1.0, ALU.mult, 1.0, ALU.add)
    raw = [k.sb("rw_raw%d" % i, [128, 15, 514]) for i in range(1)]
    sh = k.sb("rw_sh", [128, 15, 512])
    t15 = k.sb("rw_t15", [128, 15, 512])
    TW = k.sb("rw_TW", [128, 512])
    SG = k.sb("rw_SG", [128, 512])
    kkr = k.sb("rw_kkr", [128, 512])
    sq = k.sb("rw_sq", [128, 512])
    nrm = k.sb("rw_nrm", [128, 512])
    kk = k.sb("rw_kk", [128, 512])
    NO = 3
    ost = [[k.sb("rw_o%d_%d" % (j, i), [128, 512]) for i in range(7)] for j in range(NO)]
    A = k.sb("rw_A", [128, 512])
    tA = k.sb("rw_tA", [128, 512])
    sgw = k.sb("rw_sgw", [128, 512])
    cbin = k.sb("rw_cbin", [128, 512])
    pss = [k.ps("rw_ps%d" % i, [128, 512]) for i in range(5)]
    pcb = k.ps("rw_pcb", [128, 512])
    P = pr.S["P"]
    it = 0
    oi = 0
    pi = 0
    for b in range(NB):
        for (t0, n, ra, rb) in nat_tiles():
            rw = raw[0]
            it += 1
            lo = max(ra, t0 - 1)
            hi = min(rb, t0 + n + 1)
            if lo == t0:
                k.memset(rw[:, :, 0:1], 0.0, x="pool")
            if hi == t0 + n:
                k.memset(rw[:, :, n + 1:n + 2], 0.0, x="pool")
            k.dma("sp", rw[:, :, lo - (t0 - 1):hi - (t0 - 1)],
                  P[b, OFF_RW:OFF_RW + 1920, lo:hi].rr("(j p) t -> p j t", p=128))
            k.tt(sh[:, :, :n], rw[:, :, 1:n + 1], c0[:, :, None].bc([128, 15, n]), ALU.mult, x="dve")
            for jj in range(15):
                k.act(t15[:, jj, :n], rw[:, jj, 0:n], AF.Identity, scale=mu[0][:, jj:jj + 1])
            k.tt(sh[:, :, :n], sh[:, :, :n], t15[:, :, :n], ALU.add, x="dve")
            for jj in range(15):
                k.act(t15[:, jj, :n], rw[:, jj, 2:n + 2], AF.Identity, scale=mu[1][:, jj:jj + 1])
            k.tt(sh[:, :, :n], sh[:, :, :n], t15[:, :, :n], ALU.add, x="dve")
            k.act(TW[:, :n], sh[:, 12, :n], AF.Tanh)
            k.act(SG[:, :n], sh[:, 14, :n], AF.Sigmoid)
            AD = sh[:, 13, :]
            m0 = ra + rb - t0 - n
            for p in range(4):
                rS, kS, vS = sh[:, p, :], sh[:, 4 + p, :], sh[:, 8 + p, :]
                cs = slice(p * 128, (p + 1) * 128)
                k.act(kkr[:, :n], kS[:, :n], AF.Identity, scale=vec[:, 0, p:p + 1])
                k.act(sq[:, :n], kkr[:, :n], AF.Square)
                ps = pss[pi % 5]
                pi += 1
                k.mm(ps[:, :n], pr.ones_bd, sq[:, :n])
                k.act(nrm[:, :n], ps[:, :n], AF.Sqrt)
                k.ts(nrm[:, :n], nrm[:, :n], 1e-12, ALU.max)
                k.recip(nrm[:, :n], nrm[:, :n])
                k.tt(kk[:, :n], kkr[:, :n], nrm[:, :n], ALU.mult)
                ps = pss[pi % 5]
                pi += 1
                k.mm(ps[:, :n], gup[:, cs], SG[:, :n])
                o = ost[oi % NO]
                oi += 1
                k.cp(o[6][:, :n], ps[:, :n], x="act")
                k.dma("pool", pr.S["RWg"][b, p, :, t0:t0 + n], o[6][:, :n])
                for d in range(2):
                    ds = slice(d * 64, (d + 1) * 64)
                    if d == 1:
                        o = ost[oi % NO]
                        oi += 1

                    def ov(tile):
                        return tile[:, :n] if d == 0 else tile[:, :n][:, ::-1]
                    ps = pss[pi % 5]
                    pi += 1
                    k.mm(ps[:, :n], wup[ds, cs], TW[ds, :n])
                    k.act(sgw[:, :n], ps[:, :n], AF.Sigmoid, bias=w0[:, d, p:p + 1], scale=1.0)
                    k.ts(ov(o[2]), sgw[:, :n], -RW_DSCALE, ALU.mult)
                    ps = pss[pi % 5]
                    pi += 1
                    k.mm(ps[:, :n], aup[ds, cs], AD[ds, :n])
                    k.act(A[:, :n], ps[:, :n], AF.Sigmoid, bias=a0[:, d, p:p + 1], scale=1.0)
                    k.ts(tA[:, :n], A[:, :n], vec[:, 1, p:p + 1], ALU.mult, omka[:, p:p + 1], ALU.add)
                    k.tt(ov(o[1]), kS[:, :n], tA[:, :n], ALU.mult)
                    k.tt(ov(o[4]), A[:, :n], kk[:, :n], ALU.mult, x="dve")
                    k.stt(cbin[:, :n], rS[:, :n], vec[:, 2, p:p + 1], o[1][:, :n] if d == 0 else o[1][:, :n][:, ::-1],
                          ALU.mult, ALU.mult)
                    k.mm(pcb[:, :n], pr.ones_bd, cbin[:, :n], start=(d == 0), stop=(d == 1))
                    k.cp(ov(o[0]), rS[:, :n], x="act")
                    k.cp(ov(o[3]), kk[:, :n], x="act")
                    k.cp(ov(o[5]), vS[:, :n], x="act")
                    s0 = t0 if d == 0 else m0
                    for i in range(5):
                        k.dma("sp" if i % 2 == 0 else "pool", pr.S["RWop"][b, d, p, i, :, s0:s0 + n], o[i][:, :n])
                    k.dma("sp", pr.S["RWv"][b, d, p, 0, :, s0:s0 + n], o[5][:, :n])
                k.cp(cbin[:, :n], pcb[:, :n], x="dve")
                k.dma("pool", pr.S["RWcb"][b, p, :, t0:t0 + n], cbin[:, :n])
    k.pop()


def rwkv_scan(pr, l):
    k = pr.k
    k.push()
    clas = [CLA(pr, True, 1, nbanks=4) for _ in range(2)]
    jobs = [(pr.S["RWop"][b, d, p], pr.S["RWv"][b, d, p], pr.S["RWo"][b, d, p])
            for b in range(NB) for d in range(2) for p in range(4)]
    cla_run_many(clas, jobs)
    k.pop()


def rwkv_s5_scan(pr, l):
    k = pr.k
    k.push()
    clas = [CLA(pr, True, 1, nbanks=3, nbuf=1)]
    jobs = [(pr.S["RWop"][b, d, p], pr.S["RWv"][b, d, p], pr.S["RWo"][b, d, p])
            for b in range(NB) for d in range(2) for p in range(4)]
    s5g = s5_scan_gen(pr, l, npy=1, nwork=1)
    pending = list(jobs)
    active = None
    s5_alive = True
    n = 0
    while pending or active is not None or s5_alive:
        if active is None and pending:
            active = clas[0].run_gen(*pending.pop(0))
        if active is not None:
            try:
                next(active)
            except StopIteration:
                active = None
        n += 1
        if s5_alive and (n % 3 == 0 or (active is None and not pending)):
            try:
                next(s5g)
            except StopIteration:
                s5_alive = False
    k.pop()


def rwkv_readout(pr, l, ybr):
    k = pr.k
    k.push()
    vec = k.sb("rwr_vec", [128, 5, 4])
    k.dma("sp", vec, pr.IN["rw_vecT"][l].rr("i p c -> p i c"))
    NBUF = 2
    o0 = [k.sb("rwr_o0_%d" % i, [128, 512]) for i in range(NBUF)]
    o1 = [k.sb("rwr_o1_%d" % i, [128, 512]) for i in range(NBUF)]
    vv = [k.sb("rwr_v_%d" % i, [128, 512]) for i in range(NBUF)]
    gg = [k.sb("rwr_g_%d" % i, [128, 512]) for i in range(NBUF)]
    cb = [k.sb("rwr_cb_%d" % i, [128, 512]) for i in range(NBUF)]
    yo = [k.sb("rwr_y_%d" % i, [128, 512]) for i in range(NBUF)]
    sq = k.sb("rwr_sq", [128, 512])
    mean = k.sb("rwr_mean", [128, 512])
    var = k.sb("rwr_var", [128, 512])
    t1 = k.sb("rwr_t1", [128, 512])
    ps1 = k.ps("rwr_ps1", [128, 512])
    ps2 = k.ps("rwr_ps2", [128, 512])
    it = 0
    for b in range(NB):
        for p in range(4):
            for (t0, n, ra, rb) in nat_tiles():
                i = it % NBUF
                it += 1
                m0 = ra + rb - t0 - n
                k.dma("sp", o0[i][:, :n], pr.S["RWo"][b, 0, p, 0, :, t0:t0 + n])
                k.dma("pool", o1[i][:, :n], pr.S["RWo"][b, 1, p, 0, :, m0:m0 + n])
                k.dma("sp", vv[i][:, :n], pr.S["RWv"][b, 0, p, 0, :, t0:t0 + n])
                k.dma("pool", gg[i][:, :n], pr.S["RWg"][b, p, :, t0:t0 + n])
                k.dma("sp", cb[i][:, :n], pr.S["RWcb"][b, p, :, t0:t0 + n])
                o = o0[i]
                k.tt(o[:, :n], o[:, :n], o1[i][:, :n][:, ::-1], ALU.add)
                k.mm(ps1[:, :n], pr.ones_bd, o[:, :n])
                k.act(sq[:, :n], o[:, :n], AF.Square)
                k.mm(ps2[:, :n], pr.ones_bd, sq[:, :n])
                k.ts(mean[:, :n], ps1[:, :n], 1.0 / 64, ALU.mult)
                k.tt(t1[:, :n], mean[:, :n], mean[:, :n], ALU.mult, x="pool")
                k.stt(var[:, :n], ps2[:, :n], 1.0 / 64, t1[:, :n], ALU.mult, ALU.subtract)
                k.ts(var[:, :n], var[:, :n], RW_GN_EPS, ALU.add)
                k.act(var[:, :n], var[:, :n], AF.Sqrt)
                k.recip(var[:, :n], var[:, :n])
                k.tt(t1[:, :n], o[:, :n], mean[:, :n], ALU.subtract, x="pool")
                k.stt(t1[:, :n], t1[:, :n], vec[:, 3, p:p + 1], var[:, :n], ALU.mult, ALU.mult)
                k.tt(sq[:, :n], cb[i][:, :n], vv[i][:, :n], ALU.mult, x="pool")
                k.stt(t1[:, :n], t1[:, :n], vec[:, 4, p:p + 1], sq[:, :n], ALU.add, ALU.add)
                k.tt(yo[i][:, :n], t1[:, :n], gg[i][:, :n], ALU.mult)
                k.dma("sp", ybr[b, p * 128:(p + 1) * 128, t0:t0 + n], yo[i][:, :n])
    k.pop()


def col_tiles():
    return [(0, 512), (512, 512), (1024, 512), (1536, 512), (2048, 256)]


def seq_copy(k, dst, src, colmajor, rev, x, scale=None):
    def emit(o, i):
        if scale is not None:
            k.act(o, i, AF.Copy, scale=scale)
        else:
            k.cp(o, i, x=x)
    c_src = src[:, 0:TC]
    emit(dst[:, 0:TC], c_src[:, ::-1] if rev else c_src)
    if colmajor:
        l_src = src[:, TC:T].rr("p (r c) -> p c r", c=64)
        if rev:
            l_src = l_src[:, ::-1, ::-1]
        emit(dst[:, TC:T].rr("p (c r) -> p c r", r=32), l_src)
    else:
        l_src = src[:, TC:T]
        emit(dst[:, TC:T], l_src[:, ::-1] if rev else l_src)


def gla_declare(pr):
    pr.inp("gla_gate_up", [DEPTH, 2, 16, 256])
    pr.inp("gla_gate_bT", [DEPTH, 2, 128, 2])
    pr.inp("gla_norm_gT", [DEPTH, 128, 1])
    pr.scratch("GLop", [NB, 2, 2, 3, 128, T])
    pr.scratch("GLv", [NB, 2, 2, 2, 128, T])
    pr.scratch("GLo", [NB, 2, 2, 2, 128, T])


def gla_prep(pr, l):
    k = pr.k
    k.push()
    IN = pr.IN
    P = pr.S["P"]
    gup = [k.sb("gl_gup%d" % d, [16, 256]) for d in range(2)]
    nb = k.sb("gl_nb", [128, 2, 2])
    for d in range(2):
        k.dma("sp", gup[d], IN["gla_gate_up"][l, d])
    k.dma("sp", nb, IN["gla_gate_bT"][l].rr("d p c -> p d c"))
    k.ts(nb, nb, -1.0, ALU.mult)
    gd = [k.sb("gl_gd%d" % d, [16, T]) for d in range(2)]
    nat = [k.sb("gl_nat%d" % i, [128, T]) for i in range(2)]
    sq_ = [k.sb("gl_seq%d" % i, [128, T]) for i in range(3)]
    lwn = k.sb("gl_lwn", [128, T])
    ps = [k.ps("gl_ps%d" % i, [128, 512]) for i in range(2)]
    ni = 0
    si = 0
    for b in range(NB):
        for d in range(2):
            k.dma("sp", gd[d], P[b, OFF_GLA + 1536 + d * 16:OFF_GLA + 1536 + (d + 1) * 16, :])
        for p in range(2):
            for i, (row0, scale) in enumerate(((p * 128, 0.125), (256 + p * 128, None))):
                nt = nat[ni % 2]
                ni += 1
                k.dma("sp", nt, P[b, OFF_GLA + row0:OFF_GLA + row0 + 128, :])
                for d in range(2):
                    st = sq_[si % 3]
                    si += 1
                    seq_copy(k, st, nt, True, d == 1, "act", scale=scale)
                    k.dma("pool", pr.S["GLop"][b, d, p, i], st)
            for d in range(2):
                for j, (c0, n) in enumerate(col_tiles()):
                    pp = ps[j % 2]
                    k.mm(pp[:, :n], gup[d][:, p * 128:(p + 1) * 128], gd[d][:, c0:c0 + n])
                    k.act(lwn[:, c0:c0 + n], pp[:, :n], AF.Exp, bias=nb[:, d, p:p + 1], scale=-1.0)
                k.act(lwn, lwn, AF.Ln, bias=1.0, scale=1.0)
                st = sq_[si % 3]
                si += 1
                seq_copy(k, st, lwn, True, d == 1, "pool", scale=-1.0 / 16.0)
                k.dma("pool", pr.S["GLop"][b, d, p, 2], st)
            for vb in range(2):
                h = 2 * p + vb
                nt = nat[ni % 2]
                ni += 1
                k.dma("sp", nt, P[b, OFF_GLA + 512 + h * 128:OFF_GLA + 512 + (h + 1) * 128, :])
                for d in range(2):
                    st = sq_[si % 3]
                    si += 1
                    seq_copy(k, st, nt, True, d == 1, "act" if d == 0 else "dve")
                    k.dma("pool", pr.S["GLv"][b, d, p, vb], st)
    k.pop()


def gla_scan(pr, l):
    k = pr.k
    k.push()
    clas = [CLA(pr, False, 2, nbanks=4) for _ in range(2)]
    jobs = [(pr.S["GLop"][b, d, p], pr.S["GLv"][b, d, p], pr.S["GLo"][b, d, p])
            for b in range(NB) for d in range(2) for p in range(2)]
    cla_run_many(clas, jobs)
    k.pop()


def gla_readout(pr, l, ybr):
    k = pr.k
    k.push()
    P = pr.S["P"]
    ng = k.sb("glr_ng", [128, 1])
    k.dma("sp", ng, pr.IN["gla_norm_gT"][l])
    o0 = k.sb("glr_o0", [128, T])
    o1 = k.sb("glr_o1", [128, T])
    gn = k.sb("glr_gn", [128, T])
    gs = k.sb("glr_gs", [128, T])
    sq = k.sb("glr_sq", [128, T])
    rs = k.sb("glr_rs", [128, T])
    yn = k.sb("glr_yn", [128, T])
    ps = [k.ps("glr_ps%d" % i, [128, 512]) for i in range(2)]
    for b in range(NB):
        for h in range(4):
            p, vb = h // 2, h % 2
            k.dma("sp", o0, pr.S["GLo"][b, 0, p, vb])
            k.dma("pool", o1, pr.S["GLo"][b, 1, p, vb])
            k.dma("sp", gn, P[b, OFF_GLA + 1024 + h * 128:OFF_GLA + 1024 + (h + 1) * 128, :])
            k.tt(o0[:, 0:TC], o0[:, 0:TC], o1[:, 0:TC][:, ::-1], ALU.add)
            k.tt(o0[:, TC:T], o0[:, TC:T], o1[:, TC:T][:, ::-1], ALU.add)
            k.act(sq, o0, AF.Square)
            for j, (c0, n) in enumerate(col_tiles()):
                pp = ps[j % 2]
                k.mm(pp[:, :n], pr.ones, sq[:, c0:c0 + n])
                k.ts(rs[:, c0:c0 + n], pp[:, :n], 1.0 / 128, ALU.mult, RMS_EPS, ALU.add)
            k.act(rs, rs, AF.Sqrt)
            k.recip(rs, rs)
            k.stt(o0, o0, ng[:, 0:1], rs, ALU.mult, ALU.mult)
            k.act(gn, gn, AF.Silu)
            seq_copy(k, gs, gn, True, False, "pool")
            k.tt(o0, o0, gs, ALU.mult)
            k.cp(yn[:, 0:TC], o0[:, 0:TC], x="act")
            k.cp(yn[:, TC:T].rr("p (r c) -> p c r", c=64), o0[:, TC:T].rr("p (c r) -> p c r", r=32), x="pool")
            k.dma("sp", ybr[b, h * 128:(h + 1) * 128, :], yn)
    k.pop()


MAGIC = 12582912.0
TWO_PI_LO = 6.2831850


def sincos_turns(k, out_s, out_c, turns, t1, t2):
    for (dst, off) in ((out_s, 0.0), (out_c, 0.25)):
        if off != 0.0:
            k.ts(t2, turns, off, ALU.add)
            src = t2
        else:
            src = turns
        k.ts(t1, src, MAGIC, ALU.add)
        k.ts(t1, t1, -MAGIC, ALU.add)
        k.tt(t1, src, t1, ALU.subtract)
        k.ts(dst, t1, 0.5, ALU.is_gt)
        k.tt(t1, t1, dst, ALU.subtract)
        k.ts(dst, t1, -0.5, ALU.is_lt)
        k.tt(t1, t1, dst, ALU.add)
        k.act(dst, t1, AF.Sin, scale=TWO_PI_LO)


def s5_declare(pr):
    pr.inp("tau", [128, 513])
    pr.inp("s5_aT", [DEPTH, 2, 2, 128, 16])
    pr.inp("s5_ldtT", [DEPTH, 2, 128, 16])
    pr.inp("s5_bT", [DEPTH, 2, 128, 16, 16])
    pr.inp("s5_cT", [DEPTH, 2, 128, 16, 32])
    pr.inp("s5_dT", [DEPTH, 128, 4])
    pr.inp("s5_glu_w", [DEPTH, 512, 512])
    pr.inp("s5_glu_bT", [DEPTH, 128, 4])
    pr.scratch("S5y", [NB, MIXW, T])


def s5_scan(pr, l):
    k = pr.k
    k.push()
    for _ in s5_scan_gen(pr, l):
        pass
    k.pop()


def s5_scan_gen(pr, l, npy=2, nwork=2):
    k = pr.k
    IN = pr.IN
    P = pr.S["P"]
    I = pr.ident
    tau = k.sb("s5_tau", [128, 513])
    k.dma("sp", tau, IN["tau"])
    cbd32 = [k.sb("s5_c32_%d" % x, [128, 16, 32]) for x in range(2)]
    cbd = [k.sb("s5_c%d" % x, [128, 16, 32], BF16) for x in range(2)]
    for x in range(2):
        k.dma("sp", cbd32[x], IN["s5_cT"][l, x])
    k.cp(cbd[0], cbd32[0], x="act")
    k.ts(cbd[1], cbd32[1], -1.0, ALU.mult)
    bt = [k.sb("s5_b%d" % x, [128, 16, 16]) for x in range(2)]
    for x in range(2):
        k.dma("sp", bt[x], IN["s5_bT"][l, x])
    names = ["are", "aim", "dt", "mag", "tht", "s", "c", "nr", "ni", "den", "fre", "fim", "t1", "t2", "t3"]
    drv = [[k.sb("s5_drv%d%d" % (d, x), [32, 16, 128], BF16) for x in range(2)] for d in range(2)]
    rho = [k.sb("s5_rho%d" % d, [128, 16]) for d in range(2)]
    tht = [k.sb("s5_tht%d" % d, [128, 16]) for d in range(2)]
    cth = [k.sb("s5_cth%d" % d, [128, 16]) for d in range(2)]
    sth = [k.sb("s5_sth%d" % d, [128, 16]) for d in range(2)]
    pAs = [k.ps("s5_pA%d" % q, [128, 512]) for q in range(2)]
    pBs = [k.ps("s5_pB%d" % q, [128, 512]) for q in range(2)]
    pYs = [k.ps("s5_pY%d" % q, [128, 512]) for q in range(npy)]
    pst = pYs[0]
    fbd = [k.sb("s5_fbd%d" % x, [128, 16, 2, 16]) for x in range(2)]
    for x in range(2):
        k.memset(fbd[x], 0.0)
    ft = [k.sb("s5_ft%d" % x, [128, 16, 16]) for x in range(2)]
    ftmp = k.sb("s5_ftmp", [128, 16, 16])
    w = {n: k.sb("s5w_" + n, [128, 16]) for n in names}
    for d in range(2):
        k.dma("sp", w["are"], IN["s5_aT"][l, d, 0])
        k.dma("sp", w["aim"], IN["s5_aT"][l, d, 1])
        k.dma("sp", w["dt"], IN["s5_ldtT"][l, d])
        k.act(w["dt"], w["dt"], AF.Exp)
        k.tt(w["t1"], w["are"], w["dt"], ALU.mult)
        k.act(rho[d], w["t1"], AF.Exp)
        k.tt(w["t1"], w["aim"], w["dt"], ALU.mult)
        k.ts(tht[d], w["t1"], 1.0 / (2.0 * math.pi), ALU.mult)
        sincos_turns(k, sth[d], cth[d], tht[d], w["t2"], w["t3"])
        k.tt(w["nr"], rho[d], cth[d], ALU.mult)
        k.ts(w["nr"], w["nr"], -1.0, ALU.add)
        k.tt(w["ni"], rho[d], sth[d], ALU.mult)
        k.tt(w["den"], w["are"], w["are"], ALU.mult)
        k.tt(w["t1"], w["aim"], w["aim"], ALU.mult)
        k.tt(w["den"], w["den"], w["t1"], ALU.add)
        k.recip(w["den"], w["den"])
        k.tt(w["fre"], w["nr"], w["are"], ALU.mult)
        k.tt(w["t1"], w["ni"], w["aim"], ALU.mult)
        k.tt(w["fre"], w["fre"], w["t1"], ALU.add)
        k.tt(w["fre"], w["fre"], w["den"], ALU.mult)
        k.tt(w["fim"], w["ni"], w["are"], ALU.mult)
        k.tt(w["t1"], w["nr"], w["aim"], ALU.mult)
        k.tt(w["fim"], w["fim"], w["t1"], ALU.subtract)
        k.tt(w["fim"], w["fim"], w["den"], ALU.mult)
        fre3 = w["fre"][:, :, None].bc([128, 16, 16])
        fim3 = w["fim"][:, :, None].bc([128, 16, 16])
        k.tt(ft[0], bt[0], fre3, ALU.mult)
        k.tt(ftmp, bt[1], fim3, ALU.mult)
        k.tt(ft[0], ft[0], ftmp, ALU.subtract)
        k.tt(ft[1], bt[1], fre3, ALU.mult)
        k.tt(ftmp, bt[0], fim3, ALU.mult)
        k.tt(ft[1], ft[1], ftmp, ALU.add)
        for x in range(2):
            k.cp(fbd[x][0:64, :, 0, :], ft[x][0:64], x="dve")
            k.cp(fbd[x][64:128, :, 1, :], ft[x][64:128], x="dve")
            for j0 in range(0, 16, 4):
                for jj in range(4):
                    j = j0 + jj
                    k.tr(pst[0:32, jj * 128:(jj + 1) * 128], fbd[x][:, j, :, :].rr("p a b -> p (a b)"), I)
                k.cp(drv[d][x][:, j0:j0 + 4, :], pst[0:32, :].rr("p (a b) -> p a b", a=4), x="act")
        yield
    Ct = k.sb("s5_Ct", [128, 513])
    St = k.sb("s5_St", [128, 513])
    turns = k.sb("s5_turns", [128, 513])
    ta = k.sb("s5_ta", [128, 513])
    tb = k.sb("s5_tb", [128, 513])
    un32 = [k.sb("s5_un32_%d" % b, [32, T]) for b in range(NB)]
    un = [k.sb("s5_un%d" % b, [32, T], BF16) for b in range(NB)]
    ur = [k.sb("s5_ur%d" % b, [32, T], BF16) for b in range(NB)]
    yacc = [k.sb("s5_yacc%d" % b, [32, T]) for b in range(NB)]
    W = []
    for q in range(nwork):
        W.append({n_: k.sb("s5_%s%d" % (n_, q), [128, 512], BF16 if n_ in ("hr", "hi") else F32)
                  for n_ in ("m1", "m2", "m3", "m4", "xmr", "xmi", "hr", "hi")})
    ini = k.sb("s5_ini", [128, 2])
    it1 = k.sb("s5_it1", [128, 2])
    G2 = [(k.sb("s5_gr%d" % q, [128, 512]), k.sb("s5_gi%d" % q, [128, 512])) for q in range(2)]
    ytmp = k.sb("s5_ytmp", [32, 512])
    tcount = 0
    for j in range(16):
        for b in range(NB):
            k.dma("sp", un32[b], P[b, OFF_S5 + j * 32:OFF_S5 + (j + 1) * 32, :])
            k.cp(un[b], un32[b], x="act")
            seq_copy(k, ur[b], un32[b], False, True, "act")
        for d in range(2):
            k.ts(turns, tau, tht[d][:, j:j + 1], ALU.mult)
            sincos_turns(k, St, Ct, turns, ta, tb)
            yield
            for b in range(NB):
                u = un[b] if d == 0 else ur[b]
                for ti, (s0, n, _ra, _rb) in enumerate(nat_tiles()):
                    q = tcount % 2
                    tcount += 1
                    w_ = W[q % nwork]
                    pA, pB, pY = pAs[q], pBs[q], pYs[q % npy]
                    m1, m2, m3, m4, xmr, xmi, hr, hi = (w_[n_] for n_ in ("m1", "m2", "m3", "m4", "xmr", "xmi", "hr", "hi"))
                    gr, gi = G2[q]
                    k.mm(pA[:, :n], drv[d][0][:, j, :], u[:, s0:s0 + n])
                    k.mm(pB[:, :n], drv[d][1][:, j, :], u[:, s0:s0 + n])
                    k.tt(m1[:, :n], pA[:, :n], Ct[:, :n], ALU.mult, x="dve")
                    k.tt(m2[:, :n], pB[:, :n], St[:, :n], ALU.mult, x="dve")
                    k.tt(xmr[:, :n], m1[:, :n], m2[:, :n], ALU.add, x="dve")
                    k.tt(m3[:, :n], pB[:, :n], Ct[:, :n], ALU.mult, x="dve")
                    k.tt(m4[:, :n], pA[:, :n], St[:, :n], ALU.mult, x="dve")
                    k.tt(xmi[:, :n], m3[:, :n], m4[:, :n], ALU.subtract, x="dve")
                    yield
                    if ti == 0:
                        k.memset(ini, 0.0)
                    else:
                        grp, gip = G2[1 - q]
                        cc, ss = Ct[:, pn:pn + 1], St[:, pn:pn + 1]
                        k.ts(it1[:, 0:1], gip[:, pn - 1:pn], ss, ALU.mult)
                        k.stt(ini[:, 0:1], grp[:, pn - 1:pn], cc, it1[:, 0:1], ALU.mult, ALU.subtract)
                        k.ts(it1[:, 1:2], gip[:, pn - 1:pn], cc, ALU.mult)
                        k.stt(ini[:, 1:2], grp[:, pn - 1:pn], ss, it1[:, 1:2], ALU.mult, ALU.add)
                    k.scan(gr[:, :n], rho[d][:, j:j + 1].bc([128, n]), xmr[:, :n], ini[:, 0:1])
                    k.scan(gi[:, :n], rho[d][:, j:j + 1].bc([128, n]), xmi[:, :n], ini[:, 1:2])
                    yield
                    k.tt(m1[:, :n], gr[:, :n], Ct[:, :n], ALU.mult, x="dve")
                    k.tt(m2[:, :n], gi[:, :n], St[:, :n], ALU.mult, x="dve")
                    k.tt(hr[:, :n], m1[:, :n], m2[:, :n], ALU.subtract, x="dve")
                    k.tt(m3[:, :n], gr[:, :n], St[:, :n], ALU.mult, x="dve")
                    k.tt(m4[:, :n], gi[:, :n], Ct[:, :n], ALU.mult, x="dve")
                    k.tt(hi[:, :n], m3[:, :n], m4[:, :n], ALU.add, x="dve")
                    yield
                    pn = n
                    k.mm(pY[0:32, :n], cbd[0][:, j, :], hr[:, :n], start=True, stop=False)
                    k.mm(pY[0:32, :n], cbd[1][:, j, :], hi[:, :n], start=False, stop=True)
                    if d == 0:
                        k.cp(yacc[b][:, s0:s0 + n], pY[0:32, :n], x="act")
                    else:
                        if s0 < TC:
                            dst = yacc[b][:, 0:TC][:, ::-1]
                        else:
                            dst = yacc[b][:, TC:T][:, ::-1][:, s0 - TC:s0 - TC + n]
                        k.cp(ytmp[:, :n], pY[0:32, :n], x="act")
                        k.tt(dst, dst, ytmp[:, :n], ALU.add, x="pool")
        for b in range(NB):
            k.dma("sp", pr.S["S5y"][b, j * 32:(j + 1) * 32, :], yacc[b])
        yield


def s5_out(pr, l, ybr):
    k = pr.k
    k.push()
    IN = pr.IN
    P = pr.S["P"]
    dT = k.sb("s5o_d", [128, 4])
    gb = k.sb("s5o_gb", [128, 4])
    gw = k.sb("s5o_gw", [128, 4, 512])
    k.dma("sp", dT, IN["s5_dT"][l])
    k.dma("sp", gb, IN["s5_glu_bT"][l])
    k.dma("sp", gw, IN["s5_glu_w"][l].rr("(kc p) n -> p kc n", p=128))
    yt = [k.sb("s5o_y%d" % i, [128, 4, 512]) for i in range(2)]
    ut = [k.sb("s5o_u%d" % i, [128, 4, 512]) for i in range(2)]
    z = k.sb("s5o_z", [128, 4, 512])
    t1 = k.sb("s5o_t1", [128, 4, 512])
    gl = k.sb("s5o_gl", [128, 4, 512])
    ot = [k.sb("s5o_o%d" % i, [128, 512]) for i in range(2)]
    ps = [k.ps("s5o_ps%d" % i, [128, 512]) for i in range(2)]
    it = 0
    oi = 0
    for b in range(NB):
        for (t0, n, ra, rb) in nat_tiles():
            y = yt[it % 2]
            u = ut[it % 2]
            it += 1
            k.dma("sp", y[:, :, :n], pr.S["S5y"][b, :, t0:t0 + n].rr("(c p) t -> p c t", p=128))
            k.dma("pool", u[:, :, :n], P[b, OFF_S5:OFF_S5 + MIXW, t0:t0 + n].rr("(c p) t -> p c t", p=128))
            for c in range(4):
                k.stt(z[:, c, :n], u[:, c, :n], dT[:, c:c + 1], y[:, c, :n], ALU.mult, ALU.add)
            k.tt(t1[:, :, :n], z[:, :, :n], z[:, :, :n], ALU.mult, x="pool")
            k.ts(t1[:, :, :n], t1[:, :, :n], 0.044715, ALU.mult, 1.0, ALU.add)
            k.tt(t1[:, :, :n], t1[:, :, :n], z[:, :, :n], ALU.mult, x="pool")
            k.act(t1[:, :, :n], t1[:, :, :n], AF.Sigmoid, scale=2.0 * math.sqrt(2.0 / math.pi))
            k.tt(gl[:, :, :n], z[:, :, :n], t1[:, :, :n], ALU.mult)
            for oc in range(4):
                pp = ps[oi % 2]
                o = ot[oi % 2]
                oi += 1
                for kc in range(4):
                    k.mm(pp[:, :n], gw[:, kc, oc * 128:(oc + 1) * 128], gl[:, kc, :n], start=(kc == 0), stop=(kc == 3))
                k.act(o[:, :n], pp[:, :n], AF.Sigmoid, bias=gb[:, oc:oc + 1], scale=1.0)
                k.tt(o[:, :n], o[:, :n], gl[:, oc, :n], ALU.mult)
                k.dma("sp", ybr[b, oc * 128:(oc + 1) * 128, t0:t0 + n], o[:, :n])
    k.pop()


def sin_turns(k, dst, turns, t1):
    k.ts(t1, turns, MAGIC, ALU.add)
    k.ts(t1, t1, -MAGIC, ALU.add)
    k.tt(t1, turns, t1, ALU.subtract)
    k.ts(dst, t1, 0.5, ALU.is_gt)
    k.tt(t1, t1, dst, ALU.subtract)
    k.ts(dst, t1, -0.5, ALU.is_lt)
    k.tt(t1, t1, dst, ALU.add)
    k.act(dst, t1, AF.Sin, scale=TWO_PI_LO)


def hyena_declare(pr):
    for L in (TL, TC):
        pr.inp("hy_zT%d" % L, [33, L])
        pr.inp("hy_win%d" % L, [L, MIXW])
        pr.inp("dft_c%d" % L, [L, L])
        pr.inp("dft_s%d" % L, [L, L])
        pr.inp("idft_c%d" % L, [L, L])
        pr.inp("idft_s%d" % L, [L, L])
    pr.inp("hy_cwT", [DEPTH, 3, 128, 12])
    pr.inp("hy_cbT", [DEPTH, 128, 12])
    pr.inp("hy_biasT", [DEPTH, 128, 4])
    pr.inp("hy_f_w1", [DEPTH, 33, 64])
    pr.inp("hy_f_b1T", [DEPTH, 64, 1])
    pr.inp("hy_f_w2", [DEPTH, 64, 64])
    pr.inp("hy_f_b2T", [DEPTH, 64, 1])
    pr.inp("hy_f_w3", [DEPTH, 64, 1024])
    pr.inp("hy_f_b3", [DEPTH, 1, 1024])
    pr.scratch("HYx0", [NB, MIXW, TL])
    pr.scratch("HYz", [NB, MIXW, TL])
    pr.scratch("HYY", [NB, 2, TL, MIXW])


def hyena_stage(pr, l, L, toff, ybr):
    k = pr.k
    IN = pr.IN
    P = pr.S["P"]
    I = pr.ident
    NT = L // 128
    TW = min(L, 512)
    NTT = L // TW
    sfx = "%d" % L
    k.push()
    KC = k.sb("hy_KC", [128, NT, MIXW])
    KS = k.sb("hy_KS", [128, NT, MIXW])
    tabs_cur = [None]
    pacc = [k.ps("hy_pacc%d" % i, [128, 512]) for i in range(4)]
    pw = [k.ps("hy_pw%d" % i, [128, 512]) for i in range(2)]
    tabi = [0]

    def load_tabs(fc):
        tb = tabs_cur[0][tabi[0] % 3]
        tabi[0] += 1
        k.dma("sp", tb[0], IN["dft_c" + sfx][:, fc * 128:(fc + 1) * 128].rr("(tc p) f -> p tc f", p=128))
        k.dma("sp", tb[1], IN["dft_s" + sfx][:, fc * 128:(fc + 1) * 128].rr("(tc p) f -> p tc f", p=128))
        return tb

    k.push()
    tabs_cur[0] = [[k.sb("hy_tabA%d%d" % (i, x), [128, NT, 128]) for x in range(2)] for i in range(3)]
    fw1 = k.sb("hy_fw1", [33, 64])
    fw2 = k.sb("hy_fw2", [64, 64])
    fw3 = k.sb("hy_fw3", [64, 1024])
    fb = k.sb("hy_fb", [64, 2])
    fb3 = k.sb("hy_fb3", [1, 1024])
    h2 = k.sb("hy_h2", [64, L])
    k.push()
    zT = k.sb("hy_zT", [33, L])
    k.dma("sp", fw1, IN["hy_f_w1"][l])
    k.dma("sp", fw2, IN["hy_f_w2"][l])
    k.dma("sp", fw3, IN["hy_f_w3"][l])
    k.dma("sp", fb[:, 0:1], IN["hy_f_b1T"][l])
    k.dma("sp", fb[:, 1:2], IN["hy_f_b2T"][l])
    k.dma("sp", fb3, IN["hy_f_b3"][l])
    k.dma("sp", zT, IN["hy_zT" + sfx])
    h1 = k.sb("hy_h1", [64, L])
    tu = k.sb("hy_tu", [64, 512])
    tv = k.sb("hy_tv", [64, 512])
    for (src, w, bcol, dst) in ((zT, fw1, 0, h1), (h1, fw2, 1, h2)):
        for i in range(NTT):
            pp = pw[i % 2]
            k.mm(pp[0:64, :TW], w, src[:, i * TW:(i + 1) * TW])
            k.ts(tu[:, :TW], pp[0:64, :TW], fb[:, bcol:bcol + 1], ALU.add, 1.0 / (2.0 * math.pi), ALU.mult)
            sin_turns(k, dst[:, i * TW:(i + 1) * TW], tu[:, :TW], tv[:, :TW])
    k.pop()
    hsum = k.sb("hy_hsum", [128, NT, MIXW])
    hdif = k.sb("hy_hdif", [128, NT, MIXW])
    win = [k.sb("hy_win%d" % i, [128, MIXW]) for i in range(2)]
    hf = k.sb("hy_hf", [128, MIXW])
    hb = k.sb("hy_hb", [128, MIXW])
    for tc in range(NT):
        wt = win[tc % 2]
        k.dma("sp", wt, IN["hy_win" + sfx][tc * 128:(tc + 1) * 128, :])
        for half, dst in ((0, hf), (1, hb)):
            pp = pw[half]
            k.mm(pp, h2[:, tc * 128:(tc + 1) * 128], fw3[:, half * 512:(half + 1) * 512], start=True, stop=False)
            k.mm(pp, pr.ones[0:1, :], fb3[:, half * 512:(half + 1) * 512], start=False, stop=True)
            k.tt(dst, pp, wt, ALU.mult)
        k.tt(hdif[:, tc, :], hb, hf, ALU.subtract, x="pool")
        if tc == 0:
            k.memset(hb[0:1, :], 0.0)
        k.tt(hsum[:, tc, :], hf, hb, ALU.add, x="pool")
    for fc in range(NT):
        tb = load_tabs(fc)
        for x, src, dst in ((0, hsum, KC), (1, hdif, KS)):
            pp = pw[x]
            for tc in range(NT):
                k.mm(pp, tb[x][:, tc, :], src[:, tc, :], start=(tc == 0), stop=(tc == NT - 1))
            k.cp(dst[:, fc, :], pp, x=("act" if x == 0 else "dve"))
    k.pop()
    cw = k.sb("hy_cw", [128, 3, 12])
    cb = k.sb("hy_cb", [128, 12])
    hbias = k.sb("hy_bias", [128, 4])
    k.dma("sp", cw, IN["hy_cwT"][l].rr("i p c -> p i c"))
    k.dma("sp", cb, IN["hy_cbT"][l])
    k.dma("sp", hbias, IN["hy_biasT"][l])
    for b in range(NB):
        k.push()
        tabs_cur[0] = [[k.sb("hy_tabB%d%d" % (i, x), [128, NT, 128]) for x in range(2)] for i in range(3)]
        zTm = k.sb("hy_zTm", [128, NT, MIXW])
        raw = [k.sb("hy_raw%d" % i, [128, L + 2]) for i in range(2)]
        uu = [k.sb("hy_u%d" % i, [128, L]) for i in range(3)]
        ri = 0
        for i in range(4):
            for s in range(3):
                ch = s * 4 + i
                r = raw[ri % 2]
                ri += 1
                k.memset(r[:, 0:1], 0.0, x="pool")
                k.memset(r[:, L + 1:L + 2], 0.0, x="pool")
                k.dma("sp", r[:, 1:L + 1], P[b, OFF_HY + ch * 128:OFF_HY + (ch + 1) * 128, toff:toff + L])
                u = uu[s]
                k.act(u, r[:, 1:L + 1], AF.Identity, bias=cb[:, ch:ch + 1], scale=cw[:, 1, ch:ch + 1])
                k.stt(u, r[:, 0:L], cw[:, 0, ch:ch + 1], u, ALU.mult, ALU.add)
                k.stt(u, r[:, 2:L + 2], cw[:, 2, ch:ch + 1], u, ALU.mult, ALU.add)
            k.dma("pool", pr.S["HYx0"][b, i * 128:(i + 1) * 128, 0:L], uu[0])
            k.tt(uu[2], uu[2], uu[1], ALU.mult, x="pool")
            k.dma("pool", pr.S["HYz"][b, i * 128:(i + 1) * 128, 0:L], uu[2])
            for t4 in range(0, NT, 4):
                nn = min(4, NT - t4)
                pp = pw[(t4 // 4) % 2]
                for j in range(nn):
                    k.tr(pp[:, j * 128:(j + 1) * 128], uu[2][:, (t4 + j) * 128:(t4 + j + 1) * 128], I)
                k.cp(zTm[:, t4:t4 + nn, i * 128:(i + 1) * 128], pp[:, 0:nn * 128].rr("p (a b) -> p a b", b=128), x="act")
        uc = k.sb("hy_uc", [128, MIXW])
        us = k.sb("hy_us", [128, MIXW])
        yy = [[k.sb("hy_yy%d%d" % (i, x), [128, MIXW]) for x in range(2)] for i in range(2)]
        m1 = k.sb("hy_m1", [128, MIXW])
        for fc in range(NT):
            tb = load_tabs(fc)
            for x in range(2):
                pp = pw[x]
                for tc in range(NT):
                    k.mm(pp, tb[x][:, tc, :], zTm[:, tc, :], start=(tc == 0), stop=(tc == NT - 1))
            k.cp(uc, pw[0], x="act")
            k.cp(us, pw[1], x="act")
            y = yy[fc % 2]
            k.tt(y[0], uc, KC[:, fc, :], ALU.mult, x="dve")
            k.tt(m1, us, KS[:, fc, :], ALU.mult, x="pool")
            k.tt(y[0], y[0], m1, ALU.add, x="dve")
            k.tt(y[1], uc, KS[:, fc, :], ALU.mult, x="pool")
            k.tt(m1, us, KC[:, fc, :], ALU.mult, x="dve")
            k.tt(y[1], y[1], m1, ALU.subtract, x="pool")
            for x in range(2):
                k.dma("sp" if x == 0 else "pool", pr.S["HYY"][b, x, fc * 128:(fc + 1) * 128, :], y[x])
        k.pop()
        k.push()
        Y = [k.sb("hy_Y%d" % x, [128, NT, MIXW]) for x in range(2)]
        for x in range(2):
            k.dma("sp", Y[x], pr.S["HYY"][b, x, 0:L, :].rr("(fc p) c -> p fc c", p=128))
        itab = [[k.sb("hy_itab%d%d" % (i, x), [128, 512]) for x in range(2)] for i in range(6)]
        zt = [k.sb("hy_zt%d" % i, [128, 512]) for i in range(2)]
        x0t = [k.sb("hy_x0t%d" % i, [128, 512]) for i in range(2)]
        ot = [k.sb("hy_ot%d" % i, [128, 512]) for i in range(2)]
        ii = 0
        oi = 0
        for tt in range(NTT):
            for fc in range(NT):
                tb = itab[ii % 6]
                ii += 1
                k.dma("sp", tb[0][:, :TW], IN["idft_c" + sfx][fc * 128:(fc + 1) * 128, tt * TW:(tt + 1) * TW])
                k.dma("sp", tb[1][:, :TW], IN["idft_s" + sfx][fc * 128:(fc + 1) * 128, tt * TW:(tt + 1) * TW])
                for ci in range(4):
                    k.mm(pacc[ci][:, :TW], Y[0][:, fc, ci * 128:(ci + 1) * 128], tb[0][:, :TW],
                         start=(fc == 0), stop=False)
                    k.mm(pacc[ci][:, :TW], Y[1][:, fc, ci * 128:(ci + 1) * 128], tb[1][:, :TW],
                         start=False, stop=(fc == NT - 1))
            for ci in range(4):
                z = zt[oi % 2]
                x0 = x0t[oi % 2]
                o = ot[oi % 2]
                oi += 1
                k.dma("sp", z[:, :TW], pr.S["HYz"][b, ci * 128:(ci + 1) * 128, tt * TW:(tt + 1) * TW])
                k.dma("pool", x0[:, :TW], pr.S["HYx0"][b, ci * 128:(ci + 1) * 128, tt * TW:(tt + 1) * TW])
                k.stt(o[:, :TW], z[:, :TW], hbias[:, ci:ci + 1], pacc[ci][:, :TW], ALU.mult, ALU.add)
                k.tt(o[:, :TW], o[:, :TW], x0[:, :TW], ALU.mult, x="pool")
                k.dma("sp", ybr[b, ci * 128:(ci + 1) * 128, toff + tt * TW:toff + (tt + 1) * TW], o[:, :TW])
        k.pop()
    k.pop()


def ffn_declare(pr):
    pr.inp("w_branch", [DEPTH, 4, MIXW, D])
    pr.inp("w_out", [DEPTH, D, D])
    pr.inp("ffn_w1", [1, D, D_FF])
    pr.inp("ffn_w3", [1, D, D_FF])
    pr.inp("ffn_w2", [1, D_FF, D])
    pr.inp("moe_router_w", [1, D, NEXP])
    pr.inp("moe_router_b", [1, 1, NEXP])
    pr.inp("moe_w1", [1, NEXP, D, D_FFE])
    pr.inp("moe_w3", [1, NEXP, D, D_FFE])
    pr.inp("moe_w2", [1, NEXP, D_FFE, D])
    pr.inp("fin_gT", [128, 8])
    pr.out = pr.k.dram("out", [NB, D, TL], F32, kind="ExternalOutput")
    pr.scratch("XN2", [128, 8, NB * T], BF16)


def merge_stage(pr, l, hsrc, tiles):
    k = pr.k
    k.push()
    IN = pr.IN
    P = pr.S["P"]
    YBR = pr.S["YBR"]
    stg = k.sb("mg_stg", [128, 4, 1024])
    wbr = k.sb("mg_wbr", [128, 4, 4, 1024], BF16)
    wout = k.sb("mg_wout", [128, 8, 1024], BF16)
    for m in range(4):
        k.dma("sp", stg, IN["w_branch"][l, m].rr("(kc p) n -> p kc n", p=128))
        k.cp(wbr[:, m], stg, x=("act" if m % 2 == 0 else "pool"))
    for hf in range(2):
        k.dma("sp", stg, IN["w_out"][l, hf * 512:(hf + 1) * 512, :].rr("(kc p) n -> p kc n", p=128))
        k.cp(wout[:, hf * 4:(hf + 1) * 4], stg, x=("act" if hf == 0 else "pool"))
    acc = k.sb("mg_acc", [128, 8, 512])
    accb = k.sb("mg_accb", [128, 8, 512], BF16)
    yt = [k.sb("mg_y%d" % i, [128, 4, 512]) for i in range(1)]
    ybf = [k.sb("mg_ybf%d" % i, [128, 4, 512], BF16) for i in range(2)]
    gt = [k.sb("mg_g%d" % i, [128, 8, 512]) for i in range(2)]
    tmp = [k.sb("mg_tmp%d" % i, [128, 512]) for i in range(2)]
    ht = k.sb("mg_h", [128, 8, 512])
    hn = k.sb("mg_hn", [128, 8, 512])
    ps = [k.ps("mg_ps%d" % i, [128, 512]) for i in range(4)]
    it = 0
    pi = 0
    for (b, t0, n, is_ctx) in tiles:
        j = 2 if is_ctx else b
        k.dma("sp", ht[:, :, :n], hsrc[b, :, t0:t0 + n].rr("(kc p) t -> p kc t", p=128))
        for m in range(4):
            y = yt[0]
            yb = ybf[it % 2]
            g = gt[it % 2]
            it += 1
            k.dma("sp", y[:, :, :n], YBR[m, b, :, t0:t0 + n].rr("(kc p) t -> p kc t", p=128))
            k.dma("pool", g[:, :, :n], P[b, OFF_GATE + m * D:OFF_GATE + (m + 1) * D, t0:t0 + n].rr("(oc p) t -> p oc t", p=128))
            k.cp(yb[:, :, :n], y[:, :, :n], x="act")
            for oc in range(8):
                pp = ps[pi % 4]
                pi += 1
                for kc in range(4):
                    k.mm(pp[:, :n], wbr[:, m, kc, oc * 128:(oc + 1) * 128], yb[:, kc, :n], start=(kc == 0), stop=(kc == 3))
                if m == 0:
                    k.tt(acc[:, oc, :n], pp[:, :n], g[:, oc, :n], ALU.mult)
                else:
                    tp = tmp[pi % 2]
                    k.tt(tp[:, :n], pp[:, :n], g[:, oc, :n], ALU.mult)
                    k.tt(acc[:, oc, :n], acc[:, oc, :n], tp[:, :n], ALU.add, x="dve")
        k.cp(accb[:, :, :n], acc[:, :, :n], x="act")
        for oc in range(8):
            pp = ps[pi % 4]
            pi += 1
            for kc in range(8):
                k.mm(pp[:, :n], wout[:, kc, oc * 128:(oc + 1) * 128], accb[:, kc, :n], start=(kc == 0), stop=(kc == 7))
            k.stt(hn[:, oc, :n], pp[:, :n], pr.mod[l][:, 16 + oc, j:j + 1], ht[:, oc, :n], ALU.mult, ALU.add)
        k.dma("pool", pr.S["hbuf"][b, :, t0:t0 + n].rr("(kc p) t -> p kc t", p=128), hn[:, :, :n])
    k.pop()


def norm2_stage(pr, l, tiles, xn2, router=None, xn2_dram=None):
    k = pr.k
    k.push()
    hts = [k.sb("n2_ht%d" % i, [128, 8, 512]) for i in range(2)]
    sq = k.sb("n2_sq", [128, 8, 512])
    tmp = k.sb("n2_tmp", [128, 8, 512])
    rs = k.sb("n2_rs", [128, 512])
    pss = k.ps("n2_pss", [128, 512])
    if xn2 is None:
        xob = [k.sb("n2_xo%d" % i, [128, 8, 512], BF16) for i in range(2)]
    if router is not None:
        gate_tm, rw_, rb_ = router
        xf = k.sb("n2_xf", [128, 8, 512])
        plg = k.ps("n2_plg", [128, 512])
        lg = k.sb("n2_lg", [128, 8])
        e1 = k.sb("n2_e1", [128, 8])
        e2 = k.sb("n2_e2", [128, 8])
        l2 = k.sb("n2_l2", [128, 8])
        sc = k.sb("n2_sc", [128, 8])
    for i, (b, t0, n, is_ctx) in enumerate(tiles):
        ht = hts[i % 2]
        k.dma("sp", ht[:, :, :n], pr.S["hbuf"][b, :, t0:t0 + n].rr("(kc p) t -> p kc t", p=128))
        if xn2 is None:
            xo = xob[i % 2]
            pr.norm_tile(l, 2, ht, n, b, is_ctx, xo[:, :, :n], sq, pss, rs, tmp)
            k.dma("sp", xn2_dram[:, :, b * T + t0:b * T + t0 + n], xo[:, :, :n])
        else:
            pr.norm_tile(l, 2, ht, n, b, is_ctx, xn2[:, :, b * T + t0: b * T + t0 + n], sq, pss, rs, tmp)
        if router is not None:
            j = b
            for kc in range(8):
                k.act(xf[:, kc, :n], tmp[:, kc, :n], AF.Identity, bias=pr.mod[l][:, 24 + kc, j:j + 1], scale=1.0)
            for blk in range(n // 128):
                gblk = (b * TL + (t0 - TC)) // 128 + blk
                pl = plg[:, blk * 8:(blk + 1) * 8]
                for kc in range(8):
                    k.mm(pl, xf[:, kc, blk * 128:(blk + 1) * 128], rw_[:, kc, :], start=(kc == 0), stop=False)
                k.mm(pl, pr.ones[0:1, :], rb_, start=False, stop=True)
            for blk in range(n // 128):
                gblk = (b * TL + (t0 - TC)) // 128 + blk
                k.cp(lg, plg[:, blk * 8:(blk + 1) * 8])
                k.reduce(sc[:, 0:1], lg, ALU.max)
                k.ts(e1, lg, sc[:, 0:1], ALU.is_equal)
                k.stt(l2, e1, -1e30, lg, ALU.mult, ALU.add)
                k.reduce(sc[:, 1:2], l2, ALU.max)
                k.ts(e2, l2, sc[:, 1:2], ALU.is_equal)
                k.tt(sc[:, 2:3], sc[:, 1:2], sc[:, 0:1], ALU.subtract)
                k.act(sc[:, 3:4], sc[:, 2:3], AF.Exp)
                k.ts(sc[:, 4:5], sc[:, 3:4], 1.0, ALU.add)
                k.recip(sc[:, 5:6], sc[:, 4:5])
                k.tt(sc[:, 6:7], sc[:, 3:4], sc[:, 5:6], ALU.mult)
                k.ts(gate_tm[:, gblk, :], e1, sc[:, 5:6], ALU.mult)
                k.stt(gate_tm[:, gblk, :], e2, sc[:, 6:7], gate_tm[:, gblk, :], ALU.mult, ALU.add)
    k.pop()


def super_tiles(tiles, maxtok):
    sts = []
    cur = []
    tot = 0
    for t in tiles:
        if cur and (tot + t[2] > maxtok or cur[0][0] != t[0]):
            sts.append(cur)
            cur, tot = [], 0
        cur.append(t)
        tot += t[2]
    sts.append(cur)
    return sts


def swiglu_stage(pr, l, tiles, xn2, w1, w3, w2, dff, maxtok, experts=None, gate_tm=None, xn2_dram=None):
    k = pr.k
    k.push()
    nff = dff // 128
    I = pr.ident
    actb = k.sb("ff_act", [128, nff, maxtok], BF16)
    NW = 6
    wst = [k.sb("ff_wst%d" % i, [128, 8, 128]) for i in range(NW)]
    wbf = [k.sb("ff_wbf%d" % i, [128, 8, 128], BF16) for i in range(NW)]
    w2st = [k.sb("ff_w2st%d" % i, [128, nff, 128]) for i in range(1)]
    w2bf = [k.sb("ff_w2bf%d" % i, [128, nff, 128], BF16) for i in range(2)]
    st1 = [k.sb("ff_s1_%d" % i, [128, 512]) for i in range(2)]
    pa = [k.ps("ff_pa%d" % i, [128, 512]) for i in range(2)]
    pb = [k.ps("ff_pb%d" % i, [128, 512]) for i in range(2)]
    po = [k.ps("ff_po%d" % i, [128, 512]) for i in range(2)]
    ht = [k.sb("ff_h%d" % i, [128, 512]) for i in range(2)]
    hn = [k.sb("ff_hn%d" % i, [128, 512]) for i in range(2)]
    ne = 1 if experts is None else experts
    if experts is not None:
        acc = k.sb("ff_acc", [128, 8, maxtok])
        GE = [k.sb("ff_GE%d" % i, [128, maxtok]) for i in range(2)]
        gbc = [k.sb("ff_gbc%d" % i, [128, 128]) for i in range(2)]
        tmpg = [k.sb("ff_tg%d" % i, [128, 512]) for i in range(2)]
        pg = k.ps("ff_pg", [128, 512])
    if xn2 is None:
        xloc = k.sb("ff_xloc", [128, 8, maxtok], BF16)
    wi = 0
    w2i = 0
    si = 0
    hi_ = 0
    gi_ = 0
    for st in super_tiles(tiles, maxtok):
        offs = []
        o = 0
        for t in st:
            offs.append(o)
            o += t[2]
        if xn2 is None:
            for ti, (b, t0, n, is_ctx) in enumerate(st):
                k.dma("sp", xloc[:, :, offs[ti]:offs[ti] + n], xn2_dram[:, :, b * T + t0:b * T + t0 + n])
        for e in range(ne):
            W1 = w1 if experts is None else w1[e]
            W3 = w3 if experts is None else w3[e]
            W2 = w2 if experts is None else w2[e]
            if experts is not None:
                ge = GE[e % 2]
                for ti, (b, t0, n, is_ctx) in enumerate(st):
                    for blk in range(n // 128):
                        gblk = (b * TL + (t0 - TC)) // 128 + blk
                        gb_ = gbc[gi_ % 2]
                        gi_ += 1
                        k.cp(gb_, gate_tm[:, gblk, e:e + 1].bc([128, 128]), x="pool")
                        k.mm(pg[:, blk * 128:(blk + 1) * 128], gb_, I)
                    k.cp(ge[:, offs[ti]:offs[ti] + n], pg[:, :n], x="act")
            for ffc in range(nff):
                ws = []
                for wsrc in (W1, W3):
                    s_, b_ = wst[wi % NW], wbf[wi % NW]
                    k.dma("sp", s_, wsrc[:, ffc * 128:(ffc + 1) * 128].rr("(kc p) n -> p kc n", p=128))
                    k.cp(b_, s_, x=("dve" if wi % 2 == 0 else "act"))
                    wi += 1
                    ws.append(b_)
                for ti, (b, t0, n, is_ctx) in enumerate(st):
                    if xn2 is None:
                        xs = xloc[:, :, offs[ti]:offs[ti] + n]
                    else:
                        xs = xn2[:, :, b * T + t0:b * T + t0 + n]
                    p1, p3 = pa[si % 2], pb[si % 2]
                    s1 = st1[si % 2]
                    si += 1
                    for kc in range(8):
                        k.mm(p1[:, :n], ws[0][:, kc, :], xs[:, kc, :], start=(kc == 0), stop=(kc == 7))
                    for kc in range(8):
                        k.mm(p3[:, :n], ws[1][:, kc, :], xs[:, kc, :], start=(kc == 0), stop=(kc == 7))
                    k.act(s1[:, :n], p1[:, :n], AF.Silu)
                    k.tt(actb[:, ffc, offs[ti]:offs[ti] + n], s1[:, :n], p3[:, :n], ALU.mult)
            for oc in range(8):
                s_, b_ = w2st[0], w2bf[w2i % 2]
                k.dma("sp", s_, W2[:, oc * 128:(oc + 1) * 128].rr("(kc p) n -> p kc n", p=128))
                k.cp(b_, s_, x=("act" if w2i % 2 == 0 else "dve"))
                w2i += 1
                for ti, (b, t0, n, is_ctx) in enumerate(st):
                    pp = po[hi_ % 2]
                    for kc in range(nff):
                        k.mm(pp[:, :n], b_[:, kc, :], actb[:, kc, offs[ti]:offs[ti] + n], start=(kc == 0), stop=(kc == nff - 1))
                    j = 2 if is_ctx else b
                    h_, hn_ = ht[hi_ % 2], hn[hi_ % 2]
                    hi_ += 1
                    hv = pr.S["hbuf"][b, oc * 128:(oc + 1) * 128, t0:t0 + n]
                    if experts is not None:
                        a_ = acc[:, oc, offs[ti]:offs[ti] + n]
                        g_ = GE[e % 2][:, offs[ti]:offs[ti] + n]
                        if e == 0:
                            k.tt(a_, pp[:, :n], g_, ALU.mult)
                        else:
                            tg = tmpg[hi_ % 2]
                            k.tt(tg[:, :n], pp[:, :n], g_, ALU.mult)
                            k.tt(a_, a_, tg[:, :n], ALU.add, x="pool")
                        if e < ne - 1:
                            continue
                        k.dma("sp", h_[:, :n], hv)
                        k.stt(hn_[:, :n], a_, pr.mod[l][:, 40 + oc, j:j + 1], h_[:, :n], ALU.mult, ALU.add)
                    else:
                        k.dma("sp", h_[:, :n], hv)
                        k.stt(hn_[:, :n], pp[:, :n], pr.mod[l][:, 40 + oc, j:j + 1], h_[:, :n], ALU.mult, ALU.add)
                    k.dma("sp", hv, hn_[:, :n])
    k.pop()


def final_stage(pr):
    k = pr.k
    k.push()
    fg = k.sb("fin_g", [128, 8])
    k.dma("sp", fg, pr.IN["fin_gT"])
    hts = [k.sb("fin_ht%d" % i, [128, 8, 512]) for i in range(2)]
    sq = k.sb("fin_sq", [128, 8, 512])
    ot = [k.sb("fin_o%d" % i, [128, 8, 512]) for i in range(2)]
    rs = k.sb("fin_rs", [128, 512])
    pss = k.ps("fin_pss", [128, 512])
    i = 0
    for (b, t0, n, is_ctx) in tok_tiles():
        if is_ctx:
            continue
        ht = hts[i % 2]
        o = ot[i % 2]
        i += 1
        k.dma("sp", ht, pr.S["hbuf"][b, :, t0:t0 + n].rr("(kc p) t -> p kc t", p=128))
        k.act(sq, ht, AF.Square)
        for kc in range(8):
            k.mm(pss, pr.ones, sq[:, kc, :], start=(kc == 0), stop=(kc == 7))
        k.ts(rs, pss, 1.0 / D, ALU.mult, RMS_EPS, ALU.add)
        k.act(rs, rs, AF.Sqrt)
        k.recip(rs, rs)
        for kc in range(8):
            k.stt(o[:, kc, :], ht[:, kc, :], fg[:, kc:kc + 1], rs, ALU.mult, ALU.mult)
        k.dma("pool", pr.out[b, :, t0 - TC:t0 - TC + n].rr("(kc p) t -> p kc t", p=128), o)
    k.pop()


_PROG = {}


def kernel(**inputs):
    if "nc" not in _PROG:
        nc = bass.Bass("TRN2", target_bir_lowering=False)
        pr = Prog(nc)
        pr.build()
        _PROG["nc"] = nc
        _PROG["names"] = set(pr.IN.keys())
    nc = _PROG["nc"]
    sh = shared_inputs(inputs)
    maps = []
    for c in range(NCORE):
        m = dict(sh)
        m.update(core_inputs(inputs, c))
        maps.append({k_: v for k_, v in m.items() if k_ in _PROG["names"]})
    res = run_bass_kernel_spmd(nc, maps, core_ids=list(range(NCORE)))
    outs = [np.asarray(r["out"]) for r in res.results]
    full = np.concatenate(outs, axis=0)
    return np.ascontiguousarray(full.transpose(0, 2, 1)).astype(np.float32)
```
